# Optimizing a Trainium2 kernel written in Bass

```python
import jax
import jax.numpy as jnp
from jax import lax
import numpy as np


D_MODEL = 2048
BATCH = 8
SEQ = 2048
DEPTH = 4

A_HEADS = 8
A_HEAD_DIM = 128
A_WIDTH = A_HEADS * A_HEAD_DIM
MOBA_BLOCK = 256
MOBA_TOPK = 3
MOBA_Q_CHUNK = 8
B_HEADS = 16
B_HEAD_DIM = 64
B_WIDTH = B_HEADS * B_HEAD_DIM
LORA_W = 64
LORA_A = 64
LORA_G = 160
LORA_V = 32
C_WIDTH = D_MODEL
CONV_WIDTH = 3
D_FF = 5632
RMS_EPS = 1e-6
GN_EPS = 64e-5
NEG_INF = -1e30

kernel_name = 'moba_rwkv7_shortconv_hybrid'


def rms_norm(x, gain):
    xf = x.astype(jnp.float32)
    y = xf * lax.rsqrt(jnp.mean(xf * xf, axis=-1, keepdims=True) + RMS_EPS)
    return y.astype(x.dtype) * gain


def causal_dwconv(u, w):
    seq = u.shape[1]
    u_pad = jnp.pad(u, ((0, 0), (CONV_WIDTH - 1, 0), (0, 0)))
    out = w[0] * u_pad[:, :seq]
    for j in range(1, CONV_WIDTH):
        out = out + w[j] * u_pad[:, j:j + seq]
    return out


def alibi_slopes(n_heads):
    return jnp.exp2(-8.0 * jnp.arange(1, n_heads + 1, dtype=jnp.float32) / n_heads)


def moba_attention(q, k, v, q_gain, k_gain):
    bsz, seq, nh, hd = q.shape
    qn = rms_norm(q, q_gain).astype(jnp.float32).transpose(0, 2, 1, 3)
    kn = rms_norm(k, k_gain).astype(jnp.float32).transpose(0, 2, 1, 3)
    vh = v.astype(jnp.float32).transpose(0, 2, 1, 3)
    nb = -(-seq // MOBA_BLOCK)
    pad = nb * MOBA_BLOCK - seq
    kb = jnp.pad(kn, ((0, 0), (0, 0), (0, pad), (0, 0))).reshape(bsz, nh, nb, MOBA_BLOCK, hd)
    vb = jnp.pad(vh, ((0, 0), (0, 0), (0, pad), (0, 0))).reshape(bsz, nh, nb, MOBA_BLOCK, hd)
    k_mean = jnp.mean(kb, axis=3)
    n_sel = min(MOBA_TOPK, nb)
    slopes = alibi_slopes(nh)
    scale = hd ** -0.5
    b_ix = jnp.arange(bsz)[:, None, None, None]
    h_ix = jnp.arange(nh)[None, :, None, None]
    key_off = jnp.arange(MOBA_BLOCK)
    blk_ids = jnp.arange(nb)

    def chunk(c):
        t0 = c * MOBA_Q_CHUNK
        blk = t0 // MOBA_BLOCK
        qc = lax.dynamic_slice_in_dim(qn, t0, MOBA_Q_CHUNK, axis=2)
        t_pos = t0 + jnp.arange(MOBA_Q_CHUNK)
        gate = jnp.einsum('bhqd,bhnd->bhqn', qc, k_mean)
        gate = jnp.where(blk_ids < blk, gate, NEG_INF)
        _, sel = lax.top_k(gate, n_sel)
        valid = sel < blk
        k_sel = kb[b_ix, h_ix, sel]
        v_sel = vb[b_ix, h_ix, sel]
        s_past = jnp.einsum('bhqd,bhqjsd->bhqjs', qc, k_sel) * scale
        dist_past = (t_pos[:, None, None] - (sel[..., None] * MOBA_BLOCK + key_off)).astype(jnp.float32)
        s_past = jnp.where(valid[..., None], s_past - slopes[:, None, None, None] * dist_past, NEG_INF)
        k_own = lax.dynamic_index_in_dim(kb, blk, axis=2, keepdims=False)
        v_own = lax.dynamic_index_in_dim(vb, blk, axis=2, keepdims=False)
        s_own = jnp.einsum('bhqd,bhsd->bhqs', qc, k_own) * scale
        dist_own = (t_pos[:, None] - (blk * MOBA_BLOCK + key_off)[None, :]).astype(jnp.float32)
        s_own = jnp.where(dist_own >= 0, s_own - slopes[:, None, None] * dist_own, NEG_INF)
        scores = jnp.concatenate([s_past.reshape(bsz, nh, MOBA_Q_CHUNK, n_sel * MOBA_BLOCK), s_own], axis=-1)
        probs = jax.nn.softmax(scores, axis=-1)
        p_past = probs[..., :n_sel * MOBA_BLOCK].reshape(bsz, nh, MOBA_Q_CHUNK, n_sel, MOBA_BLOCK)
        p_own = probs[..., n_sel * MOBA_BLOCK:]
        return (jnp.einsum('bhqjs,bhqjsd->bhqd', p_past, v_sel)
                + jnp.einsum('bhqs,bhsd->bhqd', p_own, v_own))

    out = lax.map(chunk, jnp.arange(seq // MOBA_Q_CHUNK))
    out = out.transpose(1, 0, 3, 2, 4).reshape(bsz, seq, nh * hd)
    return out.astype(q.dtype)


def rwkv7_step(state, inp):
    r, w, k, v, a, b = inp
    sa = jnp.einsum('bhvk,bhk->bhv', state, a)
    state = state * w[:, :, None, :] + sa[..., None] * b[:, :, None, :] + v[..., None] * k[:, :, None, :]
    y = jnp.einsum('bhvk,bhk->bhv', state, r)
    return state, y


def rwkv7_time_mix(p, shift_mu, w0, w_lora, a0, a_lora, g_lora, k_k, k_a, r_k, gn_w, gn_b,
                   v0, v_lora, v_first):
    bsz, seq, _ = p.shape
    p_prev = jnp.pad(p, ((0, 0), (1, 0), (0, 0)))[:, :seq]
    p = p + shift_mu * (p_prev - p)
    r = p[..., :B_WIDTH]
    k = p[..., B_WIDTH:2 * B_WIDTH]
    v = p[..., 2 * B_WIDTH:3 * B_WIDTH]
    o = 3 * B_WIDTH
    z_w = p[..., o:o + LORA_W]
    o = o + LORA_W
    z_a = p[..., o:o + LORA_A]
    o = o + LORA_A
    z_g = p[..., o:o + LORA_G]
    o = o + LORA_G
    w = -jax.nn.softplus(-(w0 + jnp.tanh(z_w) @ w_lora)) - 0.5
    a = jax.nn.sigmoid(a0 + z_a @ a_lora)
    g = jax.nn.sigmoid(z_g) @ g_lora
    if v_lora is None:
        v_first = v
    else:
        z_v = p[..., o:o + LORA_V]
        v = v + (v_first - v) * jax.nn.sigmoid(v0 + z_v @ v_lora)

    def heads(t):
        return t.reshape(bsz, seq, B_HEADS, B_HEAD_DIM).astype(jnp.float32)

    kk = heads(k * k_k)
    kk = kk / jnp.maximum(jnp.sqrt(jnp.sum(kk * kk, axis=-1, keepdims=True)), 1e-12)
    k = k * (1.0 + (a - 1.0) * k_a)
    rh, kh, vh, ah = heads(r), heads(k), heads(v), heads(a)
    decay = jnp.exp(-jnp.exp(heads(w)))
    xs = tuple(jnp.moveaxis(t, 1, 0) for t in (rh, decay, kh, vh, -kk, kk * ah))
    state0 = jnp.zeros((bsz, B_HEADS, B_HEAD_DIM, B_HEAD_DIM), jnp.float32)
    _, y = lax.scan(rwkv7_step, state0, xs)
    y = jnp.moveaxis(y, 0, 1)
    mean = jnp.mean(y, axis=-1, keepdims=True)
    var = jnp.mean(jnp.square(y - mean), axis=-1, keepdims=True)
    y = ((y - mean) * lax.rsqrt(var + GN_EPS)).reshape(bsz, seq, B_WIDTH) * gn_w + gn_b
    bonus = jnp.sum(rh * kh * r_k, axis=-1, keepdims=True) * vh
    y = (y + bonus.reshape(bsz, seq, B_WIDTH)) * g
    return y.astype(p.dtype), v_first


def moba_rwkv_layer(x, norm_mix, w_in, q_gain, k_gain, shift_mu, w0, w_lora, a0, a_lora, g_lora,
                    k_k, k_a, r_k, gn_w, gn_b, w_out, v0, v_lora, v_first):
    bsz, seq, _ = x.shape
    proj = rms_norm(x, norm_mix) @ w_in
    qkv = proj[..., :3 * A_WIDTH].reshape(bsz, seq, 3, A_HEADS, A_HEAD_DIM)
    y_a = moba_attention(qkv[:, :, 0], qkv[:, :, 1], qkv[:, :, 2], q_gain, k_gain)
    y_b, v_first = rwkv7_time_mix(proj[..., 3 * A_WIDTH:], shift_mu, w0, w_lora, a0, a_lora, g_lora,
                                  k_k, k_a, r_k, gn_w, gn_b, v0, v_lora, v_first)
    y = jnp.concatenate([y_a, y_b], axis=-1) @ w_out
    return x + y, v_first


def short_conv_layer(x, norm_mix, conv_in, conv_w, conv_out):
    h = rms_norm(x, norm_mix) @ conv_in
    gate_b, gate_c, u = jnp.split(h, 3, axis=-1)
    return x + (gate_b * causal_dwconv(gate_c * u, conv_w)) @ conv_out


def conv_ffn(x, norm_ffn, ffn_up, ffn_conv, ffn_down):
    h = causal_dwconv(rms_norm(x, norm_ffn) @ ffn_up, ffn_conv)
    gate, val = jnp.split(h, 2, axis=-1)
    return x + (jax.nn.silu(gate) * val) @ ffn_down


def setup_inputs(seed: int = 0) -> dict:
    key = jax.random.key(seed)
    keys = list(jax.random.split(key, 128))

    def nrm(shape, scale):
        return scale * jax.random.normal(keys.pop(), shape, jnp.float32)

    def gain(shape):
        return 1.0 + nrm(shape, 0.02)

    def unif(shape, lo, hi):
        return jax.random.uniform(keys.pop(), shape, jnp.float32, lo, hi)

    inputs = {'x': nrm((BATCH, SEQ, D_MODEL), 1.0)}
    for li in range(DEPTH):
        p = 'l%d_' % li
        inputs[p + 'norm_mix'] = gain((D_MODEL,))
        if li % 2 == 0:
            first = li == 0
            lora_cols = LORA_W + LORA_A + LORA_G + (0 if first else LORA_V)
            rwkv_cols = 3 * B_WIDTH + lora_cols
            inputs[p + 'w_in'] = nrm((D_MODEL, 3 * A_WIDTH + rwkv_cols), D_MODEL ** -0.5)
            inputs[p + 'q_gain'] = gain((A_HEAD_DIM,))
            inputs[p + 'k_gain'] = gain((A_HEAD_DIM,))
            inputs[p + 'shift_mu'] = unif((rwkv_cols,), 0.2, 0.8)
            inputs[p + 'w0'] = unif((B_WIDTH,), -6.0, -1.0)
            inputs[p + 'w_lora'] = nrm((LORA_W, B_WIDTH), 0.5 * LORA_W ** -0.5)
            inputs[p + 'a0'] = nrm((B_WIDTH,), 0.1)
            inputs[p + 'a_lora'] = nrm((LORA_A, B_WIDTH), LORA_A ** -0.5)
            inputs[p + 'g_lora'] = nrm((LORA_G, B_WIDTH), LORA_G ** -0.5)
            inputs[p + 'k_k'] = 0.85 + nrm((B_WIDTH,), 0.05)
            inputs[p + 'k_a'] = 1.0 + nrm((B_WIDTH,), 0.05)
            inputs[p + 'r_k'] = nrm((B_HEADS, B_HEAD_DIM), 0.1)
            inputs[p + 'gn_w'] = gain((B_WIDTH,))
            inputs[p + 'gn_b'] = nrm((B_WIDTH,), 0.02)
            inputs[p + 'w_out'] = nrm((A_WIDTH + B_WIDTH, D_MODEL), (A_WIDTH + B_WIDTH) ** -0.5)
            if not first:
                inputs[p + 'v0'] = nrm((B_WIDTH,), 0.1)
                inputs[p + 'v_lora'] = nrm((LORA_V, B_WIDTH), LORA_V ** -0.5)
        else:
            inputs[p + 'conv_in'] = nrm((D_MODEL, 3 * C_WIDTH), D_MODEL ** -0.5)
            inputs[p + 'conv_w'] = nrm((CONV_WIDTH, C_WIDTH), 0.5)
            inputs[p + 'conv_out'] = nrm((C_WIDTH, D_MODEL), C_WIDTH ** -0.5)
        inputs[p + 'norm_ffn'] = gain((D_MODEL,))
        inputs[p + 'ffn_up'] = nrm((D_MODEL, 2 * D_FF), D_MODEL ** -0.5)
        inputs[p + 'ffn_conv'] = nrm((CONV_WIDTH, 2 * D_FF), 0.5)
        inputs[p + 'ffn_down'] = nrm((D_FF, D_MODEL), D_FF ** -0.5)
    return inputs


def reference(x,
              l0_norm_mix, l0_w_in, l0_q_gain, l0_k_gain, l0_shift_mu, l0_w0, l0_w_lora, l0_a0,
              l0_a_lora, l0_g_lora, l0_k_k, l0_k_a, l0_r_k, l0_gn_w, l0_gn_b, l0_w_out,
              l0_norm_ffn, l0_ffn_up, l0_ffn_conv, l0_ffn_down,
              l1_norm_mix, l1_conv_in, l1_conv_w, l1_conv_out,
              l1_norm_ffn, l1_ffn_up, l1_ffn_conv, l1_ffn_down,
              l2_norm_mix, l2_w_in, l2_q_gain, l2_k_gain, l2_shift_mu, l2_w0, l2_w_lora, l2_a0,
              l2_a_lora, l2_g_lora, l2_k_k, l2_k_a, l2_r_k, l2_gn_w, l2_gn_b, l2_w_out,
              l2_v0, l2_v_lora,
              l2_norm_ffn, l2_ffn_up, l2_ffn_conv, l2_ffn_down,
              l3_norm_mix, l3_conv_in, l3_conv_w, l3_conv_out,
              l3_norm_ffn, l3_ffn_up, l3_ffn_conv, l3_ffn_down):
    mix_layers = (
        (l0_norm_mix, l0_w_in, l0_q_gain, l0_k_gain, l0_shift_mu, l0_w0, l0_w_lora, l0_a0,
         l0_a_lora, l0_g_lora, l0_k_k, l0_k_a, l0_r_k, l0_gn_w, l0_gn_b, l0_w_out, None, None),
        (l1_norm_mix, l1_conv_in, l1_conv_w, l1_conv_out),
        (l2_norm_mix, l2_w_in, l2_q_gain, l2_k_gain, l2_shift_mu, l2_w0, l2_w_lora, l2_a0,
         l2_a_lora, l2_g_lora, l2_k_k, l2_k_a, l2_r_k, l2_gn_w, l2_gn_b, l2_w_out, l2_v0, l2_v_lora),
        (l3_norm_mix, l3_conv_in, l3_conv_w, l3_conv_out),
    )
    ffn_layers = (
        (l0_norm_ffn, l0_ffn_up, l0_ffn_conv, l0_ffn_down),
        (l1_norm_ffn, l1_ffn_up, l1_ffn_conv, l1_ffn_down),
        (l2_norm_ffn, l2_ffn_up, l2_ffn_conv, l2_ffn_down),
        (l3_norm_ffn, l3_ffn_up, l3_ffn_conv, l3_ffn_down),
    )
    v_first = None
    for li in range(DEPTH):
        if li % 2 == 0:
            x, v_first = moba_rwkv_layer(x, *mix_layers[li], v_first)
        else:
            x = short_conv_layer(x, *mix_layers[li])
        x = conv_ffn(x, *ffn_layers[li])
    return x
```

```python
import os
import numpy as np
from contextlib import ExitStack
import concourse.bass as bass
import concourse.mybir as mybir
from concourse.bass_utils import run_bass_kernel_spmd

F32 = mybir.dt.float32
BF16 = mybir.dt.bfloat16
AF = mybir.ActivationFunctionType
ALU = mybir.AluOpType
AX = mybir.AxisListType


class Sem:
    __slots__ = ("total", "handle", "name")

    def __init__(self, name):
        self.total = 0
        self.handle = None
        self.name = name


class Buf:
    __slots__ = ("name", "w_eng", "w_dma", "r_eng", "r_dma", "p_eng", "p_dma", "sem")

    def __init__(self, name):
        self.name = name
        self.w_eng = {}
        self.w_dma = []
        self.r_eng = {}
        self.r_dma = []
        self.p_eng = {}
        self.p_dma = []
        self.sem = None


class Prog:
    ENGS = ("pe", "act", "dve", "pool", "sp")

    def __init__(self, nc, es):
        self.nc = nc
        self.es = es
        self.ins = []
        self.nbuf = 0
        self.fence_idx = None
        self.local = []
        self.last_eng = {}
        self.dma_since = []
        self.free_sems = []
        self.all_sems = []

    def buf(self, name, persistent=False):
        self.nbuf += 1
        b = Buf("%s_%d" % (name, self.nbuf))
        if self.fence_idx is not None:
            b.p_eng = {"sp": self.fence_idx}
        if not persistent:
            self.local.append(b)
        return b

    def bufs(self, name, n, persistent=False):
        return [self.buf("%s%d" % (name, i), persistent) for i in range(n)]

    def fence(self):
        idx = len(self.ins)
        deps = set(self.last_eng.values()) | set(self.dma_since)
        rec = dict(eng="sp", fn=lambda e: e.nop(), deps=deps, dma=False, sem=None, total_at={},
                   signal=False)
        for d in deps:
            dr = self.ins[d]
            if dr["dma"]:
                rec["total_at"][d] = dr["sem"].total
        self.ins.append(rec)
        self.last_eng["sp"] = idx
        self.fence_idx = idx
        self.dma_since = []
        for b in self.local:
            if b.sem is not None:
                self.free_sems.append(b.sem)
                b.sem = None
        self.local = []

    def op(self, eng, fn, reads=(), writes=(), partial=(), dma_owner=None):
        idx = len(self.ins)
        deps = set()
        for b in reads:
            deps.update(b.w_eng.values())
            deps.update(b.w_dma)
        for b in writes:
            deps.update(b.w_eng.values())
            deps.update(b.w_dma)
            deps.update(b.r_eng.values())
            deps.update(b.r_dma)
            deps.update(b.p_eng.values())
            deps.update(b.p_dma)
        for b in partial:
            if b.r_eng or b.r_dma:
                b.p_eng, b.p_dma = b.r_eng, b.r_dma
                b.r_eng, b.r_dma = {}, []
                b.w_eng, b.w_dma = {}, []
            deps.update(b.p_eng.values())
            deps.update(b.p_dma)
        is_dma = dma_owner is not None
        rec = dict(eng=eng, fn=fn, deps=deps, dma=is_dma, sem=None, total_at={}, signal=False)
        for d in deps:
            dr = self.ins[d]
            if dr["dma"]:
                rec["total_at"][d] = dr["sem"].total
        if is_dma:
            if dma_owner.sem is None:
                kind = "sw" if eng == "pool" else "hw"
                pool = [s for s in self.free_sems if s.name.startswith(kind)]
                if pool:
                    dma_owner.sem = pool[-1]
                    self.free_sems.remove(pool[-1])
                else:
                    dma_owner.sem = Sem("%s%d" % (kind, len(self.all_sems)))
                    self.all_sems.append(dma_owner.sem)
            else:
                assert dma_owner.sem.name.startswith("sw") == (eng == "pool"), dma_owner.name
            rec["sem"] = dma_owner.sem
            dma_owner.sem.total += 16
            rec["tok"] = dma_owner.sem.total
            self.dma_since.append(idx)
        self.ins.append(rec)
        self.last_eng[eng] = idx
        for b in reads:
            if is_dma:
                b.r_dma.append(idx)
            else:
                b.r_eng[eng] = idx
        for b in writes:
            b.w_eng, b.w_dma, b.r_eng, b.r_dma, b.p_eng, b.p_dma = {}, [], {}, [], {}, []
            if is_dma:
                b.w_dma.append(idx)
            else:
                b.w_eng[eng] = idx
        for b in partial:
            if is_dma:
                b.w_dma.append(idx)
            else:
                b.w_eng[eng] = idx
        return idx

    def emit(self, final_waits=()):
        nc = self.nc
        es = self.es
        ins = self.ins
        for r in ins:
            for d in r["deps"]:
                dr = ins[d]
                if not dr["dma"]:
                    if dr["eng"] == "pe" and r["eng"] == "pe" and not r["dma"]:
                        continue
                    dr["signal"] = True
        esem = {e: es.enter_context(nc.semaphore("s_" + e)) for e in self.ENGS}
        cnt = {e: 0 for e in self.ENGS}
        for r in ins:
            if not r["dma"] and r["signal"]:
                cnt[r["eng"]] += 1
                r["cnt"] = cnt[r["eng"]]
        for s in self.all_sems:
            s.handle = es.enter_context(nc.semaphore(s.name))
        self.n_dma_sems = len(self.all_sems)
        per_eng = {e: [] for e in self.ENGS}
        for i, r in enumerate(ins):
            per_eng[r["eng"]].append(i)
        last_outs = [(b.sem.handle, b.sem.total) for b in final_waits if b.sem is not None]

        def run_engine(ename, eng):
            waited = {}
            for i in per_eng[ename]:
                r = ins[i]
                need = {}
                for d in r["deps"]:
                    dr = ins[d]
                    if dr["dma"]:
                        s = dr["sem"].handle
                        v = max(r["total_at"][d], dr["tok"])
                    else:
                        if dr["eng"] == "pe" and ename == "pe" and not r["dma"]:
                            continue
                        s = esem[dr["eng"]]
                        v = dr["cnt"]
                    key = id(s)
                    if key not in need or need[key][1] < v:
                        need[key] = (s, v)
                for key, (s, v) in need.items():
                    if waited.get(key, 0) >= v:
                        continue
                    eng.wait_ge(s, v)
                    waited[key] = v
                bi = r["fn"](eng)
                if r["dma"]:
                    bi.then_inc(r["sem"].handle, 16)
                elif r["signal"]:
                    bi.then_inc(esem[ename], 1)
            if ename == "sp":
                for h, v in last_outs:
                    eng.wait_ge(h, v)

        with nc.Block() as block:
            @block.tensor
            def _(e):
                run_engine("pe", e)

            @block.scalar
            def _(e):
                run_engine("act", e)

            @block.vector
            def _(e):
                run_engine("dve", e)

            @block.gpsimd
            def _(e):
                run_engine("pool", e)

            @block.sync
            def _(e):
                run_engine("sp", e)


T = 2048
D = 2048
NCH = D // 128
DFF = 5632
NJ = DFF // 128
RMS_EPS = 1e-6
RSTOP = int(os.environ.get('RSTOP', 99))
R8SUB = int(os.environ.get('R8SUB', 0))
GN_EPS = 64e-5
DBG_N = 16384
import os
NTT = int(os.environ.get('NTT', T // 128))
SB_BASE = 16640
SB_LIMIT = 229000


def _dtsize(dt):
    return 4 if dt == F32 else 2


class Arena:
    def __init__(self, nc):
        self.nc = nc
        self.off = SB_BASE
        self.n = 0
        self.peak = 0

    def alloc(self, shape, dtype):
        nb = _dtsize(dtype)
        for s in shape[1:]:
            nb *= s
        off = (self.off + 31) // 32 * 32
        h = self.nc.alloc_sbuf_tensor_at("t%d" % self.n, list(shape), dtype, offset=off)
        self.n += 1
        self.off = off + nb
        self.peak = max(self.peak, self.off)
        assert self.off <= getattr(self, "limit", SB_LIMIT), ("SBUF overflow", self.off)
        return h

    def mark(self):
        return self.off

    def reset(self, m):
        self.off = m


def pack_vec(v):
    v = np.asarray(v, np.float32).reshape(-1)
    n = (v.size + 127) // 128
    if v.size != n * 128:
        v = np.concatenate([v, np.zeros(n * 128 - v.size, np.float32)])
    return np.ascontiguousarray(v.reshape(n, 128).T)


class VecPack:
    def __init__(self):
        self.cols = {}
        self.n = 0
        self.parts = []

    def add(self, name, arr2d):
        self.cols[name] = (self.n, arr2d.shape[1])
        self.n += arr2d.shape[1]
        self.parts.append(arr2d)

    def array(self):
        return np.ascontiguousarray(np.concatenate(self.parts, axis=1).astype(np.float32))


def layer_vec_layout(li):
    lay = {}
    n = 0

    def add(name, c):
        nonlocal n
        lay[name] = (n, c)
        n += c
    add("norm_mix", 16)
    if li % 2 == 1:
        add("conv_w", 48)
    else:
        add("q_gain", 1)
        add("k_gain", 1)
        nmu = 27
        add("shift_mu", nmu)
        for nm in ("w0", "a0", "k_k", "k_a", "r_k", "gn_w", "gn_b", "v0"):
            add(nm, 8)
    add("norm_ffn", 16)
    add("ffn_conv", 3 * 88)
    return lay, n


def build_layer_vecs(li, inputs):
    p = "l%d_" % li
    lay, n = layer_vec_layout(li)
    out = np.zeros((128, n), np.float32)

    def put(name, arr2d):
        c, w = lay[name]
        assert arr2d.shape[1] <= w, (name, arr2d.shape, w)
        out[:, c:c + arr2d.shape[1]] = arr2d
    put("norm_mix", pack_vec(inputs[p + "norm_mix"]))
    if li % 2 == 1:
        cw = np.asarray(inputs[p + "conv_w"])
        put("conv_w", np.concatenate([pack_vec(cw[j]) for j in range(3)], axis=1))
    else:
        put("q_gain", pack_vec(inputs[p + "q_gain"]))
        put("k_gain", pack_vec(inputs[p + "k_gain"]))
        put("shift_mu", pack_vec(inputs[p + "shift_mu"]))
        for nm in ("w0", "a0", "k_k", "k_a", "gn_w", "gn_b"):
            put(nm, pack_vec(inputs[p + nm]))
        put("r_k", pack_vec(np.asarray(inputs[p + "r_k"]).reshape(-1)))
        if li > 0:
            put("v0", pack_vec(inputs[p + "v0"]))
    put("norm_ffn", pack_vec(inputs[p + "norm_ffn"]))
    fc = np.asarray(inputs[p + "ffn_conv"])
    put("ffn_conv", np.concatenate([pack_vec(fc[j]) for j in range(3)], axis=1))
    return out


C_IDENT = 0
C_BONES = 128
C_BIASCOL = 256
C_GMASK = 264
C_AMASK = 328
C_NTMASK = 840
C_ONE64 = 1352
C_ID8 = 1416
NCONST = 1928
B_IDENT = 0
B_ONES = 128
B_CAUSAL = 256
B_ONEHOT = 384
B_DL = 1408
B_DS = 3456
NCONST_BF = 4480
A_HEADS = 8


def build_consts():
    c = np.zeros((128, NCONST), np.float32)
    c[:, C_IDENT:C_IDENT + 128] = np.eye(128, dtype=np.float32)
    bo = np.zeros((128, 128), np.float32)
    bo[:64, :64] = 1.0
    bo[64:, 64:] = 1.0
    c[:, C_BONES:C_BONES + 128] = bo
    slopes = np.exp2(-8.0 * np.arange(1, A_HEADS + 1) / A_HEADS).astype(np.float32)
    pidx = np.arange(128, dtype=np.float32)
    for h in range(A_HEADS):
        c[:, C_BIASCOL + h] = slopes[h] * (pidx - 127.0)
    for i in range(8):
        qb = 4 + i // 2
        for n in range(8):
            c[:, C_GMASK + i * 8 + n] = 0.0 if n < qb else -1e30
    s = np.arange(64)[:, None]
    t = np.arange(64)[None, :]
    am = np.concatenate([(s < t), (s <= t)], axis=1).astype(np.float32)
    c[:64, C_AMASK:C_AMASK + 512] = np.tile(am, (1, 4))
    nt = (t < s).astype(np.float32)
    c[:64, C_NTMASK:C_NTMASK + 512] = np.tile(nt, (1, 8))
    c[:, C_ONE64:C_ONE64 + 64] = 1.0
    c[:64, C_ID8:C_ID8 + 512] = np.tile(np.eye(64, dtype=np.float32), (1, 8))
    b = np.zeros((128, NCONST_BF), np.float32)
    b[:, B_IDENT:B_IDENT + 128] = np.eye(128, dtype=np.float32)
    b[:, B_ONES:B_ONES + 128] = 1.0
    kk = np.arange(128)[:, None]
    qq = np.arange(128)[None, :]
    b[:, B_CAUSAL:B_CAUSAL + 128] = np.where(kk > qq, -30000.0, 0.0)
    for n in range(8):
        b[n, B_ONEHOT + n * 128:B_ONEHOT + (n + 1) * 128] = 1.0
    for dl in range(16):
        b[0, B_DL + dl * 128:B_DL + (dl + 1) * 128] = float(dl)
    for h in range(A_HEADS):
        b[0, B_DS + h * 128:B_DS + (h + 1) * 128] = -128.0 * slopes[h]
    return c, b


class Builder:
    def __init__(self):
        self.nc = nc = bass.Bass("TRN2", target_bir_lowering=False)
        self.es = ExitStack()
        self.P = Prog(nc, self.es)
        self.A = Arena(nc)
        self.w = {}
        self.x_in = nc.dram_tensor("x", [T, D], F32, kind="ExternalInput").ap()
        self.out = nc.dram_tensor("out", [T, D], F32, kind="ExternalOutput").ap()
        self.consts_d = nc.dram_tensor("consts", [128, NCONST], F32, kind="ExternalInput").ap()
        self.constsb_d = nc.dram_tensor("constsb", [128, NCONST_BF], F32, kind="ExternalInput").ap()
        self.dbg = None
        self.dbgb = self.P.buf("dbg", True)
        self.xres = nc.dram_tensor("xres", [NCH, 128, T], F32, kind="Internal").ap()
        self.vfirst = nc.dram_tensor("vfirst", [8, 128, T], F32, kind="Internal").ap()
        self.vfirstb = self.P.buf("vfirst", True)
        self.rkv = nc.dram_tensor("rkv", [24, 128, T], F32, kind="Internal").ap()
        self.rkvb = self.P.buf("rkv", True)
        self.xb = [self.P.buf("xres0", True), self.P.buf("xres1", True)]
        self.outb = self.P.buf("outb", True)
        self.ps = nc.alloc_psum_tensor("ps", [128, 4096], F32)
        self.pb = self.P.bufs("psb", 8, True)
        self.vecs = {}
        self.vlay = {}

    def bank(self, b, n=512):
        return self.ps[:, b * 512:b * 512 + n]

    def next_bank(self, lo=0, hi=8):
        k = (lo, hi)
        if not hasattr(self, "_bks"):
            self._bks = {}
        v = self._bks.get(k, lo - 1) + 1
        if v >= hi:
            v = lo
        self._bks[k] = v
        return v

    def dump(self, ap, col0, ncols, reads):
        if self.dbg is None:
            self.dbg = self.nc.dram_tensor("dbg", [128, DBG_N], F32, kind="ExternalOutput").ap()
        self.dma("pool", self.dbg[:, col0:col0 + ncols], ap, reads, partial=[self.dbgb],
                 owner=self.dbgb)

    def dram_in(self, name, shape):
        self.w[name] = self.nc.dram_tensor(name, list(shape), F32, kind="ExternalInput").ap()
        return self.w[name]

    def dma(self, q, out, in_, reads, writes=(), partial=(), owner=None):
        self.P.op(q, lambda e: e.dma_start(out=out, in_=in_), reads=reads, writes=writes,
                  partial=partial, dma_owner=owner)

    def mm(self, out, lhsT, rhs, start, stop, reads, wbuf):
        self.P.op("pe", lambda e: e.matmul(out, lhsT, rhs, start=start, stop=stop),
                  reads=reads, writes=[wbuf])

    def tr(self, out, in_, ident, reads, wbuf):
        self.P.op("pe", lambda e: e.transpose(out, in_, ident), reads=reads, writes=[wbuf])

    def act(self, out, in_, func, reads, writes=(), partial=(), scale=1.0, bias=None):
        if bias is None:
            self.P.op("act", lambda e: e.activation(out=out, in_=in_, func=func, scale=scale),
                      reads=reads, writes=writes, partial=partial)
        else:
            self.P.op("act", lambda e: e.activation(out=out, in_=in_, func=func, scale=scale,
                                                    bias=bias),
                      reads=reads, writes=writes, partial=partial)

    def ts(self, eng, out, in0, s1, s2, op0, op1, reads, writes=(), partial=()):
        if s2 is None:
            self.P.op(eng, lambda e: e.tensor_scalar(out=out, in0=in0, scalar1=s1, scalar2=None,
                                                     op0=op0),
                      reads=reads, writes=writes, partial=partial)
        else:
            self.P.op(eng, lambda e: e.tensor_scalar(out=out, in0=in0, scalar1=s1, scalar2=s2,
                                                     op0=op0, op1=op1),
                      reads=reads, writes=writes, partial=partial)

    def tt(self, eng, out, in0, in1, op, reads, writes=(), partial=()):
        self.P.op(eng, lambda e: e.tensor_tensor(out=out, in0=in0, in1=in1, op=op),
                  reads=reads, writes=writes, partial=partial)

    def stt(self, out, in0, scalar, in1, op0, op1, reads, writes=(), partial=()):
        self.P.op("dve", lambda e: e.scalar_tensor_tensor(out=out, in0=in0, scalar=scalar, in1=in1,
                                                          op0=op0, op1=op1),
                  reads=reads, writes=writes, partial=partial)

    def copy(self, eng, out, in_, reads, writes=(), partial=()):
        if eng == "act":
            self.P.op("act", lambda e: e.copy(out=out, in_=in_), reads=reads, writes=writes,
                      partial=partial)
        else:
            self.P.op(eng, lambda e: e.tensor_copy(out=out, in_=in_), reads=reads, writes=writes,
                      partial=partial)

    def load_consts(self):
        A, P = self.A, self.P
        self.cf = A.alloc([128, NCONST], F32)
        self.cfb = P.buf("cf", True)
        self.dma("sp", self.cf[:, :], self.consts_d, [], [self.cfb], owner=self.cfb)
        self.cb = A.alloc([128, 384], BF16)
        self.cbb = P.buf("cb", True)
        self.dma("pool", self.cb[:, :], self.constsb_d[:, 0:384], [], [self.cbb], owner=self.cbb)
        self.ident_f = self.cf[:, C_IDENT:C_IDENT + 128]
        self.ident_b = self.cb[:, B_IDENT:B_IDENT + 128]
        self.ones_b = self.cb[:, B_ONES:B_ONES + 128]

    def load_vecs(self, li):
        lay, n = layer_vec_layout(li)
        d = self.nc.dram_tensor("vecs%d" % li, [128, n], F32, kind="ExternalInput").ap()
        t = self.A.alloc([128, n], F32)
        b = self.P.buf("vecs%d" % li, True)
        self.dma("sp", t[:, :], d, [], [b], owner=b)
        self.vecs[li] = (t, b)
        self.vlay[li] = lay

    def vcol(self, li, name, c=0, n=1):
        t, b = self.vecs[li]
        c0, w = self.vlay[li][name]
        return t[:, c0 + c:c0 + c + n]

    def prologue(self):
        A, P = self.A, self.P
        m = A.mark()
        xin = [A.alloc([128, D], F32) for _ in range(2)]
        xinb = P.bufs("xin", 2)
        xo = [A.alloc([128, NCH, 128], F32) for _ in range(2)]
        xob = P.bufs("xo", 2)
        xresT = self.xres.rearrange("c p t -> p c t")
        for tt in range(NTT):
            s = tt % 2
            self.dma("sp", xin[s][:, :], self.x_in[tt * 128:(tt + 1) * 128, :], [], [xinb[s]],
                     owner=xinb[s])
            for g in range(4):
                bk = self.next_bank()
                for q in range(4):
                    dc = g * 4 + q
                    self.tr(self.bank(bk)[:, q * 128:(q + 1) * 128],
                            xin[s][:, dc * 128:(dc + 1) * 128], self.ident_f,
                            [xinb[s], self.cfb], self.pb[bk])
                eng = "act" if g % 2 == 0 else "dve"
                self.copy(eng, xo[s][:, g * 4:(g + 1) * 4, :],
                          self.bank(bk).rearrange("p (q t) -> p q t", q=4),
                          [self.pb[bk]], partial=[xob[s]])
            self.dma("sp", xresT[:, :, tt * 128:(tt + 1) * 128], xo[s][:, :, :], [xob[s]],
                     partial=[self.xb[tt // 8]], owner=self.xb[tt // 8])
        A.reset(m)

    def epilogue(self):
        A, P = self.A, self.P
        m = A.mark()
        xi = [A.alloc([128, NCH, 128], F32) for _ in range(2)]
        xib = P.bufs("exi", 2)
        xo = [A.alloc([128, D], F32) for _ in range(2)]
        xob = P.bufs("exo", 2)
        xresT = self.xres.rearrange("c p t -> p c t")
        for tt in range(NTT):
            s = tt % 2
            self.dma("sp", xi[s][:, :, :], xresT[:, :, tt * 128:(tt + 1) * 128], [self.xb[tt // 8]],
                     [xib[s]], owner=xib[s])
            for g in range(4):
                bk = self.next_bank()
                for q in range(4):
                    dc = g * 4 + q
                    self.tr(self.bank(bk)[:, q * 128:(q + 1) * 128], xi[s][:, dc, :], self.ident_f,
                            [xib[s], self.cfb], self.pb[bk])
                eng = "act" if g % 2 == 0 else "dve"
                self.copy(eng, xo[s][:, g * 512:(g + 1) * 512], self.bank(bk), [self.pb[bk]],
                          partial=[xob[s]])
            self.dma("sp", self.out[tt * 128:(tt + 1) * 128, :], xo[s][:, :], [xob[s]],
                     partial=[self.outb], owner=self.outb)
        A.reset(m)

    def rmsnorm(self, li, gname, xn, xnb, t0, n, tag):
        A, P = self.A, self.P
        m = A.mark()
        halves = sorted(set([t0 // 1024, (t0 + n - 1) // 1024]))
        rb = [self.xb[h] for h in halves]
        W = 512
        xc = [A.alloc([128, W], F32) for _ in range(3)]
        xcb = P.bufs("xc" + tag, 3)
        sq = [A.alloc([128, W], BF16) for _ in range(2)]
        sqb = P.bufs("sq" + tag, 2)
        rstd = A.alloc([128, n], F32)
        rsb = P.buf("rstd" + tag)
        nb = n // W
        k = 0
        q = 0
        for b in range(nb):
            bk = self.next_bank()
            for c in range(NCH):
                s = k % 3
                k += 1
                self.dma("sp", xc[s][:, :], self.xres[c, :, t0 + b * W:t0 + (b + 1) * W], rb,
                         [xcb[s]], owner=xcb[s])
                q = (q + 1) % 2
                self.act(sq[q][:, :], xc[s][:, :], AF.Square, [xcb[s]], [sqb[q]])
                self.mm(self.bank(bk), self.ones_b, sq[q][:, :], c == 0, c == NCH - 1,
                        [sqb[q], self.cbb], self.pb[bk])
            self.ts("dve", rstd[:, b * W:(b + 1) * W], self.bank(bk), 1.0 / D, RMS_EPS,
                    ALU.mult, ALU.add, [self.pb[bk]], partial=[rsb])
        self.act(rstd[:, :], rstd[:, :], AF.Sqrt, [rsb], [rsb])
        self.P.op("dve", lambda e: e.reciprocal(out=rstd[:, :], in_=rstd[:, :]), reads=[rsb],
                  writes=[rsb])
        vb = self.vecs[li][1]
        for b in range(nb):
            for c in range(NCH):
                s = k % 3
                k += 1
                self.dma("sp", xc[s][:, :], self.xres[c, :, t0 + b * W:t0 + (b + 1) * W], rb,
                         [xcb[s]], owner=xcb[s])
                self.stt(xn[:, c, b * W:(b + 1) * W], xc[s][:, :], self.vcol(li, gname, c),
                         rstd[:, b * W:(b + 1) * W], ALU.mult, ALU.mult, [xcb[s], rsb, vb],
                         partial=[xnb])
        A.reset(m)

    def ffn(self, li):
        A, P = self.A, self.P
        m = A.mark()
        p = "l%d_" % li
        w_up = self.w[p + "ffn_up"]
        w_dn = self.w[p + "ffn_down"].rearrange("(j q) n -> q j n", q=128)
        n = 1024
        W = 512
        xn = A.alloc([128, NCH, n], BF16)
        xnb = P.buf("fxn")
        aT = A.alloc([128, NJ, n], BF16)
        aTb = P.buf("faT")
        wup = [A.alloc([128, 2, NCH, 128], BF16) for _ in range(2)]
        wupb = P.bufs("fwup", 2)
        hh = [[A.alloc([128, W + 2], F32) for _ in range(2)] for _ in range(2)]
        hhb = [P.bufs("fh%d_" % gv, 2) for gv in range(2)]
        cg = A.alloc([128, W], F32)
        cv = A.alloc([128, W], F32)
        cgb, cvb = P.buf("fcg"), P.buf("fcv")
        halo = A.alloc([128, 2 * NJ, 2], F32)
        halob = P.buf("fhalo")
        wdn = [A.alloc([128, NJ, 128], BF16) for _ in range(2)]
        wdnb = P.bufs("fwdn", 2)
        xr = [A.alloc([128, W], F32) for _ in range(2)]
        xrb = P.bufs("fxr", 2)
        vb = self.vecs[li][1]
        self.P.op("dve", lambda e: e.memset(halo[:, :, :], 0.0), reads=[], writes=[halob])
        wk = 0
        hk = 0
        xk = 0
        for th in range(2):
            t0 = th * n
            self.rmsnorm(li, "norm_ffn", xn, xnb, t0, n, "f")
            for j in range(NJ):
                s = wk % 2
                wk += 1
                for gv in range(2):
                    c0 = gv * DFF + j * 128
                    src = w_up[:, c0:c0 + 128].rearrange("(c q) n -> q c n", q=128)
                    if gv == 0:
                        self.dma("pool", wup[s][:, gv, :, :], src, [], writes=[wupb[s]], owner=wupb[s])
                    else:
                        self.dma("pool", wup[s][:, gv, :, :], src, [], partial=[wupb[s]], owner=wupb[s])
                for tb in range(n // W):
                    hs = hk % 2
                    hk += 1
                    for gv in range(2):
                        bk = self.next_bank()
                        for c in range(NCH):
                            self.mm(self.bank(bk), wup[s][:, gv, c, :], xn[:, c, tb * W:(tb + 1) * W],
                                    c == 0, c == NCH - 1, [wupb[s], xnb], self.pb[bk])
                        h = hh[gv][hs]
                        hB = hhb[gv][hs]
                        col = gv * NJ + j
                        self.copy("act", h[:, 0:2], halo[:, col, :], [halob], writes=[hB])
                        self.copy("act", h[:, 2:W + 2], self.bank(bk), [self.pb[bk]], partial=[hB])
                        self.copy("act", halo[:, col, :], h[:, W:W + 2], [hB], partial=[halob])
                        cdst, cB = (cg, cgb) if gv == 0 else (cv, cvb)
                        self.ts("dve", cdst[:, :], h[:, 0:W], self.vcol(li, "ffn_conv", 0 * 88 + col),
                                None, ALU.mult, None, [hB, vb], writes=[cB])
                        self.stt(cdst[:, :], h[:, 1:W + 1], self.vcol(li, "ffn_conv", 1 * 88 + col),
                                 cdst[:, :], ALU.mult, ALU.add, [hB, vb, cB], writes=[cB])
                        self.stt(cdst[:, :], h[:, 2:W + 2], self.vcol(li, "ffn_conv", 2 * 88 + col),
                                 cdst[:, :], ALU.mult, ALU.add, [hB, vb, cB], writes=[cB])
                    self.act(cg[:, :], cg[:, :], AF.Silu, [cgb], writes=[cgb])
                    self.tt("dve", aT[:, j, tb * W:(tb + 1) * W], cg[:, :], cv[:, :], ALU.mult,
                            [cgb, cvb], partial=[aTb])
            for mch in range(NCH):
                s = mch % 2
                self.dma("pool", wdn[s][:, :, :], w_dn[:, :, mch * 128:(mch + 1) * 128], [],
                         writes=[wdnb[s]], owner=wdnb[s])
                for tb in range(n // W):
                    xs = xk % 2
                    xk += 1
                    tsl = slice(t0 + tb * W, t0 + (tb + 1) * W)
                    self.dma("sp", xr[xs][:, :], self.xres[mch, :, tsl], [self.xb[th]],
                             writes=[xrb[xs]], owner=xrb[xs])
                    bk = self.next_bank()
                    for j in range(NJ):
                        self.mm(self.bank(bk), wdn[s][:, j, :], aT[:, j, tb * W:(tb + 1) * W],
                                j == 0, j == NJ - 1, [wdnb[s], aTb], self.pb[bk])
                    self.tt("dve", xr[xs][:, :], self.bank(bk), xr[xs][:, :], ALU.add,
                            [self.pb[bk], xrb[xs]], writes=[xrb[xs]])
                    self.dma("sp", self.xres[mch, :, tsl], xr[xs][:, :], [xrb[xs]],
                             partial=[self.xb[th]], owner=self.xb[th])
        A.reset(m)


    def out_proj(self, w_ap, yT, yTb, nk, tag):
        A, P = self.A, self.P
        W = 512
        w_r = w_ap.rearrange("(j q) n -> q j n", q=128)
        wo = [A.alloc([128, nk, 128], BF16) for _ in range(2)]
        wob = P.bufs("wo" + tag, 2)
        xr = [A.alloc([128, W], F32) for _ in range(3)]
        xrb = P.bufs("xr" + tag, 3)
        xk = 0
        for mch in range(NCH):
            s = mch % 2
            self.dma("pool", wo[s][:, :, :], w_r[:, :, mch * 128:(mch + 1) * 128], [],
                     writes=[wob[s]], owner=wob[s])
            for tb in range(T // W):
                xs = xk % 3
                xk += 1
                th = (tb * W) // 1024
                tsl = slice(tb * W, (tb + 1) * W)
                self.dma("sp", xr[xs][:, :], self.xres[mch, :, tsl], [self.xb[th]],
                         writes=[xrb[xs]], owner=xrb[xs])
                bk = self.next_bank()
                for j in range(nk):
                    self.mm(self.bank(bk), wo[s][:, j, :], yT[:, j, tsl], j == 0, j == nk - 1,
                            [wob[s], yTb], self.pb[bk])
                self.tt("dve", xr[xs][:, :], self.bank(bk), xr[xs][:, :], ALU.add,
                        [self.pb[bk], xrb[xs]], writes=[xrb[xs]])
                self.dma("sp", self.xres[mch, :, tsl], xr[xs][:, :], [xrb[xs]],
                         partial=[self.xb[th]], owner=self.xb[th])

    def sconv(self, li):
        A, P = self.A, self.P
        m = A.mark()
        p = "l%d_" % li
        w_in = self.w[p + "conv_in"]
        W = 512
        xn = A.alloc([128, NCH, T], BF16)
        xnb = P.buf("sxn")
        zT = A.alloc([128, NCH, T], BF16)
        zTb = P.buf("szT")
        m2 = A.mark()
        wci = [A.alloc([128, 3, NCH, 128], BF16) for _ in range(3)]
        wcib = P.bufs("swci", 3)
        usb = [A.alloc([128, W], F32) for _ in range(2)]
        usbb = P.bufs("susb", 2)
        cu = [A.alloc([128, W + 2], F32) for _ in range(2)]
        cub = P.bufs("scu", 2)
        cc = [A.alloc([128, W], F32) for _ in range(2)]
        ccb = P.bufs("scc", 2)
        self.rmsnorm(li, "norm_mix", xn, xnb, 0, T, "s")
        vb = self.vecs[li][1]
        k = 0
        for j in range(NCH):
            s = j % 3
            for g in range(3):
                c0 = g * D + j * 128
                src = w_in[:, c0:c0 + 128].rearrange("(c q) n -> q c n", q=128)
                if g == 0:
                    self.dma("pool", wci[s][:, g, :, :], src, [], writes=[wcib[s]], owner=wcib[s])
                else:
                    self.dma("pool", wci[s][:, g, :, :], src, [], partial=[wcib[s]], owner=wcib[s])
            for tb in range(T // W):
                q = k % 2
                k += 1
                tsl = slice(tb * W, (tb + 1) * W)
                bks = []
                for g in range(3):
                    bk = self.next_bank()
                    bks.append(bk)
                    for c in range(NCH):
                        self.mm(self.bank(bk), wci[s][:, g, c, :], xn[:, c, tsl], c == 0, c == NCH - 1,
                                [wcib[s], xnb], self.pb[bk])
                self.copy("act", usb[q][:, :], self.bank(bks[2]), [self.pb[bks[2]]], writes=[usbb[q]])
                if tb == 0:
                    self.P.op("dve", (lambda t: (lambda e: e.memset(t, 0.0)))(cu[q][:, 0:2]), reads=[],
                              writes=[cub[q]])
                else:
                    self.copy("act", cu[q][:, 0:2], cu[1 - q][:, W:W + 2], [cub[1 - q]], writes=[cub[q]])
                self.tt("dve", cu[q][:, 2:W + 2], self.bank(bks[1]), usb[q][:, :], ALU.mult,
                        [self.pb[bks[1]], usbb[q]], partial=[cub[q]])
                for tap in range(3):
                    wcol = self.vcol(li, "conv_w", tap * 16 + j)
                    if tap == 0:
                        self.ts("dve", cc[q][:, :], cu[q][:, 0:W], wcol, None, ALU.mult, None,
                                [cub[q], vb], writes=[ccb[q]])
                    else:
                        self.stt(cc[q][:, :], cu[q][:, tap:W + tap], wcol, cc[q][:, :], ALU.mult, ALU.add,
                                 [cub[q], vb, ccb[q]], writes=[ccb[q]])
                self.tt("dve", zT[:, j, tsl], self.bank(bks[0]), cc[q][:, :], ALU.mult,
                        [self.pb[bks[0]], ccb[q]], partial=[zTb])
        A.reset(m2)
        P.fence()
        self.out_proj(self.w[p + "conv_out"], zT, zTb, NCH, "s")
        A.reset(m)

    def attention(self, li, xn, xnb, yT, yTb):
        A, P = self.A, self.P
        p = "l%d_" % li
        w_in = self.w[p + "w_in"]
        W = 512
        vb = self.vecs[li][1]
        w3 = [A.alloc([128, 3, NCH, 128], BF16) for _ in range(2)]
        w3b = P.bufs("aw3", 2)
        qT = A.alloc([128, T], BF16)
        kT = A.alloc([128, T], BF16)
        V = A.alloc([128, T // 128, 128], BF16)
        qTb, kTb, Vb = P.buf("aqT"), P.buf("akT"), P.buf("aV")
        sqt = [A.alloc([128, W], BF16) for _ in range(2)]
        sqtb = P.bufs("asq", 2)
        rqt = [A.alloc([128, W], F32) for _ in range(2)]
        rqtb = P.bufs("arq", 2)
        gqs = A.alloc([128, 1], F32)
        gqsb = P.buf("agqs")
        km = A.alloc([128, 8], F32)
        kmb16 = A.alloc([128, 8], BF16)
        kmb = P.buf("akm")
        gm = A.alloc([128, 64], F32)
        gmb = P.buf("agm")
        top8 = A.alloc([128, 8], F32)
        top8b = P.buf("atop8")
        selb = A.alloc([128, 64], F32)
        selbb = P.buf("aselb")
        selT = A.alloc([8, 1024], BF16)
        selTb = P.buf("aselT")
        pT = [A.alloc([128, W], BF16) for _ in range(3)]
        pTb = P.bufs("apT", 3)
        rl = [A.alloc([128, 128], F32) for _ in range(2)]
        rlb = P.bufs("arl", 2)
        cba = A.alloc([8, NCONST_BF - 384], BF16)
        cbab = P.buf("acba")
        self.dma("pool", cba[:, :], self.constsb_d[0:8, 384:NCONST_BF], [], writes=[cbab], owner=cbab)
        self.ts("dve", gqs[:, :], self.vcol(li, "q_gain"), float(128 ** -0.5), None, ALU.mult, None,
                [vb], writes=[gqsb])
        cbb, cfb = self.cbb, self.cfb
        pk = 0
        for h in range(int(os.environ.get('NAH', A_HEADS))):
            s = h % 2
            for g, blk in enumerate((h, 8 + h, 16 + h)):
                srcw = w_in[:, blk * 128:(blk + 1) * 128].rearrange("(c q) n -> q c n", q=128)
                if g == 0:
                    self.dma("pool", w3[s][:, g, :, :], srcw, [], writes=[w3b[s]], owner=w3b[s])
                else:
                    self.dma("pool", w3[s][:, g, :, :], srcw, [], partial=[w3b[s]], owner=w3b[s])
            for tb in range(T // W):
                tsl = slice(tb * W, (tb + 1) * W)
                for g, (dst, dstb, gain) in enumerate(((qT, qTb, gqs[:, 0:1]),
                                                       (kT, kTb, self.vcol(li, "k_gain")))):
                    bk = self.next_bank()
                    for c in range(NCH):
                        self.mm(self.bank(bk), w3[s][:, g, c, :], xn[:, c, tsl], c == 0, c == NCH - 1,
                                [w3b[s], xnb], self.pb[bk])
                    z = pk % 2
                    pk += 1
                    self.act(sqt[z][:, :], self.bank(bk), AF.Square, [self.pb[bk]], writes=[sqtb[z]])
                    b2 = self.next_bank()
                    self.mm(self.bank(b2), self.ones_b, sqt[z][:, :], True, True, [sqtb[z], cbb],
                            self.pb[b2])
                    self.ts("dve", rqt[z][:, :], self.bank(b2), 1.0 / 128, RMS_EPS, ALU.mult, ALU.add,
                            [self.pb[b2]], writes=[rqtb[z]])
                    self.act(rqt[z][:, :], rqt[z][:, :], AF.Sqrt, [rqtb[z]], writes=[rqtb[z]])
                    self.P.op("dve", (lambda t: (lambda e: e.reciprocal(out=t, in_=t)))(rqt[z][:, :]),
                              reads=[rqtb[z]], writes=[rqtb[z]])
                    self.stt(dst[:, tsl], self.bank(bk), gain, rqt[z][:, :], ALU.mult, ALU.mult,
                             [self.pb[bk], rqtb[z], vb, gqsb], partial=[dstb])
            for g4 in range(4):
                bv = self.next_bank()
                for i in range(4):
                    tt = g4 * 4 + i
                    for c in range(NCH):
                        self.mm(self.bank(bv)[:, i * 128:(i + 1) * 128], xn[:, c, tt * 128:(tt + 1) * 128],
                                w3[s][:, 2, c, :], c == 0, c == NCH - 1, [w3b[s], xnb], self.pb[bv])
                self.copy("act", V[:, g4 * 4:(g4 + 1) * 4, :],
                          self.bank(bv).rearrange("p (i d) -> p i d", i=4), [self.pb[bv]], partial=[Vb])
            self.P.op("dve", lambda e: e.tensor_reduce(out=km[:, :],
                                                       in_=kT[:, :].rearrange("p (n s) -> p n s", n=8),
                                                       axis=AX.X, op=ALU.add),
                      reads=[kTb], writes=[kmb])
            self.copy("dve", kmb16[:, :], km[:, :], [kmb], writes=[kmb])
            bg = self.next_bank()
            for i in range(8):
                qt = 8 + i
                self.mm(self.bank(bg)[:, i * 8:(i + 1) * 8], qT[:, qt * 128:(qt + 1) * 128], kmb16[:, :],
                        True, True, [qTb, kmb], self.pb[bg])
            self.tt("dve", gm[:, :], self.bank(bg)[:, 0:64], self.cf[:, C_GMASK:C_GMASK + 64], ALU.add,
                    [self.pb[bg], cfb], writes=[gmb])
            for i in range(8):
                self.P.op("dve", (lambda o, a: (lambda e: e.max(out=o, in_=a)))(top8[:, :],
                                                                               gm[:, i * 8:(i + 1) * 8]),
                          reads=[gmb], writes=[top8b])
                self.ts("dve", selb[:, i * 8:(i + 1) * 8], gm[:, i * 8:(i + 1) * 8], top8[:, 2:3], -30000.0,
                        ALU.is_lt, ALU.mult, [gmb, top8b], partial=[selbb])
            for g2 in range(2):
                bt = self.next_bank()
                for i in range(4):
                    ii = g2 * 4 + i
                    self.tr(self.bank(bt)[0:8, i * 128:(i + 1) * 128], selb[:, ii * 8:(ii + 1) * 8],
                            self.ident_f, [selbb, cfb], self.pb[bt])
                self.copy("act", selT[0:8, g2 * 512:(g2 + 1) * 512], self.bank(bt)[0:8, :], [self.pb[bt]],
                          partial=[selTb])
            for qt in range(T // 128):
                qb = qt // 2
                bo = 4 + qt % 2
                bl = 6 + qt % 2
                qsl = slice(qt * 128, (qt + 1) * 128)
                for g0 in range(0, qt + 1, 4):
                    grp = list(range(g0, min(g0 + 4, qt + 1)))
                    bs = self.next_bank(0, 4)
                    for i, kt in enumerate(grp):
                        o = self.bank(bs)[:, i * 128:(i + 1) * 128]
                        extras = [(cba[0:1, B_DS - 384 + h * 128:B_DS - 384 + (h + 1) * 128],
                                   cba[0:1, B_DL - 384 + (qt - kt) * 128:B_DL - 384 + (qt - kt + 1) * 128], [cbab])]
                        if qb >= 4 and kt // 2 < qb:
                            kb = kt // 2
                            extras.append((cba[0:8, B_ONEHOT - 384 + kb * 128:B_ONEHOT - 384 + (kb + 1) * 128],
                                           selT[0:8, (qt - 8) * 128:(qt - 7) * 128], [cbab, selTb]))
                        if kt == qt:
                            extras.append((self.ident_b, self.cb[:, B_CAUSAL:B_CAUSAL + 128], [cbb]))
                        self.mm(o, kT[:, kt * 128:(kt + 1) * 128], qT[:, qsl], True, False, [kTb, qTb],
                                self.pb[bs])
                        for ei, (l_, r_, rd) in enumerate(extras):
                            self.mm(o, l_, r_, False, ei == len(extras) - 1, rd, self.pb[bs])
                    z = pk % 3
                    pk += 1
                    n_ = len(grp) * 128
                    self.act(pT[z][:, 0:n_], self.bank(bs)[:, 0:n_], AF.Exp, [self.pb[bs], cfb],
                             writes=[pTb[z]], bias=self.cf[:, C_BIASCOL + h:C_BIASCOL + h + 1])
                    for i, kt in enumerate(grp):
                        self.mm(self.bank(bo)[:, 0:128], V[:, kt, :], pT[z][:, i * 128:(i + 1) * 128],
                                kt == 0, kt == qt, [Vb, pTb[z]], self.pb[bo])
                        self.mm(self.bank(bl)[:, 0:128], self.ones_b, pT[z][:, i * 128:(i + 1) * 128],
                                kt == 0, kt == qt, [cbb, pTb[z]], self.pb[bl])
                z2 = qt % 2
                self.P.op("dve", (lambda o, a: (lambda e: e.reciprocal(out=o, in_=a)))(
                    rl[z2][:, :], self.bank(bl)[:, 0:128]), reads=[self.pb[bl]], writes=[rlb[z2]])
                self.tt("dve", yT[:, h, qsl], self.bank(bo)[:, 0:128], rl[z2][:, :], ALU.mult,
                        [self.pb[bo], rlb[z2]], partial=[yTb])

    def rwkv(self, li, xn, xnb, yT, yTb, m_xn, xn_bytes):
        A, P = self.A, self.P
        p = "l%d_" % li
        first = li == 0
        w_in = self.w[p + "w_in"]
        RW0 = 3072
        n = 256
        NC = 4
        vb = self.vecs[li][1]
        cfb, cbb = self.cfb, self.cbb
        bones = self.cf[:, C_BONES:C_BONES + 128]
        c0 = float(np.exp(-0.5))

        lw = A.alloc([128, 1024], BF16)
        lg0 = A.alloc([128, 1024], BF16)
        lg1 = A.alloc([64, 1024], BF16)
        lwb = P.buf("rlw")
        self.dma("pool", lw[0:64, :], self.w[p + "w_lora"], [], writes=[lwb], owner=lwb)
        self.dma("pool", lw[64:128, :], self.w[p + "a_lora"], [], partial=[lwb], owner=lwb)
        self.dma("pool", lg0[:, :], self.w[p + "g_lora"][0:128, :], [], partial=[lwb], owner=lwb)
        self.dma("pool", lg1[0:32, :], self.w[p + "g_lora"][128:160, :], [], partial=[lwb], owner=lwb)
        if not first:
            self.dma("pool", lg1[32:64, :], self.w[p + "v_lora"], [], partial=[lwb], owner=lwb)
        zwa = A.alloc([128, T], BF16)
        sg0 = A.alloc([128, T], BF16)
        sgv = A.alloc([64, T], BF16)
        zb = P.buf("rz")
        mz = A.mark()
        wz = [A.alloc([128, NCH, 128], BF16) for _ in range(2)]
        wzb = P.bufs("rwz", 2)
        zraw = [A.alloc([128, 513], F32) for _ in range(2)]
        zrawb = P.bufs("rzraw", 2)
        zd = [A.alloc([128, 512], F32) for _ in range(2)]
        zdb = P.bufs("rzd", 2)
        zk = 0
        nz2 = 32 if first else 64
        items = [("z", zi, 3072 + zi * 128, (128, 128, nz2)[zi], 24 + zi) for zi in range(3)]
        items += [("p", b, b * 128, 128, b) for b in range(24)]
        for ii, (kind, zi, coff, ncol, mui) in enumerate(items):
            col0 = RW0 + coff
            ws = ii % 2
            self.dma("pool", wz[ws][:, :, 0:ncol],
                     w_in[:, col0:col0 + ncol].rearrange("(c q) n -> q c n", q=128),
                     [], writes=[wzb[ws]], owner=wzb[ws])
            mu = self.vcol(li, "shift_mu", mui)
            for tb in range(4):
                q = zk % 2
                zk += 1
                tsl = slice(tb * 512, (tb + 1) * 512)
                bk = self.next_bank()
                for c in range(NCH):
                    self.mm(self.bank(bk)[0:ncol, :], wz[ws][:, c, 0:ncol], xn[:, c, tsl], c == 0, c == NCH - 1,
                            [wzb[ws], xnb], self.pb[bk])
                if tb == 0:
                    self.P.op("dve", (lambda t: (lambda e: e.memset(t, 0.0)))(zraw[q][0:ncol, 0:1]), reads=[],
                              partial=[zrawb[q]])
                else:
                    self.copy("act", zraw[q][0:ncol, 0:1], zraw[1 - q][0:ncol, 512:513], [zrawb[1 - q]],
                              partial=[zrawb[q]])
                self.copy("act", zraw[q][0:ncol, 1:513], self.bank(bk)[0:ncol, :], [self.pb[bk]],
                          writes=[zrawb[q]])
                self.tt("dve", zd[q][0:ncol, :], zraw[q][0:ncol, 0:512], zraw[q][0:ncol, 1:513], ALU.subtract,
                        [zrawb[q]], writes=[zdb[q]])
                self.stt(zd[q][0:ncol, :], zd[q][0:ncol, :], mu[0:ncol, :], zraw[q][0:ncol, 1:513], ALU.mult,
                         ALU.add, [zdb[q], zrawb[q], vb], writes=[zdb[q]])
                if kind == "p":
                    self.dma("sp", self.rkv[zi, :, tsl], zd[q][:, :], [zdb[q]], partial=[self.rkvb],
                             owner=self.rkvb)
                    if first and zi >= 16:
                        self.dma("sp", self.vfirst[zi - 16, :, tsl], zd[q][:, :], [zdb[q]],
                                 partial=[self.vfirstb], owner=self.vfirstb)
                elif zi == 0:
                    self.act(zwa[0:64, tsl], zd[q][0:64, :], AF.Tanh, [zdb[q]], partial=[zb])
                    self.copy("act", zwa[64:128, tsl], zd[q][64:128, :], [zdb[q]], partial=[zb])
                elif zi == 1:
                    self.act(sg0[:, tsl], zd[q][:, :], AF.Sigmoid, [zdb[q]], partial=[zb])
                else:
                    self.act(sgv[0:32, tsl], zd[q][0:32, :], AF.Sigmoid, [zdb[q]], partial=[zb])
                    if not first:
                        self.copy("act", sgv[32:64, tsl], zd[q][32:64, :], [zdb[q]], partial=[zb])
        def stream(Ax, hps):
            def f32(shape=(128, n)):
                return Ax.alloc(list(shape), F32)
            rr, kx, vv = f32(), f32(), f32()
            rrb, kxb, vvb = P.buf("rr"), P.buf("rk"), P.buf("rv")
            dtmp = f32()
            dtmpb = P.buf("rdt")
            sw, asig, gg, vs = f32(), f32(), f32(), f32()
            swb, asigb, ggb, vsb = P.buf("rsw"), P.buf("rasig"), P.buf("rgg"), P.buf("rvs")
            vf = f32()
            vfb_ = P.buf("rvf")
            kk, sq, rn = f32(), f32(), f32()
            kkb, sqb, rnb = P.buf("rkk"), P.buf("rsq"), P.buf("rrn")
            k2, bvec = f32(), f32()
            k2b, bvecb = P.buf("rk2"), P.buf("rbvec")
            Lp, Lx = f32(), f32()
            Lpb, Lxb = P.buf("rLp"), P.buf("rLx")
            Win, Wout, Wex, Wend = f32(), f32(), f32(), f32()
            Winb, Woutb, Wexb, Wendb = P.buf("rWin"), P.buf("rWout"), P.buf("rWex"), P.buf("rWend")
            AR = Ax.alloc([128, NC, 2, 64], F32)
            BK = Ax.alloc([128, NC, 2, 64], F32)
            ARb, BKb = P.buf("rAR"), P.buf("rBK")
            khat, bhat = Wex, Wout
            khatb, bhatb = Wexb, Woutb
            rk, bon = sq, f32()
            rkb, bonb = sqb, P.buf("rbon")
            Vtm = Ax.alloc([64, NC, 128], F32)
            Ktm = Ax.alloc([64, NC, 128], F32)
            Btm = Ax.alloc([64, NC, 128], F32)
            Vtmb, Ktmb, Btmb = P.buf("rVtm"), P.buf("rKtm"), P.buf("rBtm")
            Ak = Ax.alloc([64, 8, 128], F32)
            Ab = Ax.alloc([64, 8, 128], F32)
            NT = Ax.alloc([64, 8, 64], BF16)
            P0b = Ax.alloc([64, 8, 64], BF16)
            Akb, Abb, NTb, P0bb = P.buf("rAk"), P.buf("rAb"), P.buf("rNT"), P.buf("rP0b")
            Pw = [Ax.alloc([64, 8, 64], BF16) for _ in range(2)]
            PTw = [Ax.alloc([64, 8, 64], BF16) for _ in range(2)]
            Pwb, PTwb = P.bufs("rPw", 2), P.bufs("rPTw", 2)
            Tm = Ax.alloc([64, 8, 64], BF16)
            Tmb = P.buf("rTm")
            TmF = Ax.alloc([64, 8, 64], F32)
            TmFb = P.buf("rTmF")
            Xs = Ax.alloc([64, 128], F32)
            Us = Ax.alloc([64, 128], F32)
            Xsb, Usb = P.buf("rXs"), P.buf("rUs")
            S = Ax.alloc([128, 64], F32)
            Sb = P.buf("rS")
            yraw, yc, ysq, yrs = Lp, Lx, sq, rn
            yrawb, ycb, ysqb, yrsb = Lpb, Lxb, sqb, rnb
            ones64 = self.cf[:, C_ONE64:C_ONE64 + 64]
            amask = self.cf[0:64, C_AMASK:C_AMASK + 512]
            ntmask = self.cf[0:64, C_NTMASK:C_NTMASK + 512]
            id8 = self.cf[0:64, C_ID8:C_ID8 + 512]
            pk = 0
            for hp in hps:
                cols = slice(hp * 128, (hp + 1) * 128)
                self.P.op("dve", lambda e: e.memset(S[:, :], 0.0), reads=[], writes=[Sb])
                for tb in range(T // n):
                    q = pk % 2
                    pk += 1
                    tsl = slice(tb * n, (tb + 1) * n)
                    for g, (dst, dstb) in enumerate(((rr, rrb), (kx, kxb), (vv, vvb))):
                        self.dma("sp", dst[:, :], self.rkv[g * 8 + hp, :, tsl], [self.rkvb], writes=[dstb],
                                 owner=dstb)
                    yield
                    if RSTOP == 1:
                        return
                    bw = self.next_bank()
                    self.mm(self.bank(bw)[:, 0:n], lw[0:64, cols], zwa[0:64, tsl], True, True, [lwb, zb],
                            self.pb[bw])
                    self.act(sw[:, :], self.bank(bw)[:, 0:n], AF.Sigmoid, [self.pb[bw], vb], writes=[swb],
                             bias=self.vcol(li, "w0", hp))
                    ba = self.next_bank()
                    self.mm(self.bank(ba)[:, 0:n], lw[64:128, cols], zwa[64:128, tsl], True, True, [lwb, zb],
                            self.pb[ba])
                    self.act(asig[:, :], self.bank(ba)[:, 0:n], AF.Sigmoid, [self.pb[ba], vb], writes=[asigb],
                             bias=self.vcol(li, "a0", hp))
                    bg = self.next_bank()
                    self.mm(self.bank(bg)[:, 0:n], lg0[:, cols], sg0[:, tsl], True, False, [lwb, zb], self.pb[bg])
                    self.mm(self.bank(bg)[:, 0:n], lg1[0:32, cols], sgv[0:32, tsl], False, True, [lwb, zb],
                            self.pb[bg])
                    self.copy("act", gg[:, :], self.bank(bg)[:, 0:n], [self.pb[bg]], writes=[ggb])
                    yield
                    if RSTOP == 2:
                        return
                    if not first:
                        bv = self.next_bank()
                        self.mm(self.bank(bv)[:, 0:n], lg1[32:64, cols], sgv[32:64, tsl], True, True, [lwb, zb],
                                self.pb[bv])
                        self.act(vs[:, :], self.bank(bv)[:, 0:n], AF.Sigmoid, [self.pb[bv], vb], writes=[vsb],
                                 bias=self.vcol(li, "v0", hp))
                        self.dma("sp", vf[:, :], self.vfirst[hp, :, tsl], [self.vfirstb], writes=[vfb_],
                                 owner=vfb_)
                        self.tt("dve", dtmp[:, :], vf[:, :], vv[:, :], ALU.subtract, [vfb_, vvb], writes=[dtmpb])
                        self.tt("dve", dtmp[:, :], dtmp[:, :], vs[:, :], ALU.mult, [dtmpb, vsb], writes=[dtmpb])
                        self.tt("dve", vv[:, :], vv[:, :], dtmp[:, :], ALU.add, [vvb, dtmpb], writes=[vvb])
                    yield
                    if RSTOP == 3:
                        return
                    self.ts("dve", kk[:, :], kx[:, :], self.vcol(li, "k_k", hp), None, ALU.mult, None, [kxb, vb],
                            writes=[kkb])
                    self.act(sq[:, :], kk[:, :], AF.Square, [kkb], writes=[sqb])
                    bn = self.next_bank()
                    self.mm(self.bank(bn)[:, 0:n], bones, sq[:, :], True, True, [cfb, sqb], self.pb[bn])
                    self.act(rn[:, :], self.bank(bn)[:, 0:n], AF.Sqrt, [self.pb[bn]], writes=[rnb])
                    self.ts("dve", rn[:, :], rn[:, :], 1e-12, None, ALU.max, None, [rnb], writes=[rnb])
                    self.P.op("dve", lambda e: e.reciprocal(out=rn[:, :], in_=rn[:, :]), reads=[rnb], writes=[rnb])
                    self.tt("dve", kk[:, :], kk[:, :], rn[:, :], ALU.mult, [kkb, rnb], writes=[kkb])
                    self.ts("dve", dtmp[:, :], asig[:, :], -1.0, self.vcol(li, "k_a", hp), ALU.add, ALU.mult,
                            [asigb, vb], writes=[dtmpb])
                    self.stt(k2[:, :], dtmp[:, :], 1.0, kx[:, :], ALU.add, ALU.mult, [dtmpb, kxb], writes=[k2b])
                    self.tt("dve", bvec[:, :], kk[:, :], asig[:, :], ALU.mult, [kkb, asigb], writes=[bvecb])
                    yield
                    if RSTOP == 4:
                        return
                    for c in range(NC):
                        cs = slice(c * 64, (c + 1) * 64)
                        self.P.op("dve", (lambda o, d1: (lambda e: e.tensor_tensor_scan(
                            out=o, data0=ones64, data1=d1, initial=0.0, op0=ALU.mult, op1=ALU.add)))(
                            Lp[:, cs], sw[:, cs]), reads=[swb, cfb], partial=[Lpb])
                    self.act(Win[:, :], Lp[:, :], AF.Exp, [Lpb], writes=[Winb], scale=-c0)
                    self.act(Wout[:, :], Lp[:, :], AF.Exp, [Lpb], writes=[Woutb], scale=c0)
                    self.tt("dve", Lx[:, :], Lp[:, :], sw[:, :], ALU.subtract, [Lpb, swb], writes=[Lxb])
                    self.act(Wex[:, :], Lx[:, :], AF.Exp, [Lxb], writes=[Wexb], scale=-c0)
                    for c in range(NC):
                        cs = slice(c * 64, (c + 1) * 64)
                        self.ts("dve", Lx[:, cs], Lp[:, cs], -1.0, Lp[:, c * 64 + 63:c * 64 + 64], ALU.mult,
                                ALU.add, [Lpb, Wexb], partial=[Lxb])
                    self.act(Wend[:, :], Lx[:, :], AF.Exp, [Lxb], writes=[Wendb], scale=-c0)
                    yield
                    if RSTOP == 5:
                        return

                    def v3(t):
                        return t[:, :].rearrange("p (c s) -> p c s", c=NC)
                    self.stt(AR[:, :, 0, :], v3(kk), -1.0, v3(Wex), ALU.mult, ALU.mult, [kkb, Wexb], partial=[ARb])
                    self.tt("dve", AR[:, :, 1, :], v3(rr), v3(Win), ALU.mult, [rrb, Winb], partial=[ARb])
                    self.tt("dve", BK[:, :, 0, :], v3(bvec), v3(Wout), ALU.mult, [bvecb, Woutb], partial=[BKb])
                    self.tt("dve", BK[:, :, 1, :], v3(k2), v3(Wout), ALU.mult, [k2b, Woutb], partial=[BKb])
                    self.tt("dve", khat[:, :], k2[:, :], Wend[:, :], ALU.mult, [k2b, Wendb], writes=[khatb])
                    self.tt("dve", bhat[:, :], bvec[:, :], Wend[:, :], ALU.mult, [bvecb, Wendb], writes=[bhatb])
                    self.stt(rk[:, :], rr[:, :], self.vcol(li, "r_k", hp), k2[:, :], ALU.mult, ALU.mult,
                             [rrb, k2b, vb], writes=[rkb])
                    bb_ = self.next_bank()
                    self.mm(self.bank(bb_)[:, 0:n], bones, rk[:, :], True, True, [cfb, rkb], self.pb[bb_])
                    self.tt("dve", bon[:, :], self.bank(bb_)[:, 0:n], vv[:, :], ALU.mult, [self.pb[bb_], vvb],
                            writes=[bonb])
                    yield
                    if RSTOP == 6:
                        return
                    for (src_, srcb, dst_, dstb_) in ((vv, vvb, Vtm, Vtmb), (khat, khatb, Ktm, Ktmb),
                                                      (bhat, bhatb, Btm, Btmb)):
                        bt = self.next_bank()
                        for c in range(NC):
                            self.tr(self.bank(bt)[0:64, c * 128:(c + 1) * 128], src_[:, c * 64:(c + 1) * 64],
                                    self.ident_f, [srcb, cfb], self.pb[bt])
                        self.copy("act", dst_[:, :, :], self.bank(bt)[0:64, :].rearrange("p (c f) -> p c f", c=NC),
                                  [self.pb[bt]], writes=[dstb_])
                    yield
                    if RSTOP == 7:
                        return
                    for (dst_, dstb_, which) in ((Ak, Akb, 1), (Ab, Abb, 0)):
                        for hh in range(2):
                            hs = slice(hh * 64, hh * 64 + 64)
                            bsx = self.next_bank()
                            for c in range(NC):
                                self.mm(self.bank(bsx)[0:64, c * 128:(c + 1) * 128], BK[hs, c, which, :],
                                        AR[hs, c, :, :].rearrange("p a s -> p (a s)"), True, True, [BKb, ARb],
                                        self.pb[bsx])
                            if R8SUB == 1 or (R8SUB == 3 and hh == 1):
                                return
                            self.tt("dve", dst_[:, hh * 4:(hh + 1) * 4, :],
                                    self.bank(bsx)[0:64, :].rearrange("p (u f) -> p u f", u=4),
                                    amask.rearrange("p (u f) -> p u f", u=4), ALU.mult, [self.pb[bsx], cfb],
                                    partial=[dstb_])
                            if R8SUB == 2:
                                return
                    for hh in range(2):
                        hs = slice(hh * 64, hh * 64 + 64)
                        bsx = self.next_bank()
                        for c in range(NC):
                            self.mm(self.bank(bsx)[0:64, c * 64:(c + 1) * 64], AR[hs, c, 0, :], BK[hs, c, 0, :], True,
                                    True, [BKb, ARb], self.pb[bsx])
                        self.tt("dve", NT[:, hh * 4:(hh + 1) * 4, :],
                                self.bank(bsx)[0:64, 0:256].rearrange("p (u f) -> p u f", u=4),
                                ntmask[:, 0:256].rearrange("p (u f) -> p u f", u=4), ALU.mult, [self.pb[bsx], cfb],
                                partial=[NTb])
                    yield
                    if RSTOP == 8:
                        return
                    self.tt("dve", Tm[:, :, :], Ab[:, :, 0:64], id8.rearrange("p (u f) -> p u f", u=8), ALU.add,
                            [Abb, cfb], writes=[Tmb])
                    self.copy("act", P0b[:, :, :], Ab[:, :, 0:64], [Abb], writes=[P0bb])
                    curP = lambda u: P0b[:, u, :]
                    curPT = lambda u: NT[:, u, :]
                    curPb, curPTb = P0bb, NTb
                    for lv in range(5):
                        yield
                        z = lv % 2
                        last = lv == 4
                        bpt = self.next_bank()
                        for u in range(8):
                            self.mm(self.bank(bpt)[0:64, u * 64:(u + 1) * 64], curP(u), curPT(u), True, True,
                                    [curPb, curPTb], self.pb[bpt])
                        if not last:
                            bp = self.next_bank()
                            for u in range(8):
                                self.mm(self.bank(bp)[0:64, u * 64:(u + 1) * 64], curPT(u), curP(u), True, True,
                                        [curPb, curPTb], self.pb[bp])
                        self.copy("act", PTw[z][:, :, :], self.bank(bpt)[0:64, :].rearrange("p (u f) -> p u f", u=8),
                                  [self.pb[bpt]], writes=[PTwb[z]])
                        if not last:
                            self.copy("act", Pw[z][:, :, :], self.bank(bp)[0:64, :].rearrange("p (u f) -> p u f", u=8),
                                      [self.pb[bp]], writes=[Pwb[z]])
                        bT = self.next_bank()
                        for u in range(8):
                            self.mm(self.bank(bT)[0:64, u * 64:(u + 1) * 64], PTw[z][:, u, :], Tm[:, u, :], True, True,
                                    [PTwb[z], Tmb], self.pb[bT])
                        if not last:
                            self.tt("dve", Tm[:, :, :], self.bank(bT)[0:64, :].rearrange("p (u f) -> p u f", u=8),
                                    Tm[:, :, :], ALU.add, [self.pb[bT], Tmb], writes=[Tmb])
                        else:
                            self.tt("dve", TmF[:, :, :], self.bank(bT)[0:64, :].rearrange("p (u f) -> p u f", u=8),
                                    Tm[:, :, :], ALU.add, [self.pb[bT], Tmb], writes=[TmFb])
                        curP = (lambda zz: (lambda u: Pw[zz][:, u, :]))(z)
                        curPT = (lambda zz: (lambda u: PTw[zz][:, u, :]))(z)
                        curPb, curPTb = Pwb[z], PTwb[z]
                    yield
                    if RSTOP == 9:
                        return
                    for c in range(NC):
                        yield
                        for hh in range(2):
                            u = hh * 4 + c
                            hs = slice(hh * 64, hh * 64 + 64)
                            fs = slice(hh * 64, hh * 64 + 64)
                            bX = self.next_bank()
                            o = self.bank(bX)[0:64, 0:64]
                            if hh == 0:
                                self.mm(o, AR[hs, c, 0, :], S[hs, :], True, False, [ARb, Sb], self.pb[bX])
                                self.mm(o, Ak[:, u, 0:64], Vtm[:, c, fs], False, True, [Akb, Vtmb], self.pb[bX])
                                self.copy("act", Xs[:, fs], o, [self.pb[bX]], partial=[Xsb])
                            else:
                                self.mm(o, AR[hs, c, 0, :], S[hs, :], True, True, [ARb, Sb], self.pb[bX])
                                bX2 = self.next_bank()
                                o2 = self.bank(bX2)[0:64, 0:64]
                                self.mm(o2, Ak[:, u, 0:64], Vtm[:, c, fs], True, True, [Akb, Vtmb], self.pb[bX2])
                                self.copy("act", Xs[:, fs], o, [self.pb[bX]], partial=[Xsb])
                                self.tt("dve", Xs[:, fs], o2, Xs[:, fs], ALU.add, [self.pb[bX2], Xsb], partial=[Xsb])
                        bU = self.next_bank()
                        for hh in range(2):
                            u = hh * 4 + c
                            fs = slice(hh * 64, hh * 64 + 64)
                            self.mm(self.bank(bU)[0:64, fs], TmF[:, u, :], Xs[:, fs], True, True, [TmFb, Xsb],
                                    self.pb[bU])
                        self.copy("act", Us[:, :], self.bank(bU)[0:64, 0:128], [self.pb[bU]], writes=[Usb])
                        for hh in range(2):
                            u = hh * 4 + c
                            hs = slice(hh * 64, hh * 64 + 64)
                            fs = slice(hh * 64, hh * 64 + 64)
                            cs = slice(c * 64, (c + 1) * 64)
                            bY = self.next_bank()
                            bS = self.next_bank()
                            oy = self.bank(bY)[hs, 0:64]
                            os_ = self.bank(bS)[hs, 0:64]
                            if hh == 0:
                                self.mm(oy, S[hs, :], AR[hs, c, 1, :], True, False, [Sb, ARb], self.pb[bY])
                                self.mm(oy, Us[:, fs], Ab[:, u, 64:128], False, False, [Usb, Abb], self.pb[bY])
                                self.mm(oy, Vtm[:, c, fs], Ak[:, u, 64:128], False, True, [Vtmb, Akb], self.pb[bY])
                                self.copy("act", yraw[hs, cs], oy, [self.pb[bY]], partial=[yrawb])
                            else:
                                self.mm(oy, S[hs, :], AR[hs, c, 1, :], True, True, [Sb, ARb], self.pb[bY])
                                bY2 = self.next_bank()
                                oy2 = self.bank(bY2)[hs, 0:64]
                                self.mm(oy2, Us[:, fs], Ab[:, u, 64:128], True, False, [Usb, Abb], self.pb[bY2])
                                self.mm(oy2, Vtm[:, c, fs], Ak[:, u, 64:128], False, True, [Vtmb, Akb], self.pb[bY2])
                                self.copy("act", yraw[hs, cs], oy, [self.pb[bY]], partial=[yrawb])
                                self.tt("dve", yraw[hs, cs], oy2, yraw[hs, cs], ALU.add, [self.pb[bY2], yrawb],
                                        partial=[yrawb])
                            self.mm(os_, Btm[:, c, fs], Us[:, fs], True, False, [Btmb, Usb], self.pb[bS])
                            self.mm(os_, Ktm[:, c, fs], Vtm[:, c, fs], False, True, [Ktmb, Vtmb], self.pb[bS])
                            self.stt(S[hs, :], S[hs, :], Win[hs, c * 64 + 63:c * 64 + 64], os_,
                                     ALU.mult, ALU.add, [Sb, Winb, self.pb[bS]], partial=[Sb])
                    yield
                    if RSTOP == 10:
                        return
                    bm = self.next_bank()
                    self.mm(self.bank(bm)[:, 0:n], bones, yraw[:, :], True, True, [cfb, yrawb], self.pb[bm])
                    self.stt(yc[:, :], self.bank(bm)[:, 0:n], -1.0 / 64, yraw[:, :], ALU.mult, ALU.add,
                             [self.pb[bm], yrawb], writes=[ycb])
                    self.act(ysq[:, :], yc[:, :], AF.Square, [ycb], writes=[ysqb])
                    bv2 = self.next_bank()
                    self.mm(self.bank(bv2)[:, 0:n], bones, ysq[:, :], True, True, [cfb, ysqb], self.pb[bv2])
                    self.ts("dve", yrs[:, :], self.bank(bv2)[:, 0:n], 1.0 / 64, GN_EPS, ALU.mult, ALU.add,
                            [self.pb[bv2]], writes=[yrsb])
                    self.act(yrs[:, :], yrs[:, :], AF.Sqrt, [yrsb], writes=[yrsb])
                    self.P.op("dve", lambda e: e.reciprocal(out=yrs[:, :], in_=yrs[:, :]), reads=[yrsb],
                              writes=[yrsb])
                    self.tt("dve", yc[:, :], yc[:, :], yrs[:, :], ALU.mult, [ycb, yrsb], writes=[ycb])
                    self.ts("dve", yc[:, :], yc[:, :], self.vcol(li, "gn_w", hp), self.vcol(li, "gn_b", hp),
                            ALU.mult, ALU.add, [ycb, vb], writes=[ycb])
                    self.tt("dve", yc[:, :], yc[:, :], bon[:, :], ALU.add, [ycb, bonb], writes=[ycb])
                    self.tt("dve", yT[:, hp, tsl], yc[:, :], gg[:, :], ALU.mult, [ycb, ggb], partial=[yTb])

        A.reset(mz)
        P.fence()
        A2 = Arena(self.nc)
        A2.off = m_xn
        A2.limit = m_xn + xn_bytes
        nhp = int(os.environ.get('NHP', 8))
        gens = [stream(A2, [h for h in range(nhp) if h % 2 == 0]),
                stream(A, [h for h in range(nhp) if h % 2 == 1])]
        active = list(gens)
        while active:
            for g_ in list(active):
                try:
                    next(g_)
                except StopIteration:
                    active.remove(g_)
        self.A.peak = max(self.A.peak, A2.peak)

    def moba_rwkv(self, li, only=None):
        A, P = self.A, self.P
        m = A.mark()
        p = "l%d_" % li
        yT = A.alloc([128, 8, T], BF16)
        yTb = P.buf("eyT")
        m_xn = A.mark()
        xn = A.alloc([128, NCH, T], BF16)
        xnb = P.buf("exn")
        m1 = A.mark()
        xn_bytes = m1 - m_xn
        if only == "B":
            A.off = m1 + 70 * 1024
            self.rmsnorm(li, "norm_mix", xn, xnb, 0, T, "e")
            A.reset(m1)
            P.fence()
            self.rwkv(li, xn, xnb, yT, yTb, m_xn, xn_bytes)
            for h in range(int(os.environ.get('NHP', 8))):
                self.dump(yT[:, h, :], h * T, T, [yTb])
            A.reset(m)
            return
        self._attn_phase(li, xn, xnb, yT, yTb)
        if only == "A":
            for h in range(8):
                self.dump(yT[:, h, :], h * T, T, [yTb])
            A.reset(m)
            return
        A.reset(m1)
        P.fence()
        self.out_proj(self.w[p + "w_out"][0:1024, :], yT, yTb, 8, "ea")
        A.reset(m1)
        P.fence()
        self.rwkv(li, xn, xnb, yT, yTb, m_xn, xn_bytes)
        A.reset(m1)
        P.fence()
        self.out_proj(self.w[p + "w_out"][1024:2048, :], yT, yTb, 8, "eb")
        A.reset(m)

    def _attn_phase(self, li, xn, xnb, yT, yTb):
        A = self.A
        m = A.mark()
        top = A.mark()
        self.attention_alloc_only = True
        A.reset(top)
        A.off = top + 62 * 1024
        self.rmsnorm(li, "norm_mix", xn, xnb, 0, T, "e")
        A.reset(top)
        self.attention(li, xn, xnb, yT, yTb)
        assert A.off <= top + 62 * 1024, A.off - top
        A.reset(m)

    def mixer(self, li):
        if li % 2 == 1:
            self.sconv(li)
        else:
            self.moba_rwkv(li)


BIG_WEIGHTS = {
    0: [("w_in", (D, 6432)), ("w_out", (D, D)), ("w_lora", (64, 1024)), ("a_lora", (64, 1024)),
        ("g_lora", (160, 1024)), ("ffn_up", (D, 2 * DFF)), ("ffn_down", (DFF, D))],
    1: [("conv_in", (D, 3 * D)), ("conv_out", (D, D)), ("ffn_up", (D, 2 * DFF)), ("ffn_down", (DFF, D))],
    2: [("w_in", (D, 6464)), ("w_out", (D, D)), ("w_lora", (64, 1024)), ("a_lora", (64, 1024)),
        ("g_lora", (160, 1024)), ("v_lora", (32, 1024)), ("ffn_up", (D, 2 * DFF)), ("ffn_down", (DFF, D))],
    3: [("conv_in", (D, 3 * D)), ("conv_out", (D, D)), ("ffn_up", (D, 2 * DFF)), ("ffn_down", (DFF, D))],
}


def build_program(stages):
    B = Builder()
    layers = sorted(set(int(s[-1]) for s in stages))
    B.load_consts()
    for li in layers:
        B.load_vecs(li)
    for st in stages:
        li = int(st[-1])
        for nm, shp in BIG_WEIGHTS[li]:
            key = "l%d_%s" % (li, nm)
            if key not in B.w and ((st.startswith("ffn") and nm.startswith("ffn")) or
                                   (st.startswith("mix") and not nm.startswith("ffn"))):
                B.dram_in(key, shp)
    B.P.fence()
    B.prologue()
    for st in stages:
        li = int(st[-1])
        B.P.fence()
        if st.startswith("ffn"):
            B.ffn(li)
        elif st.startswith("mixA"):
            B.moba_rwkv(li, only="A")
        elif st.startswith("mixB"):
            B.moba_rwkv(li, only="B")
        else:
            B.mixer(li)
    B.P.fence()
    B.epilogue()
    B.P.emit(final_waits=[B.outb, B.dbgb])
    return B


def make_in_map(B, inputs, xb):
    cf, cb = build_consts()
    m = {"x": np.ascontiguousarray(xb, dtype=np.float32), "consts": cf, "constsb": cb}
    for li in B.vecs:
        m["vecs%d" % li] = build_layer_vecs(li, inputs)
    for key in B.w:
        m[key] = np.ascontiguousarray(inputs[key], dtype=np.float32)
    return m


ALL_STAGES = ["mix0", "ffn0", "mix1", "ffn1", "mix2", "ffn2", "mix3", "ffn3"]
_CACHE = {}


def kernel(**inputs):
    x = np.asarray(inputs["x"], dtype=np.float32)
    nb = x.shape[0]
    if "B" not in _CACHE:
        _CACHE["B"] = build_program(ALL_STAGES)
    B = _CACHE["B"]
    shared = make_in_map(B, inputs, x[0])
    in_maps = []
    for b in range(nb):
        m = dict(shared)
        m["x"] = np.ascontiguousarray(x[b])
        in_maps.append(m)
    res = run_bass_kernel_spmd(B.nc, in_maps, core_ids=list(range(nb)))
    out = np.stack([np.asarray(res.results[b]["out"], dtype=np.float32) for b in range(nb)], axis=0)
    return out
```

```python
import os
import numpy as np
from contextlib import ExitStack
import concourse.bass as bass
import concourse.mybir as mybir
from concourse.bass_utils import run_bass_kernel_spmd

F32 = mybir.dt.float32
BF16 = mybir.dt.bfloat16
AF = mybir.ActivationFunctionType
ALU = mybir.AluOpType
AX = mybir.AxisListType


class Sem:
    __slots__ = ("total", "handle", "name")

    def __init__(self, name):
        self.total = 0
        self.handle = None
        self.name = name


class Buf:
    __slots__ = ("name", "w_eng", "w_dma", "r_eng", "r_dma", "p_eng", "p_dma", "sem")

    def __init__(self, name):
        self.name = name
        self.w_eng = {}
        self.w_dma = []
        self.r_eng = {}
        self.r_dma = []
        self.p_eng = {}
        self.p_dma = []
        self.sem = None


class Prog:
    ENGS = ("pe", "act", "dve", "pool", "sp")

    def __init__(self, nc, es):
        self.nc = nc
        self.es = es
        self.ins = []
        self.nbuf = 0
        self.fence_idx = None
        self.local = []
        self.last_eng = {}
        self.dma_since = []
        self.free_sems = []
        self.all_sems = []

    def buf(self, name, persistent=False):
        self.nbuf += 1
        b = Buf("%s_%d" % (name, self.nbuf))
        if self.fence_idx is not None:
            b.p_eng = {"sp": self.fence_idx}
        if not persistent:
            self.local.append(b)
        return b

    def bufs(self, name, n, persistent=False):
        return [self.buf("%s%d" % (name, i), persistent) for i in range(n)]

    def fence(self):
        idx = len(self.ins)
        deps = set(self.last_eng.values()) | set(self.dma_since)
        rec = dict(eng="sp", fn=lambda e: e.nop(), deps=deps, dma=False, sem=None, total_at={},
                   signal=False)
        for d in deps:
            dr = self.ins[d]
            if dr["dma"]:
                rec["total_at"][d] = dr["sem"].total
        self.ins.append(rec)
        self.last_eng["sp"] = idx
        self.fence_idx = idx
        self.dma_since = []
        for b in self.local:
            if b.sem is not None:
                self.free_sems.append(b.sem)
                b.sem = None
        self.local = []

    def op(self, eng, fn, reads=(), writes=(), partial=(), dma_owner=None):
        idx = len(self.ins)
        deps = set()
        for b in reads:
            deps.update(b.w_eng.values())
            deps.update(b.w_dma)
        for b in writes:
            deps.update(b.w_eng.values())
            deps.update(b.w_dma)
            deps.update(b.r_eng.values())
            deps.update(b.r_dma)
            deps.update(b.p_eng.values())
            deps.update(b.p_dma)
        for b in partial:
            if b.r_eng or b.r_dma:
                b.p_eng, b.p_dma = b.r_eng, b.r_dma
                b.r_eng, b.r_dma = {}, []
                b.w_eng, b.w_dma = {}, []
            deps.update(b.p_eng.values())
            deps.update(b.p_dma)
        is_dma = dma_owner is not None
        rec = dict(eng=eng, fn=fn, deps=deps, dma=is_dma, sem=None, total_at={}, signal=False)
        for d in deps:
            dr = self.ins[d]
            if dr["dma"]:
                rec["total_at"][d] = dr["sem"].total
        if is_dma:
            if dma_owner.sem is None:
                kind = "sw" if eng == "pool" else "hw"
                pool = [s for s in self.free_sems if s.name.startswith(kind)]
                if pool:
                    dma_owner.sem = pool[-1]
                    self.free_sems.remove(pool[-1])
                else:
                    dma_owner.sem = Sem("%s%d" % (kind, len(self.all_sems)))
                    self.all_sems.append(dma_owner.sem)
            else:
                assert dma_owner.sem.name.startswith("sw") == (eng == "pool"), dma_owner.name
            rec["sem"] = dma_owner.sem
            dma_owner.sem.total += 16
            rec["tok"] = dma_owner.sem.total
            self.dma_since.append(idx)
        self.ins.append(rec)
        self.last_eng[eng] = idx
        for b in reads:
            if is_dma:
                b.r_dma.append(idx)
            else:
                b.r_eng[eng] = idx
        for b in writes:
            b.w_eng, b.w_dma, b.r_eng, b.r_dma, b.p_eng, b.p_dma = {}, [], {}, [], {}, []
            if is_dma:
                b.w_dma.append(idx)
            else:
                b.w_eng[eng] = idx
        for b in partial:
            if is_dma:
                b.w_dma.append(idx)
            else:
                b.w_eng[eng] = idx
        return idx

    def emit(self, final_waits=()):
        nc = self.nc
        es = self.es
        ins = self.ins
        for r in ins:
            for d in r["deps"]:
                dr = ins[d]
                if not dr["dma"]:
                    if dr["eng"] == "pe" and r["eng"] == "pe" and not r["dma"]:
                        continue
                    dr["signal"] = True
        esem = {e: es.enter_context(nc.semaphore("s_" + e)) for e in self.ENGS}
        cnt = {e: 0 for e in self.ENGS}
        for r in ins:
            if not r["dma"] and r["signal"]:
                cnt[r["eng"]] += 1
                r["cnt"] = cnt[r["eng"]]
        for s in self.all_sems:
            s.handle = es.enter_context(nc.semaphore(s.name))
        self.n_dma_sems = len(self.all_sems)
        per_eng = {e: [] for e in self.ENGS}
        for i, r in enumerate(ins):
            per_eng[r["eng"]].append(i)
        last_outs = [(b.sem.handle, b.sem.total) for b in final_waits if b.sem is not None]

        def run_engine(ename, eng):
            waited = {}
            for i in per_eng[ename]:
                r = ins[i]
                need = {}
                for d in r["deps"]:
                    dr = ins[d]
                    if dr["dma"]:
                        s = dr["sem"].handle
                        v = max(r["total_at"][d], dr["tok"])
                    else:
                        if dr["eng"] == "pe" and ename == "pe" and not r["dma"]:
                            continue
                        s = esem[dr["eng"]]
                        v = dr["cnt"]
                    key = id(s)
                    if key not in need or need[key][1] < v:
                        need[key] = (s, v)
                for key, (s, v) in need.items():
                    if waited.get(key, 0) >= v:
                        continue
                    eng.wait_ge(s, v)
                    waited[key] = v
                bi = r["fn"](eng)
                if r["dma"]:
                    bi.then_inc(r["sem"].handle, 16)
                elif r["signal"]:
                    bi.then_inc(esem[ename], 1)
            if ename == "sp":
                for h, v in last_outs:
                    eng.wait_ge(h, v)

        with nc.Block() as block:
            @block.tensor
            def _(e):
                run_engine("pe", e)

            @block.scalar
            def _(e):
                run_engine("act", e)

            @block.vector
            def _(e):
                run_engine("dve", e)

            @block.gpsimd
            def _(e):
                run_engine("pool", e)

            @block.sync
            def _(e):
                run_engine("sp", e)


T = 2048
D = 2048
NCH = D // 128
DFF = 5632
NJ = DFF // 128
RMS_EPS = 1e-6
RSTOP = int(os.environ.get('RSTOP', 99))
R8SUB = int(os.environ.get('R8SUB', 0))
GN_EPS = 64e-5
DBG_N = 16384
import os
NTT = int(os.environ.get('NTT', T // 128))
SB_BASE = 16640
SB_LIMIT = 229000


def _dtsize(dt):
    return 4 if dt == F32 else 2


class Arena:
    def __init__(self, nc):
        self.nc = nc
        self.off = SB_BASE
        self.n = 0
        self.peak = 0

    def alloc(self, shape, dtype):
        nb = _dtsize(dtype)
        for s in shape[1:]:
            nb *= s
        off = (self.off + 31) // 32 * 32
        h = self.nc.alloc_sbuf_tensor_at("t%d" % self.n, list(shape), dtype, offset=off)
        self.n += 1
        self.off = off + nb
        self.peak = max(self.peak, self.off)
        assert self.off <= getattr(self, "limit", SB_LIMIT), ("SBUF overflow", self.off)
        return h

    def mark(self):
        return self.off

    def reset(self, m):
        self.off = m


def pack_vec(v):
    v = np.asarray(v, np.float32).reshape(-1)
    n = (v.size + 127) // 128
    if v.size != n * 128:
        v = np.concatenate([v, np.zeros(n * 128 - v.size, np.float32)])
    return np.ascontiguousarray(v.reshape(n, 128).T)


class VecPack:
    def __init__(self):
        self.cols = {}
        self.n = 0
        self.parts = []

    def add(self, name, arr2d):
        self.cols[name] = (self.n, arr2d.shape[1])
        self.n += arr2d.shape[1]
        self.parts.append(arr2d)

    def array(self):
        return np.ascontiguousarray(np.concatenate(self.parts, axis=1).astype(np.float32))


def layer_vec_layout(li):
    lay = {}
    n = 0

    def add(name, c):
        nonlocal n
        lay[name] = (n, c)
        n += c
    add("norm_mix", 16)
    if li % 2 == 1:
        add("conv_w", 48)
    else:
        add("q_gain", 1)
        add("k_gain", 1)
        nmu = 27
        add("shift_mu", nmu)
        for nm in ("w0", "a0", "k_k", "k_a", "r_k", "gn_w", "gn_b", "v0"):
            add(nm, 8)
    add("norm_ffn", 16)
    add("ffn_conv", 3 * 88)
    return lay, n


def build_layer_vecs(li, inputs):
    p = "l%d_" % li
    lay, n = layer_vec_layout(li)
    out = np.zeros((128, n), np.float32)

    def put(name, arr2d):
        c, w = lay[name]
        assert arr2d.shape[1] <= w, (name, arr2d.shape, w)
        out[:, c:c + arr2d.shape[1]] = arr2d
    put("norm_mix", pack_vec(inputs[p + "norm_mix"]))
    if li % 2 == 1:
        cw = np.asarray(inputs[p + "conv_w"])
        put("conv_w", np.concatenate([pack_vec(cw[j]) for j in range(3)], axis=1))
    else:
        put("q_gain", pack_vec(inputs[p + "q_gain"]))
        put("k_gain", pack_vec(inputs[p + "k_gain"]))
        put("shift_mu", pack_vec(inputs[p + "shift_mu"]))
        for nm in ("w0", "a0", "k_k", "k_a", "gn_w", "gn_b"):
            put(nm, pack_vec(inputs[p + nm]))
        put("r_k", pack_vec(np.asarray(inputs[p + "r_k"]).reshape(-1)))
        if li > 0:
            put("v0", pack_vec(inputs[p + "v0"]))
    put("norm_ffn", pack_vec(inputs[p + "norm_ffn"]))
    fc = np.asarray(inputs[p + "ffn_conv"])
    put("ffn_conv", np.concatenate([pack_vec(fc[j]) for j in range(3)], axis=1))
    return out


C_IDENT = 0
C_BONES = 128
C_BIASCOL = 256
C_GMASK = 264
C_AMASK = 328
C_NTMASK = 840
C_ONE64 = 1352
C_ID8 = 1416
NCONST = 1928
B_IDENT = 0
B_ONES = 128
B_CAUSAL = 256
B_ONEHOT = 384
B_DL = 1408
B_DS = 3456
NCONST_BF = 4480
A_HEADS = 8


def build_consts():
    c = np.zeros((128, NCONST), np.float32)
    c[:, C_IDENT:C_IDENT + 128] = np.eye(128, dtype=np.float32)
    bo = np.zeros((128, 128), np.float32)
    bo[:64, :64] = 1.0
    bo[64:, 64:] = 1.0
    c[:, C_BONES:C_BONES + 128] = bo
    slopes = np.exp2(-8.0 * np.arange(1, A_HEADS + 1) / A_HEADS).astype(np.float32)
    pidx = np.arange(128, dtype=np.float32)
    for h in range(A_HEADS):
        c[:, C_BIASCOL + h] = slopes[h] * (pidx - 127.0)
    for i in range(8):
        qb = 4 + i // 2
        for n in range(8):
            c[:, C_GMASK + i * 8 + n] = 0.0 if n < qb else -1e30
    s = np.arange(64)[:, None]
    t = np.arange(64)[None, :]
    am = np.concatenate([(s < t), (s <= t)], axis=1).astype(np.float32)
    c[:64, C_AMASK:C_AMASK + 512] = np.tile(am, (1, 4))
    nt = (t < s).astype(np.float32)
    c[:64, C_NTMASK:C_NTMASK + 512] = np.tile(nt, (1, 8))
    c[:, C_ONE64:C_ONE64 + 64] = 1.0
    c[:64, C_ID8:C_ID8 + 512] = np.tile(np.eye(64, dtype=np.float32), (1, 8))
    b = np.zeros((128, NCONST_BF), np.float32)
    b[:, B_IDENT:B_IDENT + 128] = np.eye(128, dtype=np.float32)
    b[:, B_ONES:B_ONES + 128] = 1.0
    kk = np.arange(128)[:, None]
    qq = np.arange(128)[None, :]
    b[:, B_CAUSAL:B_CAUSAL + 128] = np.where(kk > qq, -30000.0, 0.0)
    for n in range(8):
        b[n, B_ONEHOT + n * 128:B_ONEHOT + (n + 1) * 128] = 1.0
    for dl in range(16):
        b[0, B_DL + dl * 128:B_DL + (dl + 1) * 128] = float(dl)
    for h in range(A_HEADS):
        b[0, B_DS + h * 128:B_DS + (h + 1) * 128] = -128.0 * slopes[h]
    return c, b


class Builder:
    def __init__(self):
        self.nc = nc = bass.Bass("TRN2", target_bir_lowering=False)
        self.es = ExitStack()
        self.P = Prog(nc, self.es)
        self.A = Arena(nc)
        self.w = {}
        self.x_in = nc.dram_tensor("x", [T, D], F32, kind="ExternalInput").ap()
        self.out = nc.dram_tensor("out", [T, D], F32, kind="ExternalOutput").ap()
        self.consts_d = nc.dram_tensor("consts", [128, NCONST], F32, kind="ExternalInput").ap()
        self.constsb_d = nc.dram_tensor("constsb", [128, NCONST_BF], F32, kind="ExternalInput").ap()
        self.dbg = None
        self.dbgb = self.P.buf("dbg", True)
        self.xres = nc.dram_tensor("xres", [NCH, 128, T], F32, kind="Internal").ap()
        self.vfirst = nc.dram_tensor("vfirst", [8, 128, T], F32, kind="Internal").ap()
        self.vfirstb = self.P.buf("vfirst", True)
        self.rkv = nc.dram_tensor("rkv", [24, 128, T], F32, kind="Internal").ap()
        self.rkvb = self.P.buf("rkv", True)
        self.xb = [self.P.buf("xres0", True), self.P.buf("xres1", True)]
        self.outb = self.P.buf("outb", True)
        self.ps = nc.alloc_psum_tensor("ps", [128, 4096], F32)
        self.pb = self.P.bufs("psb", 8, True)
        self.vecs = {}
        self.vlay = {}

    def bank(self, b, n=512):
        return self.ps[:, b * 512:b * 512 + n]

    def next_bank(self, lo=0, hi=8):
        k = (lo, hi)
        if not hasattr(self, "_bks"):
            self._bks = {}
        v = self._bks.get(k, lo - 1) + 1
        if v >= hi:
            v = lo
        self._bks[k] = v
        return v

    def dump(self, ap, col0, ncols, reads):
        if self.dbg is None:
            self.dbg = self.nc.dram_tensor("dbg", [128, DBG_N], F32, kind="ExternalOutput").ap()
        self.dma("pool", self.dbg[:, col0:col0 + ncols], ap, reads, partial=[self.dbgb],
                 owner=self.dbgb)

    def dram_in(self, name, shape):
        self.w[name] = self.nc.dram_tensor(name, list(shape), F32, kind="ExternalInput").ap()
        return self.w[name]

    def dma(self, q, out, in_, reads, writes=(), partial=(), owner=None):
        self.P.op(q, lambda e: e.dma_start(out=out, in_=in_), reads=reads, writes=writes,
                  partial=partial, dma_owner=owner)

    def mm(self, out, lhsT, rhs, start, stop, reads, wbuf):
        self.P.op("pe", lambda e: e.matmul(out, lhsT, rhs, start=start, stop=stop),
                  reads=reads, writes=[wbuf])

    def tr(self, out, in_, ident, reads, wbuf):
        self.P.op("pe", lambda e: e.transpose(out, in_, ident), reads=reads, writes=[wbuf])

    def act(self, out, in_, func, reads, writes=(), partial=(), scale=1.0, bias=None):
        if bias is None:
            self.P.op("act", lambda e: e.activation(out=out, in_=in_, func=func, scale=scale),
                      reads=reads, writes=writes, partial=partial)
        else:
            self.P.op("act", lambda e: e.activation(out=out, in_=in_, func=func, scale=scale,
                                                    bias=bias),
                      reads=reads, writes=writes, partial=partial)

    def ts(self, eng, out, in0, s1, s2, op0, op1, reads, writes=(), partial=()):
        if s2 is None:
            self.P.op(eng, lambda e: e.tensor_scalar(out=out, in0=in0, scalar1=s1, scalar2=None,
                                                     op0=op0),
                      reads=reads, writes=writes, partial=partial)
        else:
            self.P.op(eng, lambda e: e.tensor_scalar(out=out, in0=in0, scalar1=s1, scalar2=s2,
                                                     op0=op0, op1=op1),
                      reads=reads, writes=writes, partial=partial)

    def tt(self, eng, out, in0, in1, op, reads, writes=(), partial=()):
        self.P.op(eng, lambda e: e.tensor_tensor(out=out, in0=in0, in1=in1, op=op),
                  reads=reads, writes=writes, partial=partial)

    def stt(self, out, in0, scalar, in1, op0, op1, reads, writes=(), partial=()):
        self.P.op("dve", lambda e: e.scalar_tensor_tensor(out=out, in0=in0, scalar=scalar, in1=in1,
                                                          op0=op0, op1=op1),
                  reads=reads, writes=writes, partial=partial)

    def copy(self, eng, out, in_, reads, writes=(), partial=()):
        if eng == "act":
            self.P.op("act", lambda e: e.copy(out=out, in_=in_), reads=reads, writes=writes,
                      partial=partial)
        else:
            self.P.op(eng, lambda e: e.tensor_copy(out=out, in_=in_), reads=reads, writes=writes,
                      partial=partial)

    def load_consts(self):
        A, P = self.A, self.P
        self.cf = A.alloc([128, NCONST], F32)
        self.cfb = P.buf("cf", True)
        self.dma("sp", self.cf[:, :], self.consts_d, [], [self.cfb], owner=self.cfb)
        self.cb = A.alloc([128, 384], BF16)
        self.cbb = P.buf("cb", True)
        self.dma("pool", self.cb[:, :], self.constsb_d[:, 0:384], [], [self.cbb], owner=self.cbb)
        self.ident_f = self.cf[:, C_IDENT:C_IDENT + 128]
        self.ident_b = self.cb[:, B_IDENT:B_IDENT + 128]
        self.ones_b = self.cb[:, B_ONES:B_ONES + 128]

    def load_vecs(self, li):
        lay, n = layer_vec_layout(li)
        d = self.nc.dram_tensor("vecs%d" % li, [128, n], F32, kind="ExternalInput").ap()
        t = self.A.alloc([128, n], F32)
        b = self.P.buf("vecs%d" % li, True)
        self.dma("sp", t[:, :], d, [], [b], owner=b)
        self.vecs[li] = (t, b)
        self.vlay[li] = lay

    def vcol(self, li, name, c=0, n=1):
        t, b = self.vecs[li]
        c0, w = self.vlay[li][name]
        return t[:, c0 + c:c0 + c + n]

    def prologue(self):
        A, P = self.A, self.P
        m = A.mark()
        xin = [A.alloc([128, D], F32) for _ in range(2)]
        xinb = P.bufs("xin", 2)
        xo = [A.alloc([128, NCH, 128], F32) for _ in range(2)]
        xob = P.bufs("xo", 2)
        xresT = self.xres.rearrange("c p t -> p c t")
        for tt in range(NTT):
            s = tt % 2
            self.dma("sp", xin[s][:, :], self.x_in[tt * 128:(tt + 1) * 128, :], [], [xinb[s]],
                     owner=xinb[s])
            for g in range(4):
                bk = self.next_bank()
                for q in range(4):
                    dc = g * 4 + q
                    self.tr(self.bank(bk)[:, q * 128:(q + 1) * 128],
                            xin[s][:, dc * 128:(dc + 1) * 128], self.ident_f,
                            [xinb[s], self.cfb], self.pb[bk])
                eng = "act" if g % 2 == 0 else "dve"
                self.copy(eng, xo[s][:, g * 4:(g + 1) * 4, :],
                          self.bank(bk).rearrange("p (q t) -> p q t", q=4),
                          [self.pb[bk]], partial=[xob[s]])
            self.dma("sp", xresT[:, :, tt * 128:(tt + 1) * 128], xo[s][:, :, :], [xob[s]],
                     partial=[self.xb[tt // 8]], owner=self.xb[tt // 8])
        A.reset(m)

    def epilogue(self):
        A, P = self.A, self.P
        m = A.mark()
        xi = [A.alloc([128, NCH, 128], F32) for _ in range(2)]
        xib = P.bufs("exi", 2)
        xo = [A.alloc([128, D], F32) for _ in range(2)]
        xob = P.bufs("exo", 2)
        xresT = self.xres.rearrange("c p t -> p c t")
        for tt in range(NTT):
            s = tt % 2
            self.dma("sp", xi[s][:, :, :], xresT[:, :, tt * 128:(tt + 1) * 128], [self.xb[tt // 8]],
                     [xib[s]], owner=xib[s])
            for g in range(4):
                bk = self.next_bank()
                for q in range(4):
                    dc = g * 4 + q
                    self.tr(self.bank(bk)[:, q * 128:(q + 1) * 128], xi[s][:, dc, :], self.ident_f,
                            [xib[s], self.cfb], self.pb[bk])
                eng = "act" if g % 2 == 0 else "dve"
                self.copy(eng, xo[s][:, g * 512:(g + 1) * 512], self.bank(bk), [self.pb[bk]],
                          partial=[xob[s]])
            self.dma("sp", self.out[tt * 128:(tt + 1) * 128, :], xo[s][:, :], [xob[s]],
                     partial=[self.outb], owner=self.outb)
        A.reset(m)

    def rmsnorm(self, li, gname, xn, xnb, t0, n, tag):
        A, P = self.A, self.P
        m = A.mark()
        halves = sorted(set([t0 // 1024, (t0 + n - 1) // 1024]))
        rb = [self.xb[h] for h in halves]
        W = 512
        xc = [A.alloc([128, W], F32) for _ in range(3)]
        xcb = P.bufs("xc" + tag, 3)
        sq = [A.alloc([128, W], BF16) for _ in range(2)]
        sqb = P.bufs("sq" + tag, 2)
        rstd = A.alloc([128, n], F32)
        rsb = P.buf("rstd" + tag)
        nb = n // W
        k = 0
        q = 0
        for b in range(nb):
            bk = self.next_bank()
            for c in range(NCH):
                s = k % 3
                k += 1
                self.dma("sp", xc[s][:, :], self.xres[c, :, t0 + b * W:t0 + (b + 1) * W], rb,
                         [xcb[s]], owner=xcb[s])
                q = (q + 1) % 2
                self.act(sq[q][:, :], xc[s][:, :], AF.Square, [xcb[s]], [sqb[q]])
                self.mm(self.bank(bk), self.ones_b, sq[q][:, :], c == 0, c == NCH - 1,
                        [sqb[q], self.cbb], self.pb[bk])
            self.ts("dve", rstd[:, b * W:(b + 1) * W], self.bank(bk), 1.0 / D, RMS_EPS,
                    ALU.mult, ALU.add, [self.pb[bk]], partial=[rsb])
        self.act(rstd[:, :], rstd[:, :], AF.Sqrt, [rsb], [rsb])
        self.P.op("dve", lambda e: e.reciprocal(out=rstd[:, :], in_=rstd[:, :]), reads=[rsb],
                  writes=[rsb])
        vb = self.vecs[li][1]
        for b in range(nb):
            for c in range(NCH):
                s = k % 3
                k += 1
                self.dma("sp", xc[s][:, :], self.xres[c, :, t0 + b * W:t0 + (b + 1) * W], rb,
                         [xcb[s]], owner=xcb[s])
                self.stt(xn[:, c, b * W:(b + 1) * W], xc[s][:, :], self.vcol(li, gname, c),
                         rstd[:, b * W:(b + 1) * W], ALU.mult, ALU.mult, [xcb[s], rsb, vb],
                         partial=[xnb])
        A.reset(m)

    def ffn(self, li):
        A, P = self.A, self.P
        m = A.mark()
        p = "l%d_" % li
        w_up = self.w[p + "ffn_up"]
        w_dn = self.w[p + "ffn_down"].rearrange("(j q) n -> q j n", q=128)
        n = 1024
        W = 512
        xn = A.alloc([128, NCH, n], BF16)
        xnb = P.buf("fxn")
        aT = A.alloc([128, NJ, n], BF16)
        aTb = P.buf("faT")
        wup = [A.alloc([128, 2, NCH, 128], BF16) for _ in range(2)]
        wupb = P.bufs("fwup", 2)
        hh = [[A.alloc([128, W + 2], F32) for _ in range(2)] for _ in range(2)]
        hhb = [P.bufs("fh%d_" % gv, 2) for gv in range(2)]
        cg = A.alloc([128, W], F32)
        cv = A.alloc([128, W], F32)
        cgb, cvb = P.buf("fcg"), P.buf("fcv")
        halo = A.alloc([128, 2 * NJ, 2], F32)
        halob = P.buf("fhalo")
        wdn = [A.alloc([128, NJ, 128], BF16) for _ in range(2)]
        wdnb = P.bufs("fwdn", 2)
        xr = [A.alloc([128, W], F32) for _ in range(2)]
        xrb = P.bufs("fxr", 2)
        vb = self.vecs[li][1]
        self.P.op("dve", lambda e: e.memset(halo[:, :, :], 0.0), reads=[], writes=[halob])
        wk = 0
        hk = 0
        xk = 0
        for th in range(2):
            t0 = th * n
            self.rmsnorm(li, "norm_ffn", xn, xnb, t0, n, "f")
            for j in range(NJ):
                s = wk % 2
                wk += 1
                for gv in range(2):
                    c0 = gv * DFF + j * 128
                    src = w_up[:, c0:c0 + 128].rearrange("(c q) n -> q c n", q=128)
                    if gv == 0:
                        self.dma("pool", wup[s][:, gv, :, :], src, [], writes=[wupb[s]], owner=wupb[s])
                    else:
                        self.dma("pool", wup[s][:, gv, :, :], src, [], partial=[wupb[s]], owner=wupb[s])
                for tb in range(n // W):
                    hs = hk % 2
                    hk += 1
                    for gv in range(2):
                        bk = self.next_bank()
                        for c in range(NCH):
                            self.mm(self.bank(bk), wup[s][:, gv, c, :], xn[:, c, tb * W:(tb + 1) * W],
                                    c == 0, c == NCH - 1, [wupb[s], xnb], self.pb[bk])
                        h = hh[gv][hs]
                        hB = hhb[gv][hs]
                        col = gv * NJ + j
                        self.copy("act", h[:, 0:2], halo[:, col, :], [halob], writes=[hB])
                        self.copy("act", h[:, 2:W + 2], self.bank(bk), [self.pb[bk]], partial=[hB])
                        self.copy("act", halo[:, col, :], h[:, W:W + 2], [hB], partial=[halob])
                        cdst, cB = (cg, cgb) if gv == 0 else (cv, cvb)
                        self.ts("dve", cdst[:, :], h[:, 0:W], self.vcol(li, "ffn_conv", 0 * 88 + col),
                                None, ALU.mult, None, [hB, vb], writes=[cB])
                        self.stt(cdst[:, :], h[:, 1:W + 1], self.vcol(li, "ffn_conv", 1 * 88 + col),
                                 cdst[:, :], ALU.mult, ALU.add, [hB, vb, cB], writes=[cB])
                        self.stt(cdst[:, :], h[:, 2:W + 2], self.vcol(li, "ffn_conv", 2 * 88 + col),
                                 cdst[:, :], ALU.mult, ALU.add, [hB, vb, cB], writes=[cB])
                    self.act(cg[:, :], cg[:, :], AF.Silu, [cgb], writes=[cgb])
                    self.tt("dve", aT[:, j, tb * W:(tb + 1) * W], cg[:, :], cv[:, :], ALU.mult,
                            [cgb, cvb], partial=[aTb])
            for mch in range(NCH):
                s = mch % 2
                self.dma("pool", wdn[s][:, :, :], w_dn[:, :, mch * 128:(mch + 1) * 128], [],
                         writes=[wdnb[s]], owner=wdnb[s])
                for tb in range(n // W):
                    xs = xk % 2
                    xk += 1
                    tsl = slice(t0 + tb * W, t0 + (tb + 1) * W)
                    self.dma("sp", xr[xs][:, :], self.xres[mch, :, tsl], [self.xb[th]],
                             writes=[xrb[xs]], owner=xrb[xs])
                    bk = self.next_bank()
                    for j in range(NJ):
                        self.mm(self.bank(bk), wdn[s][:, j, :], aT[:, j, tb * W:(tb + 1) * W],
                                j == 0, j == NJ - 1, [wdnb[s], aTb], self.pb[bk])
                    self.tt("dve", xr[xs][:, :], self.bank(bk), xr[xs][:, :], ALU.add,
                            [self.pb[bk], xrb[xs]], writes=[xrb[xs]])
                    self.dma("sp", self.xres[mch, :, tsl], xr[xs][:, :], [xrb[xs]],
                             partial=[self.xb[th]], owner=self.xb[th])
        A.reset(m)


    def out_proj(self, w_ap, yT, yTb, nk, tag):
        A, P = self.A, self.P
        W = 512
        w_r = w_ap.rearrange("(j q) n -> q j n", q=128)
        wo = [A.alloc([128, nk, 128], BF16) for _ in range(2)]
        wob = P.bufs("wo" + tag, 2)
        xr = [A.alloc([128, W], F32) for _ in range(3)]
        xrb = P.bufs("xr" + tag, 3)
        xk = 0
        for mch in range(NCH):
            s = mch % 2
            self.dma("pool", wo[s][:, :, :], w_r[:, :, mch * 128:(mch + 1) * 128], [],
                     writes=[wob[s]], owner=wob[s])
            for tb in range(T // W):
                xs = xk % 3
                xk += 1
                th = (tb * W) // 1024
                tsl = slice(tb * W, (tb + 1) * W)
                self.dma("sp", xr[xs][:, :], self.xres[mch, :, tsl], [self.xb[th]],
                         writes=[xrb[xs]], owner=xrb[xs])
                bk = self.next_bank()
                for j in range(nk):
                    self.mm(self.bank(bk), wo[s][:, j, :], yT[:, j, tsl], j == 0, j == nk - 1,
                            [wob[s], yTb], self.pb[bk])
                self.tt("dve", xr[xs][:, :], self.bank(bk), xr[xs][:, :], ALU.add,
                        [self.pb[bk], xrb[xs]], writes=[xrb[xs]])
                self.dma("sp", self.xres[mch, :, tsl], xr[xs][:, :], [xrb[xs]],
                         partial=[self.xb[th]], owner=self.xb[th])

    def sconv(self, li):
        A, P = self.A, self.P
        m = A.mark()
        p = "l%d_" % li
        w_in = self.w[p + "conv_in"]
        W = 512
        xn = A.alloc([128, NCH, T], BF16)
        xnb = P.buf("sxn")
        zT = A.alloc([128, NCH, T], BF16)
        zTb = P.buf("szT")
        m2 = A.mark()
        wci = [A.alloc([128, 3, NCH, 128], BF16) for _ in range(3)]
        wcib = P.bufs("swci", 3)
        usb = [A.alloc([128, W], F32) for _ in range(2)]
        usbb = P.bufs("susb", 2)
        cu = [A.alloc([128, W + 2], F32) for _ in range(2)]
        cub = P.bufs("scu", 2)
        cc = [A.alloc([128, W], F32) for _ in range(2)]
        ccb = P.bufs("scc", 2)
        self.rmsnorm(li, "norm_mix", xn, xnb, 0, T, "s")
        vb = self.vecs[li][1]
        k = 0
        for j in range(NCH):
            s = j % 3
            for g in range(3):
                c0 = g * D + j * 128
                src = w_in[:, c0:c0 + 128].rearrange("(c q) n -> q c n", q=128)
                if g == 0:
                    self.dma("pool", wci[s][:, g, :, :], src, [], writes=[wcib[s]], owner=wcib[s])
                else:
                    self.dma("pool", wci[s][:, g, :, :], src, [], partial=[wcib[s]], owner=wcib[s])
            for tb in range(T // W):
                q = k % 2
                k += 1
                tsl = slice(tb * W, (tb + 1) * W)
                bks = []
                for g in range(3):
                    bk = self.next_bank()
                    bks.append(bk)
                    for c in range(NCH):
                        self.mm(self.bank(bk), wci[s][:, g, c, :], xn[:, c, tsl], c == 0, c == NCH - 1,
                                [wcib[s], xnb], self.pb[bk])
                self.copy("act", usb[q][:, :], self.bank(bks[2]), [self.pb[bks[2]]], writes=[usbb[q]])
                if tb == 0:
                    self.P.op("dve", (lambda t: (lambda e: e.memset(t, 0.0)))(cu[q][:, 0:2]), reads=[],
                              writes=[cub[q]])
                else:
                    self.copy("act", cu[q][:, 0:2], cu[1 - q][:, W:W + 2], [cub[1 - q]], writes=[cub[q]])
                self.tt("dve", cu[q][:, 2:W + 2], self.bank(bks[1]), usb[q][:, :], ALU.mult,
                        [self.pb[bks[1]], usbb[q]], partial=[cub[q]])
                for tap in range(3):
                    wcol = self.vcol(li, "conv_w", tap * 16 + j)
                    if tap == 0:
                        self.ts("dve", cc[q][:, :], cu[q][:, 0:W], wcol, None, ALU.mult, None,
                                [cub[q], vb], writes=[ccb[q]])
                    else:
                        self.stt(cc[q][:, :], cu[q][:, tap:W + tap], wcol, cc[q][:, :], ALU.mult, ALU.add,
                                 [cub[q], vb, ccb[q]], writes=[ccb[q]])
                self.tt("dve", zT[:, j, tsl], self.bank(bks[0]), cc[q][:, :], ALU.mult,
                        [self.pb[bks[0]], ccb[q]], partial=[zTb])
        A.reset(m2)
        P.fence()
        self.out_proj(self.w[p + "conv_out"], zT, zTb, NCH, "s")
        A.reset(m)

    def attention(self, li, xn, xnb, yT, yTb):
        A, P = self.A, self.P
        p = "l%d_" % li
        w_in = self.w[p + "w_in"]
        W = 512
        vb = self.vecs[li][1]
        w3 = [A.alloc([128, 3, NCH, 128], BF16) for _ in range(2)]
        w3b = P.bufs("aw3", 2)
        qT = A.alloc([128, T], BF16)
        kT = A.alloc([128, T], BF16)
        V = A.alloc([128, T // 128, 128], BF16)
        qTb, kTb, Vb = P.buf("aqT"), P.buf("akT"), P.buf("aV")
        sqt = [A.alloc([128, W], BF16) for _ in range(2)]
        sqtb = P.bufs("asq", 2)
        rqt = [A.alloc([128, W], F32) for _ in range(2)]
        rqtb = P.bufs("arq", 2)
        gqs = A.alloc([128, 1], F32)
        gqsb = P.buf("agqs")
        km = A.alloc([128, 8], F32)
        kmb16 = A.alloc([128, 8], BF16)
        kmb = P.buf("akm")
        gm = A.alloc([128, 64], F32)
        gmb = P.buf("agm")
        top8 = A.alloc([128, 8], F32)
        top8b = P.buf("atop8")
        selb = A.alloc([128, 64], F32)
        selbb = P.buf("aselb")
        selT = A.alloc([8, 1024], BF16)
        selTb = P.buf("aselT")
        pT = [A.alloc([128, W], BF16) for _ in range(3)]
        pTb = P.bufs("apT", 3)
        rl = [A.alloc([128, 128], F32) for _ in range(2)]
        rlb = P.bufs("arl", 2)
        cba = A.alloc([8, NCONST_BF - 384], BF16)
        cbab = P.buf("acba")
        self.dma("pool", cba[:, :], self.constsb_d[0:8, 384:NCONST_BF], [], writes=[cbab], owner=cbab)
        self.ts("dve", gqs[:, :], self.vcol(li, "q_gain"), float(128 ** -0.5), None, ALU.mult, None,
                [vb], writes=[gqsb])
        cbb, cfb = self.cbb, self.cfb
        pk = 0
        for h in range(int(os.environ.get('NAH', A_HEADS))):
            s = h % 2
            for g, blk in enumerate((h, 8 + h, 16 + h)):
                srcw = w_in[:, blk * 128:(blk + 1) * 128].rearrange("(c q) n -> q c n", q=128)
                if g == 0:
                    self.dma("pool", w3[s][:, g, :, :], srcw, [], writes=[w3b[s]], owner=w3b[s])
                else:
                    self.dma("pool", w3[s][:, g, :, :], srcw, [], partial=[w3b[s]], owner=w3b[s])
            for tb in range(T // W):
                tsl = slice(tb * W, (tb + 1) * W)
                for g, (dst, dstb, gain) in enumerate(((qT, qTb, gqs[:, 0:1]),
                                                       (kT, kTb, self.vcol(li, "k_gain")))):
                    bk = self.next_bank()
                    for c in range(NCH):
                        self.mm(self.bank(bk), w3[s][:, g, c, :], xn[:, c, tsl], c == 0, c == NCH - 1,
                                [w3b[s], xnb], self.pb[bk])
                    z = pk % 2
                    pk += 1
                    self.act(sqt[z][:, :], self.bank(bk), AF.Square, [self.pb[bk]], writes=[sqtb[z]])
                    b2 = self.next_bank()
                    self.mm(self.bank(b2), self.ones_b, sqt[z][:, :], True, True, [sqtb[z], cbb],
                            self.pb[b2])
                    self.ts("dve", rqt[z][:, :], self.bank(b2), 1.0 / 128, RMS_EPS, ALU.mult, ALU.add,
                            [self.pb[b2]], writes=[rqtb[z]])
                    self.act(rqt[z][:, :], rqt[z][:, :], AF.Ln, [rqtb[z]], writes=[rqtb[z]])
                    self.act(rqt[z][:, :], rqt[z][:, :], AF.Exp, [rqtb[z]], writes=[rqtb[z]], scale=-0.5)
                    self.stt(dst[:, tsl], self.bank(bk), gain, rqt[z][:, :], ALU.mult, ALU.mult,
                             [self.pb[bk], rqtb[z], vb, gqsb], partial=[dstb])
            for g4 in range(4):
                bv = self.next_bank()
                for i in range(4):
                    tt = g4 * 4 + i
                    for c in range(NCH):
                        self.mm(self.bank(bv)[:, i * 128:(i + 1) * 128], xn[:, c, tt * 128:(tt + 1) * 128],
                                w3[s][:, 2, c, :], c == 0, c == NCH - 1, [w3b[s], xnb], self.pb[bv])
                self.copy("act", V[:, g4 * 4:(g4 + 1) * 4, :],
                          self.bank(bv).rearrange("p (i d) -> p i d", i=4), [self.pb[bv]], partial=[Vb])
            self.P.op("dve", lambda e: e.tensor_reduce(out=km[:, :],
                                                       in_=kT[:, :].rearrange("p (n s) -> p n s", n=8),
                                                       axis=AX.X, op=ALU.add),
                      reads=[kTb], writes=[kmb])
            self.copy("dve", kmb16[:, :], km[:, :], [kmb], writes=[kmb])
            bg = self.next_bank()
            for i in range(8):
                qt = 8 + i
                self.mm(self.bank(bg)[:, i * 8:(i + 1) * 8], qT[:, qt * 128:(qt + 1) * 128], kmb16[:, :],
                        True, True, [qTb, kmb], self.pb[bg])
            self.tt("dve", gm[:, :], self.bank(bg)[:, 0:64], self.cf[:, C_GMASK:C_GMASK + 64], ALU.add,
                    [self.pb[bg], cfb], writes=[gmb])
            for i in range(8):
                self.P.op("dve", (lambda o, a: (lambda e: e.max(out=o, in_=a)))(top8[:, :],
                                                                               gm[:, i * 8:(i + 1) * 8]),
                          reads=[gmb], writes=[top8b])
                self.ts("dve", selb[:, i * 8:(i + 1) * 8], gm[:, i * 8:(i + 1) * 8], top8[:, 2:3], -30000.0,
                        ALU.is_lt, ALU.mult, [gmb, top8b], partial=[selbb])
            for g2 in range(2):
                bt = self.next_bank()
                for i in range(4):
                    ii = g2 * 4 + i
                    self.tr(self.bank(bt)[0:8, i * 128:(i + 1) * 128], selb[:, ii * 8:(ii + 1) * 8],
                            self.ident_f, [selbb, cfb], self.pb[bt])
                self.copy("act", selT[0:8, g2 * 512:(g2 + 1) * 512], self.bank(bt)[0:8, :], [self.pb[bt]],
                          partial=[selTb])
            pending = []
            for qt in range(T // 128):
                qb = qt // 2
                bo = 4 + qt % 2
                bl = 6 + qt % 2
                qsl = slice(qt * 128, (qt + 1) * 128)
                for g0 in range(0, qt + 1, 4):
                    grp = list(range(g0, min(g0 + 4, qt + 1)))
                    bs = self.next_bank(0, 4)
                    for i, kt in enumerate(grp):
                        o = self.bank(bs)[:, i * 128:(i + 1) * 128]
                        extras = [(cba[0:1, B_DS - 384 + h * 128:B_DS - 384 + (h + 1) * 128],
                                   cba[0:1, B_DL - 384 + (qt - kt) * 128:B_DL - 384 + (qt - kt + 1) * 128], [cbab])]
                        if qb >= 4 and kt // 2 < qb:
                            kb = kt // 2
                            extras.append((cba[0:8, B_ONEHOT - 384 + kb * 128:B_ONEHOT - 384 + (kb + 1) * 128],
                                           selT[0:8, (qt - 8) * 128:(qt - 7) * 128], [cbab, selTb]))
                        if kt == qt:
                            extras.append((self.ident_b, self.cb[:, B_CAUSAL:B_CAUSAL + 128], [cbb]))
                        self.mm(o, kT[:, kt * 128:(kt + 1) * 128], qT[:, qsl], True, False, [kTb, qTb],
                                self.pb[bs])
                        for ei, (l_, r_, rd) in enumerate(extras):
                            self.mm(o, l_, r_, False, ei == len(extras) - 1, rd, self.pb[bs])
                    z = pk % 3
                    pk += 1
                    n_ = len(grp) * 128
                    self.act(pT[z][:, 0:n_], self.bank(bs)[:, 0:n_], AF.Exp, [self.pb[bs], cfb],
                             writes=[pTb[z]], bias=self.cf[:, C_BIASCOL + h:C_BIASCOL + h + 1])
                    for fn_ in pending:
                        fn_()
                    pending = []

                    def pv(grp=grp, z=z, bo=bo, bl=bl, qt=qt):
                        for i, kt in enumerate(grp):
                            self.mm(self.bank(bo)[:, 0:128], V[:, kt, :], pT[z][:, i * 128:(i + 1) * 128],
                                    kt == 0, kt == qt, [Vb, pTb[z]], self.pb[bo])
                            self.mm(self.bank(bl)[:, 0:128], self.ones_b, pT[z][:, i * 128:(i + 1) * 128],
                                    kt == 0, kt == qt, [cbb, pTb[z]], self.pb[bl])
                    pending.append(pv)

                def norm(qt=qt, bo=bo, bl=bl, qsl=qsl, h=h):
                    z2 = qt % 2
                    self.P.op("dve", (lambda o, a: (lambda e: e.reciprocal(out=o, in_=a)))(
                        rl[z2][:, :], self.bank(bl)[:, 0:128]), reads=[self.pb[bl]], writes=[rlb[z2]])
                    self.tt("dve", yT[:, h, qsl], self.bank(bo)[:, 0:128], rl[z2][:, :], ALU.mult,
                            [self.pb[bo], rlb[z2]], partial=[yTb])
                pending.append(norm)
            for fn_ in pending:
                fn_()

    def rwkv(self, li, xn, xnb, yT, yTb, m_xn, xn_bytes):
        A, P = self.A, self.P
        p = "l%d_" % li
        first = li == 0
        w_in = self.w[p + "w_in"]
        RW0 = 3072
        n = 256
        NC = 4
        vb = self.vecs[li][1]
        cfb, cbb = self.cfb, self.cbb
        bones = self.cf[:, C_BONES:C_BONES + 128]
        c0 = float(np.exp(-0.5))

        lw = A.alloc([128, 1024], BF16)
        lg0 = A.alloc([128, 1024], BF16)
        lg1 = A.alloc([64, 1024], BF16)
        lwb = P.buf("rlw")
        self.dma("pool", lw[0:64, :], self.w[p + "w_lora"], [], writes=[lwb], owner=lwb)
        self.dma("pool", lw[64:128, :], self.w[p + "a_lora"], [], partial=[lwb], owner=lwb)
        self.dma("pool", lg0[:, :], self.w[p + "g_lora"][0:128, :], [], partial=[lwb], owner=lwb)
        self.dma("pool", lg1[0:32, :], self.w[p + "g_lora"][128:160, :], [], partial=[lwb], owner=lwb)
        if not first:
            self.dma("pool", lg1[32:64, :], self.w[p + "v_lora"], [], partial=[lwb], owner=lwb)
        zwa = A.alloc([128, T], BF16)
        sg0 = A.alloc([128, T], BF16)
        sgv = A.alloc([64, T], BF16)
        zb = P.buf("rz")
        mz = A.mark()
        wz = [A.alloc([128, NCH, 128], BF16) for _ in range(2)]
        wzb = P.bufs("rwz", 2)
        zraw = [A.alloc([128, 513], F32) for _ in range(2)]
        zrawb = P.bufs("rzraw", 2)
        zd = [A.alloc([128, 512], F32) for _ in range(2)]
        zdb = P.bufs("rzd", 2)
        zk = 0
        nz2 = 32 if first else 64
        items = [("z", zi, 3072 + zi * 128, (128, 128, nz2)[zi], 24 + zi) for zi in range(3)]
        items += [("p", b, b * 128, 128, b) for b in range(24)]
        for ii, (kind, zi, coff, ncol, mui) in enumerate(items):
            col0 = RW0 + coff
            ws = ii % 2
            self.dma("pool", wz[ws][:, :, 0:ncol],
                     w_in[:, col0:col0 + ncol].rearrange("(c q) n -> q c n", q=128),
                     [], writes=[wzb[ws]], owner=wzb[ws])
            mu = self.vcol(li, "shift_mu", mui)
            for tb in range(4):
                q = zk % 2
                zk += 1
                tsl = slice(tb * 512, (tb + 1) * 512)
                bk = self.next_bank()
                for c in range(NCH):
                    self.mm(self.bank(bk)[0:ncol, :], wz[ws][:, c, 0:ncol], xn[:, c, tsl], c == 0, c == NCH - 1,
                            [wzb[ws], xnb], self.pb[bk])
                if tb == 0:
                    self.P.op("dve", (lambda t: (lambda e: e.memset(t, 0.0)))(zraw[q][0:ncol, 0:1]), reads=[],
                              partial=[zrawb[q]])
                else:
                    self.copy("act", zraw[q][0:ncol, 0:1], zraw[1 - q][0:ncol, 512:513], [zrawb[1 - q]],
                              partial=[zrawb[q]])
                self.copy("act", zraw[q][0:ncol, 1:513], self.bank(bk)[0:ncol, :], [self.pb[bk]],
                          writes=[zrawb[q]])
                self.tt("dve", zd[q][0:ncol, :], zraw[q][0:ncol, 0:512], zraw[q][0:ncol, 1:513], ALU.subtract,
                        [zrawb[q]], writes=[zdb[q]])
                self.stt(zd[q][0:ncol, :], zd[q][0:ncol, :], mu[0:ncol, :], zraw[q][0:ncol, 1:513], ALU.mult,
                         ALU.add, [zdb[q], zrawb[q], vb], writes=[zdb[q]])
                if kind == "p":
                    self.dma("sp", self.rkv[zi, :, tsl], zd[q][:, :], [zdb[q]], partial=[self.rkvb],
                             owner=self.rkvb)
                    if first and zi >= 16:
                        self.dma("sp", self.vfirst[zi - 16, :, tsl], zd[q][:, :], [zdb[q]],
                                 partial=[self.vfirstb], owner=self.vfirstb)
                elif zi == 0:
                    self.act(zwa[0:64, tsl], zd[q][0:64, :], AF.Tanh, [zdb[q]], partial=[zb])
                    self.copy("act", zwa[64:128, tsl], zd[q][64:128, :], [zdb[q]], partial=[zb])
                elif zi == 1:
                    self.act(sg0[:, tsl], zd[q][:, :], AF.Sigmoid, [zdb[q]], partial=[zb])
                else:
                    self.act(sgv[0:32, tsl], zd[q][0:32, :], AF.Sigmoid, [zdb[q]], partial=[zb])
                    if not first:
                        self.copy("act", sgv[32:64, tsl], zd[q][32:64, :], [zdb[q]], partial=[zb])
        def stream(Ax, hps):
            def f32(shape=(128, n)):
                return Ax.alloc(list(shape), F32)
            rr, kx, vv = f32(), f32(), f32()
            rrb, kxb, vvb = P.buf("rr"), P.buf("rk"), P.buf("rv")
            dtmp = f32()
            dtmpb = P.buf("rdt")
            sw, asig, gg, vs = f32(), f32(), f32(), f32()
            swb, asigb, ggb, vsb = P.buf("rsw"), P.buf("rasig"), P.buf("rgg"), P.buf("rvs")
            vf = f32()
            vfb_ = P.buf("rvf")
            kk, sq, rn = f32(), f32(), f32()
            kkb, sqb, rnb = P.buf("rkk"), P.buf("rsq"), P.buf("rrn")
            k2, bvec = f32(), f32()
            k2b, bvecb = P.buf("rk2"), P.buf("rbvec")
            Lp, Lx = f32(), f32()
            Lpb, Lxb = P.buf("rLp"), P.buf("rLx")
            Win, Wout, Wex, Wend = f32(), f32(), f32(), f32()
            Winb, Woutb, Wexb, Wendb = P.buf("rWin"), P.buf("rWout"), P.buf("rWex"), P.buf("rWend")
            AR = Ax.alloc([128, NC, 2, 64], F32)
            BK = Ax.alloc([128, NC, 2, 64], F32)
            ARb, BKb = P.buf("rAR"), P.buf("rBK")
            khat, bhat = Wex, Wout
            khatb, bhatb = Wexb, Woutb
            rk, bon = sq, f32()
            rkb, bonb = sqb, P.buf("rbon")
            Vtm = Ax.alloc([64, NC, 128], F32)
            Ktm = Ax.alloc([64, NC, 128], F32)
            Btm = Ax.alloc([64, NC, 128], F32)
            Vtmb, Ktmb, Btmb = P.buf("rVtm"), P.buf("rKtm"), P.buf("rBtm")
            Ak = Ax.alloc([64, 8, 128], F32)
            Ab = Ax.alloc([64, 8, 128], F32)
            NT = Ax.alloc([64, 8, 64], BF16)
            P0b = Ax.alloc([64, 8, 64], BF16)
            Akb, Abb, NTb, P0bb = P.buf("rAk"), P.buf("rAb"), P.buf("rNT"), P.buf("rP0b")
            Pw = [Ax.alloc([64, 8, 64], BF16) for _ in range(2)]
            PTw = [Ax.alloc([64, 8, 64], BF16) for _ in range(2)]
            Pwb, PTwb = P.bufs("rPw", 2), P.bufs("rPTw", 2)
            Tm = Ax.alloc([64, 8, 64], BF16)
            Tmb = P.buf("rTm")
            TmF = Ax.alloc([64, 8, 64], F32)
            TmFb = P.buf("rTmF")
            Xs = Ax.alloc([64, 128], F32)
            Us = Ax.alloc([64, 128], F32)
            Xsb, Usb = P.buf("rXs"), P.buf("rUs")
            S = Ax.alloc([128, 64], F32)
            Sb = P.buf("rS")
            yraw, yc, ysq, yrs = Lp, Lx, sq, rn
            yrawb, ycb, ysqb, yrsb = Lpb, Lxb, sqb, rnb
            ones64 = self.cf[:, C_ONE64:C_ONE64 + 64]
            amask = self.cf[0:64, C_AMASK:C_AMASK + 512]
            ntmask = self.cf[0:64, C_NTMASK:C_NTMASK + 512]
            id8 = self.cf[0:64, C_ID8:C_ID8 + 512]
            pk = 0
            for hp in hps:
                cols = slice(hp * 128, (hp + 1) * 128)
                self.P.op("dve", lambda e: e.memset(S[:, :], 0.0), reads=[], writes=[Sb])
                for tb in range(T // n):
                    q = pk % 2
                    pk += 1
                    tsl = slice(tb * n, (tb + 1) * n)
                    for g, (dst, dstb) in enumerate(((rr, rrb), (kx, kxb), (vv, vvb))):
                        self.dma("sp", dst[:, :], self.rkv[g * 8 + hp, :, tsl], [self.rkvb], writes=[dstb],
                                 owner=dstb)
                    yield
                    if RSTOP == 1:
                        return
                    bw = self.next_bank()
                    self.mm(self.bank(bw)[:, 0:n], lw[0:64, cols], zwa[0:64, tsl], True, True, [lwb, zb],
                            self.pb[bw])
                    self.act(sw[:, :], self.bank(bw)[:, 0:n], AF.Sigmoid, [self.pb[bw], vb], writes=[swb],
                             bias=self.vcol(li, "w0", hp))
                    ba = self.next_bank()
                    self.mm(self.bank(ba)[:, 0:n], lw[64:128, cols], zwa[64:128, tsl], True, True, [lwb, zb],
                            self.pb[ba])
                    self.act(asig[:, :], self.bank(ba)[:, 0:n], AF.Sigmoid, [self.pb[ba], vb], writes=[asigb],
                             bias=self.vcol(li, "a0", hp))
                    bg = self.next_bank()
                    self.mm(self.bank(bg)[:, 0:n], lg0[:, cols], sg0[:, tsl], True, False, [lwb, zb], self.pb[bg])
                    self.mm(self.bank(bg)[:, 0:n], lg1[0:32, cols], sgv[0:32, tsl], False, True, [lwb, zb],
                            self.pb[bg])
                    self.copy("act", gg[:, :], self.bank(bg)[:, 0:n], [self.pb[bg]], writes=[ggb])
                    yield
                    if RSTOP == 2:
                        return
                    if not first:
                        bv = self.next_bank()
                        self.mm(self.bank(bv)[:, 0:n], lg1[32:64, cols], sgv[32:64, tsl], True, True, [lwb, zb],
                                self.pb[bv])
                        self.act(vs[:, :], self.bank(bv)[:, 0:n], AF.Sigmoid, [self.pb[bv], vb], writes=[vsb],
                                 bias=self.vcol(li, "v0", hp))
                        self.dma("sp", vf[:, :], self.vfirst[hp, :, tsl], [self.vfirstb], writes=[vfb_],
                                 owner=vfb_)
                        self.tt("dve", dtmp[:, :], vf[:, :], vv[:, :], ALU.subtract, [vfb_, vvb], writes=[dtmpb])
                        self.tt("dve", dtmp[:, :], dtmp[:, :], vs[:, :], ALU.mult, [dtmpb, vsb], writes=[dtmpb])
                        self.tt("dve", vv[:, :], vv[:, :], dtmp[:, :], ALU.add, [vvb, dtmpb], writes=[vvb])
                    yield
                    if RSTOP == 3:
                        return
                    self.ts("dve", kk[:, :], kx[:, :], self.vcol(li, "k_k", hp), None, ALU.mult, None, [kxb, vb],
                            writes=[kkb])
                    self.act(sq[:, :], kk[:, :], AF.Square, [kkb], writes=[sqb])
                    bn = self.next_bank()
                    self.mm(self.bank(bn)[:, 0:n], bones, sq[:, :], True, True, [cfb, sqb], self.pb[bn])
                    self.act(rn[:, :], self.bank(bn)[:, 0:n], AF.Sqrt, [self.pb[bn]], writes=[rnb])
                    self.ts("dve", rn[:, :], rn[:, :], 1e-12, None, ALU.max, None, [rnb], writes=[rnb])
                    self.P.op("dve", lambda e: e.reciprocal(out=rn[:, :], in_=rn[:, :]), reads=[rnb], writes=[rnb])
                    self.tt("dve", kk[:, :], kk[:, :], rn[:, :], ALU.mult, [kkb, rnb], writes=[kkb])
                    self.ts("dve", dtmp[:, :], asig[:, :], -1.0, self.vcol(li, "k_a", hp), ALU.add, ALU.mult,
                            [asigb, vb], writes=[dtmpb])
                    self.stt(k2[:, :], dtmp[:, :], 1.0, kx[:, :], ALU.add, ALU.mult, [dtmpb, kxb], writes=[k2b])
                    self.tt("dve", bvec[:, :], kk[:, :], asig[:, :], ALU.mult, [kkb, asigb], writes=[bvecb])
                    yield
                    if RSTOP == 4:
                        return
                    for c in range(NC):
                        cs = slice(c * 64, (c + 1) * 64)
                        self.P.op("dve", (lambda o, d1: (lambda e: e.tensor_tensor_scan(
                            out=o, data0=ones64, data1=d1, initial=0.0, op0=ALU.mult, op1=ALU.add)))(
                            Lp[:, cs], sw[:, cs]), reads=[swb, cfb], partial=[Lpb])
                    self.act(Win[:, :], Lp[:, :], AF.Exp, [Lpb], writes=[Winb], scale=-c0)
                    self.act(Wout[:, :], Lp[:, :], AF.Exp, [Lpb], writes=[Woutb], scale=c0)
                    self.tt("dve", Lx[:, :], Lp[:, :], sw[:, :], ALU.subtract, [Lpb, swb], writes=[Lxb])
                    self.act(Wex[:, :], Lx[:, :], AF.Exp, [Lxb], writes=[Wexb], scale=-c0)
                    for c in range(NC):
                        cs = slice(c * 64, (c + 1) * 64)
                        self.ts("dve", Lx[:, cs], Lp[:, cs], -1.0, Lp[:, c * 64 + 63:c * 64 + 64], ALU.mult,
                                ALU.add, [Lpb, Wexb], partial=[Lxb])
                    self.act(Wend[:, :], Lx[:, :], AF.Exp, [Lxb], writes=[Wendb], scale=-c0)
                    yield
                    if RSTOP == 5:
                        return

                    def v3(t):
                        return t[:, :].rearrange("p (c s) -> p c s", c=NC)
                    self.stt(AR[:, :, 0, :], v3(kk), -1.0, v3(Wex), ALU.mult, ALU.mult, [kkb, Wexb], partial=[ARb])
                    self.tt("dve", AR[:, :, 1, :], v3(rr), v3(Win), ALU.mult, [rrb, Winb], partial=[ARb])
                    self.tt("dve", BK[:, :, 0, :], v3(bvec), v3(Wout), ALU.mult, [bvecb, Woutb], partial=[BKb])
                    self.tt("dve", BK[:, :, 1, :], v3(k2), v3(Wout), ALU.mult, [k2b, Woutb], partial=[BKb])
                    self.tt("dve", khat[:, :], k2[:, :], Wend[:, :], ALU.mult, [k2b, Wendb], writes=[khatb])
                    self.tt("dve", bhat[:, :], bvec[:, :], Wend[:, :], ALU.mult, [bvecb, Wendb], writes=[bhatb])
                    self.stt(rk[:, :], rr[:, :], self.vcol(li, "r_k", hp), k2[:, :], ALU.mult, ALU.mult,
                             [rrb, k2b, vb], writes=[rkb])
                    bb_ = self.next_bank()
                    self.mm(self.bank(bb_)[:, 0:n], bones, rk[:, :], True, True, [cfb, rkb], self.pb[bb_])
                    self.tt("dve", bon[:, :], self.bank(bb_)[:, 0:n], vv[:, :], ALU.mult, [self.pb[bb_], vvb],
                            writes=[bonb])
                    yield
                    if RSTOP == 6:
                        return
                    for (src_, srcb, dst_, dstb_) in ((vv, vvb, Vtm, Vtmb), (khat, khatb, Ktm, Ktmb),
                                                      (bhat, bhatb, Btm, Btmb)):
                        bt = self.next_bank()
                        for c in range(NC):
                            self.tr(self.bank(bt)[0:64, c * 128:(c + 1) * 128], src_[:, c * 64:(c + 1) * 64],
                                    self.ident_f, [srcb, cfb], self.pb[bt])
                        self.copy("act", dst_[:, :, :], self.bank(bt)[0:64, :].rearrange("p (c f) -> p c f", c=NC),
                                  [self.pb[bt]], writes=[dstb_])
                    yield
                    if RSTOP == 7:
                        return
                    for (dst_, dstb_, which) in ((Ak, Akb, 1), (Ab, Abb, 0)):
                        for hh in range(2):
                            hs = slice(hh * 64, hh * 64 + 64)
                            bsx = self.next_bank()
                            for c in range(NC):
                                self.mm(self.bank(bsx)[0:64, c * 128:(c + 1) * 128], BK[hs, c, which, :],
                                        AR[hs, c, :, :].rearrange("p a s -> p (a s)"), True, True, [BKb, ARb],
                                        self.pb[bsx])
                            if R8SUB == 1 or (R8SUB == 3 and hh == 1):
                                return
                            self.tt("dve", dst_[:, hh * 4:(hh + 1) * 4, :],
                                    self.bank(bsx)[0:64, :].rearrange("p (u f) -> p u f", u=4),
                                    amask.rearrange("p (u f) -> p u f", u=4), ALU.mult, [self.pb[bsx], cfb],
                                    partial=[dstb_])
                            if R8SUB == 2:
                                return
                    for hh in range(2):
                        hs = slice(hh * 64, hh * 64 + 64)
                        bsx = self.next_bank()
                        for c in range(NC):
                            self.mm(self.bank(bsx)[0:64, c * 64:(c + 1) * 64], AR[hs, c, 0, :], BK[hs, c, 0, :], True,
                                    True, [BKb, ARb], self.pb[bsx])
                        self.tt("dve", NT[:, hh * 4:(hh + 1) * 4, :],
                                self.bank(bsx)[0:64, 0:256].rearrange("p (u f) -> p u f", u=4),
                                ntmask[:, 0:256].rearrange("p (u f) -> p u f", u=4), ALU.mult, [self.pb[bsx], cfb],
                                partial=[NTb])
                    yield
                    if RSTOP == 8:
                        return
                    self.tt("dve", Tm[:, :, :], Ab[:, :, 0:64], id8.rearrange("p (u f) -> p u f", u=8), ALU.add,
                            [Abb, cfb], writes=[Tmb])
                    self.copy("act", P0b[:, :, :], Ab[:, :, 0:64], [Abb], writes=[P0bb])
                    curP = lambda u: P0b[:, u, :]
                    curPT = lambda u: NT[:, u, :]
                    curPb, curPTb = P0bb, NTb
                    for lv in range(5):
                        yield
                        z = lv % 2
                        last = lv == 4
                        bpt = self.next_bank()
                        for u in range(8):
                            self.mm(self.bank(bpt)[0:64, u * 64:(u + 1) * 64], curP(u), curPT(u), True, True,
                                    [curPb, curPTb], self.pb[bpt])
                        if not last:
                            bp = self.next_bank()
                            for u in range(8):
                                self.mm(self.bank(bp)[0:64, u * 64:(u + 1) * 64], curPT(u), curP(u), True, True,
                                        [curPb, curPTb], self.pb[bp])
                        self.copy("act", PTw[z][:, :, :], self.bank(bpt)[0:64, :].rearrange("p (u f) -> p u f", u=8),
                                  [self.pb[bpt]], writes=[PTwb[z]])
                        if not last:
                            self.copy("act", Pw[z][:, :, :], self.bank(bp)[0:64, :].rearrange("p (u f) -> p u f", u=8),
                                      [self.pb[bp]], writes=[Pwb[z]])
                        bT = self.next_bank()
                        for u in range(8):
                            self.mm(self.bank(bT)[0:64, u * 64:(u + 1) * 64], PTw[z][:, u, :], Tm[:, u, :], True, True,
                                    [PTwb[z], Tmb], self.pb[bT])
                        if not last:
                            self.tt("dve", Tm[:, :, :], self.bank(bT)[0:64, :].rearrange("p (u f) -> p u f", u=8),
                                    Tm[:, :, :], ALU.add, [self.pb[bT], Tmb], writes=[Tmb])
                        else:
                            self.tt("dve", TmF[:, :, :], self.bank(bT)[0:64, :].rearrange("p (u f) -> p u f", u=8),
                                    Tm[:, :, :], ALU.add, [self.pb[bT], Tmb], writes=[TmFb])
                        curP = (lambda zz: (lambda u: Pw[zz][:, u, :]))(z)
                        curPT = (lambda zz: (lambda u: PTw[zz][:, u, :]))(z)
                        curPb, curPTb = Pwb[z], PTwb[z]
                    yield
                    if RSTOP == 9:
                        return
                    for c in range(NC):
                        yield
                        for hh in range(2):
                            u = hh * 4 + c
                            hs = slice(hh * 64, hh * 64 + 64)
                            fs = slice(hh * 64, hh * 64 + 64)
                            bX = self.next_bank()
                            o = self.bank(bX)[0:64, 0:64]
                            if hh == 0:
                                self.mm(o, AR[hs, c, 0, :], S[hs, :], True, False, [ARb, Sb], self.pb[bX])
                                self.mm(o, Ak[:, u, 0:64], Vtm[:, c, fs], False, True, [Akb, Vtmb], self.pb[bX])
                                self.copy("act", Xs[:, fs], o, [self.pb[bX]], partial=[Xsb])
                            else:
                                self.mm(o, AR[hs, c, 0, :], S[hs, :], True, True, [ARb, Sb], self.pb[bX])
                                bX2 = self.next_bank()
                                o2 = self.bank(bX2)[0:64, 0:64]
                                self.mm(o2, Ak[:, u, 0:64], Vtm[:, c, fs], True, True, [Akb, Vtmb], self.pb[bX2])
                                self.copy("act", Xs[:, fs], o, [self.pb[bX]], partial=[Xsb])
                                self.tt("dve", Xs[:, fs], o2, Xs[:, fs], ALU.add, [self.pb[bX2], Xsb], partial=[Xsb])
                        bU = self.next_bank()
                        for hh in range(2):
                            u = hh * 4 + c
                            fs = slice(hh * 64, hh * 64 + 64)
                            self.mm(self.bank(bU)[0:64, fs], TmF[:, u, :], Xs[:, fs], True, True, [TmFb, Xsb],
                                    self.pb[bU])
                        self.copy("act", Us[:, :], self.bank(bU)[0:64, 0:128], [self.pb[bU]], writes=[Usb])
                        for hh in range(2):
                            u = hh * 4 + c
                            hs = slice(hh * 64, hh * 64 + 64)
                            fs = slice(hh * 64, hh * 64 + 64)
                            cs = slice(c * 64, (c + 1) * 64)
                            bY = self.next_bank()
                            bS = self.next_bank()
                            oy = self.bank(bY)[hs, 0:64]
                            os_ = self.bank(bS)[hs, 0:64]
                            if hh == 0:
                                self.mm(oy, S[hs, :], AR[hs, c, 1, :], True, False, [Sb, ARb], self.pb[bY])
                                self.mm(oy, Us[:, fs], Ab[:, u, 64:128], False, False, [Usb, Abb], self.pb[bY])
                                self.mm(oy, Vtm[:, c, fs], Ak[:, u, 64:128], False, True, [Vtmb, Akb], self.pb[bY])
                                self.copy("act", yraw[hs, cs], oy, [self.pb[bY]], partial=[yrawb])
                            else:
                                self.mm(oy, S[hs, :], AR[hs, c, 1, :], True, True, [Sb, ARb], self.pb[bY])
                                bY2 = self.next_bank()
                                oy2 = self.bank(bY2)[hs, 0:64]
                                self.mm(oy2, Us[:, fs], Ab[:, u, 64:128], True, False, [Usb, Abb], self.pb[bY2])
                                self.mm(oy2, Vtm[:, c, fs], Ak[:, u, 64:128], False, True, [Vtmb, Akb], self.pb[bY2])
                                self.copy("act", yraw[hs, cs], oy, [self.pb[bY]], partial=[yrawb])
                                self.tt("dve", yraw[hs, cs], oy2, yraw[hs, cs], ALU.add, [self.pb[bY2], yrawb],
                                        partial=[yrawb])
                            self.mm(os_, Btm[:, c, fs], Us[:, fs], True, False, [Btmb, Usb], self.pb[bS])
                            self.mm(os_, Ktm[:, c, fs], Vtm[:, c, fs], False, True, [Ktmb, Vtmb], self.pb[bS])
                            self.stt(S[hs, :], S[hs, :], Win[hs, c * 64 + 63:c * 64 + 64], os_,
                                     ALU.mult, ALU.add, [Sb, Winb, self.pb[bS]], partial=[Sb])
                    yield
                    if RSTOP == 10:
                        return
                    bm = self.next_bank()
                    self.mm(self.bank(bm)[:, 0:n], bones, yraw[:, :], True, True, [cfb, yrawb], self.pb[bm])
                    self.stt(yc[:, :], self.bank(bm)[:, 0:n], -1.0 / 64, yraw[:, :], ALU.mult, ALU.add,
                             [self.pb[bm], yrawb], writes=[ycb])
                    self.act(ysq[:, :], yc[:, :], AF.Square, [ycb], writes=[ysqb])
                    bv2 = self.next_bank()
                    self.mm(self.bank(bv2)[:, 0:n], bones, ysq[:, :], True, True, [cfb, ysqb], self.pb[bv2])
                    self.ts("dve", yrs[:, :], self.bank(bv2)[:, 0:n], 1.0 / 64, GN_EPS, ALU.mult, ALU.add,
                            [self.pb[bv2]], writes=[yrsb])
                    self.act(yrs[:, :], yrs[:, :], AF.Sqrt, [yrsb], writes=[yrsb])
                    self.P.op("dve", lambda e: e.reciprocal(out=yrs[:, :], in_=yrs[:, :]), reads=[yrsb],
                              writes=[yrsb])
                    self.tt("dve", yc[:, :], yc[:, :], yrs[:, :], ALU.mult, [ycb, yrsb], writes=[ycb])
                    self.ts("dve", yc[:, :], yc[:, :], self.vcol(li, "gn_w", hp), self.vcol(li, "gn_b", hp),
                            ALU.mult, ALU.add, [ycb, vb], writes=[ycb])
                    self.tt("dve", yc[:, :], yc[:, :], bon[:, :], ALU.add, [ycb, bonb], writes=[ycb])
                    self.tt("dve", yT[:, hp, tsl], yc[:, :], gg[:, :], ALU.mult, [ycb, ggb], partial=[yTb])

        A.reset(mz)
        P.fence()
        A2 = Arena(self.nc)
        A2.off = m_xn
        A2.limit = m_xn + xn_bytes
        nhp = int(os.environ.get('NHP', 8))
        gens = [stream(A2, [h for h in range(nhp) if h % 2 == 0]),
                stream(A, [h for h in range(nhp) if h % 2 == 1])]
        active = list(gens)
        while active:
            for g_ in list(active):
                try:
                    next(g_)
                except StopIteration:
                    active.remove(g_)
        self.A.peak = max(self.A.peak, A2.peak)

    def moba_rwkv(self, li, only=None):
        A, P = self.A, self.P
        m = A.mark()
        p = "l%d_" % li
        yT = A.alloc([128, 8, T], BF16)
        yTb = P.buf("eyT")
        m_xn = A.mark()
        xn = A.alloc([128, NCH, T], BF16)
        xnb = P.buf("exn")
        m1 = A.mark()
        xn_bytes = m1 - m_xn
        if only == "B":
            A.off = m1 + 70 * 1024
            self.rmsnorm(li, "norm_mix", xn, xnb, 0, T, "e")
            A.reset(m1)
            P.fence()
            self.rwkv(li, xn, xnb, yT, yTb, m_xn, xn_bytes)
            for h in range(int(os.environ.get('NHP', 8))):
                self.dump(yT[:, h, :], h * T, T, [yTb])
            A.reset(m)
            return
        self._attn_phase(li, xn, xnb, yT, yTb)
        if only == "A":
            for h in range(8):
                self.dump(yT[:, h, :], h * T, T, [yTb])
            A.reset(m)
            return
        A.reset(m1)
        P.fence()
        self.out_proj(self.w[p + "w_out"][0:1024, :], yT, yTb, 8, "ea")
        A.reset(m1)
        P.fence()
        self.rwkv(li, xn, xnb, yT, yTb, m_xn, xn_bytes)
        A.reset(m1)
        P.fence()
        self.out_proj(self.w[p + "w_out"][1024:2048, :], yT, yTb, 8, "eb")
        A.reset(m)

    def _attn_phase(self, li, xn, xnb, yT, yTb):
        A = self.A
        m = A.mark()
        top = A.mark()
        self.attention_alloc_only = True
        A.reset(top)
        A.off = top + 62 * 1024
        self.rmsnorm(li, "norm_mix", xn, xnb, 0, T, "e")
        A.reset(top)
        self.attention(li, xn, xnb, yT, yTb)
        assert A.off <= top + 62 * 1024, A.off - top
        A.reset(m)

    def mixer(self, li):
        if li % 2 == 1:
            self.sconv(li)
        else:
            self.moba_rwkv(li)


BIG_WEIGHTS = {
    0: [("w_in", (D, 6432)), ("w_out", (D, D)), ("w_lora", (64, 1024)), ("a_lora", (64, 1024)),
        ("g_lora", (160, 1024)), ("ffn_up", (D, 2 * DFF)), ("ffn_down", (DFF, D))],
    1: [("conv_in", (D, 3 * D)), ("conv_out", (D, D)), ("ffn_up", (D, 2 * DFF)), ("ffn_down", (DFF, D))],
    2: [("w_in", (D, 6464)), ("w_out", (D, D)), ("w_lora", (64, 1024)), ("a_lora", (64, 1024)),
        ("g_lora", (160, 1024)), ("v_lora", (32, 1024)), ("ffn_up", (D, 2 * DFF)), ("ffn_down", (DFF, D))],
    3: [("conv_in", (D, 3 * D)), ("conv_out", (D, D)), ("ffn_up", (D, 2 * DFF)), ("ffn_down", (DFF, D))],
}


def build_program(stages):
    B = Builder()
    layers = sorted(set(int(s[-1]) for s in stages))
    B.load_consts()
    for li in layers:
        B.load_vecs(li)
    for st in stages:
        li = int(st[-1])
        for nm, shp in BIG_WEIGHTS[li]:
            key = "l%d_%s" % (li, nm)
            if key not in B.w and ((st.startswith("ffn") and nm.startswith("ffn")) or
                                   (st.startswith("mix") and not nm.startswith("ffn"))):
                B.dram_in(key, shp)
    B.P.fence()
    B.prologue()
    for st in stages:
        li = int(st[-1])
        B.P.fence()
        if st.startswith("ffn"):
            B.ffn(li)
        elif st.startswith("mixA"):
            B.moba_rwkv(li, only="A")
        elif st.startswith("mixB"):
            B.moba_rwkv(li, only="B")
        else:
            B.mixer(li)
    B.P.fence()
    B.epilogue()
    B.P.emit(final_waits=[B.outb, B.dbgb])
    return B


def make_in_map(B, inputs, xb):
    cf, cb = build_consts()
    m = {"x": np.ascontiguousarray(xb, dtype=np.float32), "consts": cf, "constsb": cb}
    for li in B.vecs:
        m["vecs%d" % li] = build_layer_vecs(li, inputs)
    for key in B.w:
        m[key] = np.ascontiguousarray(inputs[key], dtype=np.float32)
    return m


ALL_STAGES = ["mix0", "ffn0", "mix1", "ffn1", "mix2", "ffn2", "mix3", "ffn3"]
_CACHE = {}


def kernel(**inputs):
    x = np.asarray(inputs["x"], dtype=np.float32)
    nb = x.shape[0]
    if "B" not in _CACHE:
        _CACHE["B"] = build_program(ALL_STAGES)
    B = _CACHE["B"]
    shared = make_in_map(B, inputs, x[0])
    in_maps = []
    for b in range(nb):
        m = dict(shared)
        m["x"] = np.ascontiguousarray(x[b])
        in_maps.append(m)
    res = run_bass_kernel_spmd(B.nc, in_maps, core_ids=list(range(nb)))
    out = np.stack([np.asarray(res.results[b]["out"], dtype=np.float32) for b in range(nb)], axis=0)
    return out
```

```python
import os
import numpy as np
from contextlib import ExitStack
import concourse.bass as bass
import concourse.mybir as mybir
from concourse.bass_utils import run_bass_kernel_spmd

F32 = mybir.dt.float32
BF16 = mybir.dt.bfloat16
AF = mybir.ActivationFunctionType
ALU = mybir.AluOpType
AX = mybir.AxisListType


class Sem:
    __slots__ = ("total", "handle", "name")

    def __init__(self, name):
        self.total = 0
        self.handle = None
        self.name = name


class Buf:
    __slots__ = ("name", "w_eng", "w_dma", "r_eng", "r_dma", "p_eng", "p_dma", "sem")

    def __init__(self, name):
        self.name = name
        self.w_eng = {}
        self.w_dma = []
        self.r_eng = {}
        self.r_dma = []
        self.p_eng = {}
        self.p_dma = []
        self.sem = None


class Prog:
    ENGS = ("pe", "act", "dve", "pool", "sp")

    def __init__(self, nc, es):
        self.nc = nc
        self.es = es
        self.ins = []
        self.nbuf = 0
        self.fence_idx = None
        self.local = []
        self.last_eng = {}
        self.dma_since = []
        self.free_sems = []
        self.all_sems = []

    def buf(self, name, persistent=False):
        self.nbuf += 1
        b = Buf("%s_%d" % (name, self.nbuf))
        if self.fence_idx is not None:
            b.p_eng = {"sp": self.fence_idx}
        if not persistent:
            self.local.append(b)
        return b

    def bufs(self, name, n, persistent=False):
        return [self.buf("%s%d" % (name, i), persistent) for i in range(n)]

    def fence(self):
        idx = len(self.ins)
        deps = set(self.last_eng.values()) | set(self.dma_since)
        rec = dict(eng="sp", fn=lambda e: e.nop(), deps=deps, dma=False, sem=None, total_at={},
                   signal=False)
        for d in deps:
            dr = self.ins[d]
            if dr["dma"]:
                rec["total_at"][d] = dr["sem"].total
        self.ins.append(rec)
        self.last_eng["sp"] = idx
        self.fence_idx = idx
        self.dma_since = []
        for b in self.local:
            if b.sem is not None:
                self.free_sems.append(b.sem)
                b.sem = None
        self.local = []

    def op(self, eng, fn, reads=(), writes=(), partial=(), dma_owner=None):
        idx = len(self.ins)
        deps = set()
        for b in reads:
            deps.update(b.w_eng.values())
            deps.update(b.w_dma)
        for b in writes:
            deps.update(b.w_eng.values())
            deps.update(b.w_dma)
            deps.update(b.r_eng.values())
            deps.update(b.r_dma)
            deps.update(b.p_eng.values())
            deps.update(b.p_dma)
        for b in partial:
            if b.r_eng or b.r_dma:
                b.p_eng, b.p_dma = b.r_eng, b.r_dma
                b.r_eng, b.r_dma = {}, []
                b.w_eng, b.w_dma = {}, []
            deps.update(b.p_eng.values())
            deps.update(b.p_dma)
        is_dma = dma_owner is not None
        rec = dict(eng=eng, fn=fn, deps=deps, dma=is_dma, sem=None, total_at={}, signal=False)
        for d in deps:
            dr = self.ins[d]
            if dr["dma"]:
                rec["total_at"][d] = dr["sem"].total
        if is_dma:
            if dma_owner.sem is None:
                kind = "sw" if eng == "pool" else "hw"
                pool = [s for s in self.free_sems if s.name.startswith(kind)]
                if pool:
                    dma_owner.sem = pool[-1]
                    self.free_sems.remove(pool[-1])
                else:
                    dma_owner.sem = Sem("%s%d" % (kind, len(self.all_sems)))
                    self.all_sems.append(dma_owner.sem)
            else:
                assert dma_owner.sem.name.startswith("sw") == (eng == "pool"), dma_owner.name
            rec["sem"] = dma_owner.sem
            dma_owner.sem.total += 16
            rec["tok"] = dma_owner.sem.total
            self.dma_since.append(idx)
        self.ins.append(rec)
        self.last_eng[eng] = idx
        for b in reads:
            if is_dma:
                b.r_dma.append(idx)
            else:
                b.r_eng[eng] = idx
        for b in writes:
            b.w_eng, b.w_dma, b.r_eng, b.r_dma, b.p_eng, b.p_dma = {}, [], {}, [], {}, []
            if is_dma:
                b.w_dma.append(idx)
            else:
                b.w_eng[eng] = idx
        for b in partial:
            if is_dma:
                b.w_dma.append(idx)
            else:
                b.w_eng[eng] = idx
        return idx

    def emit(self, final_waits=()):
        nc = self.nc
        es = self.es
        ins = self.ins
        for r in ins:
            for d in r["deps"]:
                dr = ins[d]
                if not dr["dma"]:
                    if dr["eng"] == "pe" and r["eng"] == "pe" and not r["dma"]:
                        continue
                    dr["signal"] = True
        esem = {e: es.enter_context(nc.semaphore("s_" + e)) for e in self.ENGS}
        cnt = {e: 0 for e in self.ENGS}
        for r in ins:
            if not r["dma"] and r["signal"]:
                cnt[r["eng"]] += 1
                r["cnt"] = cnt[r["eng"]]
        for s in self.all_sems:
            s.handle = es.enter_context(nc.semaphore(s.name))
        self.n_dma_sems = len(self.all_sems)
        per_eng = {e: [] for e in self.ENGS}
        for i, r in enumerate(ins):
            per_eng[r["eng"]].append(i)
        last_outs = [(b.sem.handle, b.sem.total) for b in final_waits if b.sem is not None]

        def run_engine(ename, eng):
            waited = {}
            for i in per_eng[ename]:
                r = ins[i]
                need = {}
                for d in r["deps"]:
                    dr = ins[d]
                    if dr["dma"]:
                        s = dr["sem"].handle
                        v = max(r["total_at"][d], dr["tok"])
                    else:
                        if dr["eng"] == "pe" and ename == "pe" and not r["dma"]:
                            continue
                        s = esem[dr["eng"]]
                        v = dr["cnt"]
                    key = id(s)
                    if key not in need or need[key][1] < v:
                        need[key] = (s, v)
                for key, (s, v) in need.items():
                    if waited.get(key, 0) >= v:
                        continue
                    eng.wait_ge(s, v)
                    waited[key] = v
                bi = r["fn"](eng)
                if r["dma"]:
                    bi.then_inc(r["sem"].handle, 16)
                elif r["signal"]:
                    bi.then_inc(esem[ename], 1)
            if ename == "sp":
                for h, v in last_outs:
                    eng.wait_ge(h, v)

        with nc.Block() as block:
            @block.tensor
            def _(e):
                run_engine("pe", e)

            @block.scalar
            def _(e):
                run_engine("act", e)

            @block.vector
            def _(e):
                run_engine("dve", e)

            @block.gpsimd
            def _(e):
                run_engine("pool", e)

            @block.sync
            def _(e):
                run_engine("sp", e)


T = 2048
D = 2048
NCH = D // 128
DFF = 5632
NJ = DFF // 128
RMS_EPS = 1e-6
RSTOP = int(os.environ.get('RSTOP', 99))
R8SUB = int(os.environ.get('R8SUB', 0))
GN_EPS = 64e-5
DBG_N = 16384
import os
NTT = int(os.environ.get('NTT', T // 128))
SB_BASE = 16640
SB_LIMIT = 229000


def _dtsize(dt):
    return 4 if dt == F32 else 2


class Arena:
    def __init__(self, nc):
        self.nc = nc
        self.off = SB_BASE
        self.n = 0
        self.peak = 0

    def alloc(self, shape, dtype):
        nb = _dtsize(dtype)
        for s in shape[1:]:
            nb *= s
        off = (self.off + 31) // 32 * 32
        h = self.nc.alloc_sbuf_tensor_at("t%d" % self.n, list(shape), dtype, offset=off)
        self.n += 1
        self.off = off + nb
        self.peak = max(self.peak, self.off)
        assert self.off <= getattr(self, "limit", SB_LIMIT), ("SBUF overflow", self.off)
        return h

    def mark(self):
        return self.off

    def reset(self, m):
        self.off = m


def pack_vec(v):
    v = np.asarray(v, np.float32).reshape(-1)
    n = (v.size + 127) // 128
    if v.size != n * 128:
        v = np.concatenate([v, np.zeros(n * 128 - v.size, np.float32)])
    return np.ascontiguousarray(v.reshape(n, 128).T)


class VecPack:
    def __init__(self):
        self.cols = {}
        self.n = 0
        self.parts = []

    def add(self, name, arr2d):
        self.cols[name] = (self.n, arr2d.shape[1])
        self.n += arr2d.shape[1]
        self.parts.append(arr2d)

    def array(self):
        return np.ascontiguousarray(np.concatenate(self.parts, axis=1).astype(np.float32))


def layer_vec_layout(li):
    lay = {}
    n = 0

    def add(name, c):
        nonlocal n
        lay[name] = (n, c)
        n += c
    add("norm_mix", 16)
    if li % 2 == 1:
        add("conv_w", 48)
    else:
        add("q_gain", 1)
        add("k_gain", 1)
        nmu = 27
        add("shift_mu", nmu)
        for nm in ("w0", "a0", "k_k", "k_a", "r_k", "gn_w", "gn_b", "v0"):
            add(nm, 8)
    add("norm_ffn", 16)
    add("ffn_conv", 3 * 88)
    return lay, n


def build_layer_vecs(li, inputs):
    p = "l%d_" % li
    lay, n = layer_vec_layout(li)
    out = np.zeros((128, n), np.float32)

    def put(name, arr2d):
        c, w = lay[name]
        assert arr2d.shape[1] <= w, (name, arr2d.shape, w)
        out[:, c:c + arr2d.shape[1]] = arr2d
    put("norm_mix", pack_vec(inputs[p + "norm_mix"]))
    if li % 2 == 1:
        cw = np.asarray(inputs[p + "conv_w"])
        put("conv_w", np.concatenate([pack_vec(cw[j]) for j in range(3)], axis=1))
    else:
        put("q_gain", pack_vec(inputs[p + "q_gain"]))
        put("k_gain", pack_vec(inputs[p + "k_gain"]))
        put("shift_mu", pack_vec(inputs[p + "shift_mu"]))
        for nm in ("w0", "a0", "k_k", "k_a", "gn_w", "gn_b"):
            put(nm, pack_vec(inputs[p + nm]))
        put("r_k", pack_vec(np.asarray(inputs[p + "r_k"]).reshape(-1)))
        if li > 0:
            put("v0", pack_vec(inputs[p + "v0"]))
    put("norm_ffn", pack_vec(inputs[p + "norm_ffn"]))
    fc = np.asarray(inputs[p + "ffn_conv"])
    put("ffn_conv", np.concatenate([pack_vec(fc[j]) for j in range(3)], axis=1))
    return out


C_IDENT = 0
C_BONES = 128
C_BIASCOL = 256
C_GMASK = 264
C_AMASK = 328
C_NTMASK = 840
C_ONE64 = 1352
C_ID8 = 1416
NCONST = 1928
B_IDENT = 0
B_ONES = 128
B_CAUSAL = 256
B_ONEHOT = 384
B_DL = 1408
B_DS = 3456
NCONST_BF = 4480
A_HEADS = 8


def build_consts():
    c = np.zeros((128, NCONST), np.float32)
    c[:, C_IDENT:C_IDENT + 128] = np.eye(128, dtype=np.float32)
    bo = np.zeros((128, 128), np.float32)
    bo[:64, :64] = 1.0
    bo[64:, 64:] = 1.0
    c[:, C_BONES:C_BONES + 128] = bo
    slopes = np.exp2(-8.0 * np.arange(1, A_HEADS + 1) / A_HEADS).astype(np.float32)
    pidx = np.arange(128, dtype=np.float32)
    for h in range(A_HEADS):
        c[:, C_BIASCOL + h] = slopes[h] * (pidx - 127.0)
    for i in range(8):
        qb = 4 + i // 2
        for n in range(8):
            c[:, C_GMASK + i * 8 + n] = 0.0 if n < qb else -1e30
    s = np.arange(64)[:, None]
    t = np.arange(64)[None, :]
    am = np.concatenate([(s < t), (s <= t)], axis=1).astype(np.float32)
    c[:64, C_AMASK:C_AMASK + 512] = np.tile(am, (1, 4))
    nt = (t < s).astype(np.float32)
    c[:64, C_NTMASK:C_NTMASK + 512] = np.tile(nt, (1, 8))
    c[:, C_ONE64:C_ONE64 + 64] = 1.0
    c[:64, C_ID8:C_ID8 + 512] = np.tile(np.eye(64, dtype=np.float32), (1, 8))
    b = np.zeros((128, NCONST_BF), np.float32)
    b[:, B_IDENT:B_IDENT + 128] = np.eye(128, dtype=np.float32)
    b[:, B_ONES:B_ONES + 128] = 1.0
    kk = np.arange(128)[:, None]
    qq = np.arange(128)[None, :]
    b[:, B_CAUSAL:B_CAUSAL + 128] = np.where(kk > qq, -30000.0, 0.0)
    for n in range(8):
        b[n, B_ONEHOT + n * 128:B_ONEHOT + (n + 1) * 128] = 1.0
    for dl in range(16):
        b[0, B_DL + dl * 128:B_DL + (dl + 1) * 128] = float(dl)
    for h in range(A_HEADS):
        b[0, B_DS + h * 128:B_DS + (h + 1) * 128] = -128.0 * slopes[h]
    return c, b


class Builder:
    def __init__(self):
        self.nc = nc = bass.Bass("TRN2", target_bir_lowering=False)
        self.es = ExitStack()
        self.P = Prog(nc, self.es)
        self.A = Arena(nc)
        self.w = {}
        self.x_in = nc.dram_tensor("x", [T, D], F32, kind="ExternalInput").ap()
        self.out = nc.dram_tensor("out", [T, D], F32, kind="ExternalOutput").ap()
        self.consts_d = nc.dram_tensor("consts", [128, NCONST], F32, kind="ExternalInput").ap()
        self.constsb_d = nc.dram_tensor("constsb", [128, NCONST_BF], F32, kind="ExternalInput").ap()
        self.dbg = None
        self.dbgb = self.P.buf("dbg", True)
        self.xres = nc.dram_tensor("xres", [NCH, 128, T], F32, kind="Internal").ap()
        self.vfirst = nc.dram_tensor("vfirst", [8, 128, T], F32, kind="Internal").ap()
        self.vfirstb = self.P.buf("vfirst", True)
        self.rkv = nc.dram_tensor("rkv", [24, 128, T], F32, kind="Internal").ap()
        self.rkvb = self.P.buf("rkv", True)
        self.xb = [self.P.buf("xres0", True), self.P.buf("xres1", True)]
        self.outb = self.P.buf("outb", True)
        self.ps = nc.alloc_psum_tensor("ps", [128, 4096], F32)
        self.pb = self.P.bufs("psb", 8, True)
        self.vecs = {}
        self.vlay = {}

    def bank(self, b, n=512):
        return self.ps[:, b * 512:b * 512 + n]

    def next_bank(self, lo=0, hi=8):
        k = (lo, hi)
        if not hasattr(self, "_bks"):
            self._bks = {}
        v = self._bks.get(k, lo - 1) + 1
        if v >= hi:
            v = lo
        self._bks[k] = v
        return v

    def dump(self, ap, col0, ncols, reads):
        if self.dbg is None:
            self.dbg = self.nc.dram_tensor("dbg", [128, DBG_N], F32, kind="ExternalOutput").ap()
        self.dma("pool", self.dbg[:, col0:col0 + ncols], ap, reads, partial=[self.dbgb],
                 owner=self.dbgb)

    def dram_in(self, name, shape):
        self.w[name] = self.nc.dram_tensor(name, list(shape), F32, kind="ExternalInput").ap()
        return self.w[name]

    def dma(self, q, out, in_, reads, writes=(), partial=(), owner=None):
        self.P.op(q, lambda e: e.dma_start(out=out, in_=in_), reads=reads, writes=writes,
                  partial=partial, dma_owner=owner)

    def mm(self, out, lhsT, rhs, start, stop, reads, wbuf):
        self.P.op("pe", lambda e: e.matmul(out, lhsT, rhs, start=start, stop=stop),
                  reads=reads, writes=[wbuf])

    def tr(self, out, in_, ident, reads, wbuf):
        self.P.op("pe", lambda e: e.transpose(out, in_, ident), reads=reads, writes=[wbuf])

    def act(self, out, in_, func, reads, writes=(), partial=(), scale=1.0, bias=None):
        if bias is None:
            self.P.op("act", lambda e: e.activation(out=out, in_=in_, func=func, scale=scale),
                      reads=reads, writes=writes, partial=partial)
        else:
            self.P.op("act", lambda e: e.activation(out=out, in_=in_, func=func, scale=scale,
                                                    bias=bias),
                      reads=reads, writes=writes, partial=partial)

    def ts(self, eng, out, in0, s1, s2, op0, op1, reads, writes=(), partial=()):
        if s2 is None:
            self.P.op(eng, lambda e: e.tensor_scalar(out=out, in0=in0, scalar1=s1, scalar2=None,
                                                     op0=op0),
                      reads=reads, writes=writes, partial=partial)
        else:
            self.P.op(eng, lambda e: e.tensor_scalar(out=out, in0=in0, scalar1=s1, scalar2=s2,
                                                     op0=op0, op1=op1),
                      reads=reads, writes=writes, partial=partial)

    def tt(self, eng, out, in0, in1, op, reads, writes=(), partial=()):
        self.P.op(eng, lambda e: e.tensor_tensor(out=out, in0=in0, in1=in1, op=op),
                  reads=reads, writes=writes, partial=partial)

    def stt(self, out, in0, scalar, in1, op0, op1, reads, writes=(), partial=()):
        self.P.op("dve", lambda e: e.scalar_tensor_tensor(out=out, in0=in0, scalar=scalar, in1=in1,
                                                          op0=op0, op1=op1),
                  reads=reads, writes=writes, partial=partial)

    def copy(self, eng, out, in_, reads, writes=(), partial=()):
        if eng == "act":
            self.P.op("act", lambda e: e.copy(out=out, in_=in_), reads=reads, writes=writes,
                      partial=partial)
        else:
            self.P.op(eng, lambda e: e.tensor_copy(out=out, in_=in_), reads=reads, writes=writes,
                      partial=partial)

    def load_consts(self):
        A, P = self.A, self.P
        self.cf = A.alloc([128, NCONST], F32)
        self.cfb = P.buf("cf", True)
        self.dma("sp", self.cf[:, :], self.consts_d, [], [self.cfb], owner=self.cfb)
        self.cb = A.alloc([128, 384], BF16)
        self.cbb = P.buf("cb", True)
        self.dma("pool", self.cb[:, :], self.constsb_d[:, 0:384], [], [self.cbb], owner=self.cbb)
        self.ident_f = self.cf[:, C_IDENT:C_IDENT + 128]
        self.ident_b = self.cb[:, B_IDENT:B_IDENT + 128]
        self.ones_b = self.cb[:, B_ONES:B_ONES + 128]

    def load_vecs(self, li):
        lay, n = layer_vec_layout(li)
        d = self.nc.dram_tensor("vecs%d" % li, [128, n], F32, kind="ExternalInput").ap()
        t = self.A.alloc([128, n], F32)
        b = self.P.buf("vecs%d" % li, True)
        self.dma("sp", t[:, :], d, [], [b], owner=b)
        self.vecs[li] = (t, b)
        self.vlay[li] = lay

    def vcol(self, li, name, c=0, n=1):
        t, b = self.vecs[li]
        c0, w = self.vlay[li][name]
        return t[:, c0 + c:c0 + c + n]

    def prologue(self):
        A, P = self.A, self.P
        m = A.mark()
        xin = [A.alloc([128, D], F32) for _ in range(2)]
        xinb = P.bufs("xin", 2)
        xo = [A.alloc([128, NCH, 128], F32) for _ in range(2)]
        xob = P.bufs("xo", 2)
        xresT = self.xres.rearrange("c p t -> p c t")
        for tt in range(NTT):
            s = tt % 2
            self.dma("sp", xin[s][:, :], self.x_in[tt * 128:(tt + 1) * 128, :], [], [xinb[s]],
                     owner=xinb[s])
            for g in range(4):
                bk = self.next_bank()
                for q in range(4):
                    dc = g * 4 + q
                    self.tr(self.bank(bk)[:, q * 128:(q + 1) * 128],
                            xin[s][:, dc * 128:(dc + 1) * 128], self.ident_f,
                            [xinb[s], self.cfb], self.pb[bk])
                eng = "act" if g % 2 == 0 else "dve"
                self.copy(eng, xo[s][:, g * 4:(g + 1) * 4, :],
                          self.bank(bk).rearrange("p (q t) -> p q t", q=4),
                          [self.pb[bk]], partial=[xob[s]])
            self.dma("sp", xresT[:, :, tt * 128:(tt + 1) * 128], xo[s][:, :, :], [xob[s]],
                     partial=[self.xb[tt // 8]], owner=self.xb[tt // 8])
        A.reset(m)

    def epilogue(self):
        A, P = self.A, self.P
        m = A.mark()
        xi = [A.alloc([128, NCH, 128], F32) for _ in range(2)]
        xib = P.bufs("exi", 2)
        xo = [A.alloc([128, D], F32) for _ in range(2)]
        xob = P.bufs("exo", 2)
        xresT = self.xres.rearrange("c p t -> p c t")
        for tt in range(NTT):
            s = tt % 2
            self.dma("sp", xi[s][:, :, :], xresT[:, :, tt * 128:(tt + 1) * 128], [self.xb[tt // 8]],
                     [xib[s]], owner=xib[s])
            for g in range(4):
                bk = self.next_bank()
                for q in range(4):
                    dc = g * 4 + q
                    self.tr(self.bank(bk)[:, q * 128:(q + 1) * 128], xi[s][:, dc, :], self.ident_f,
                            [xib[s], self.cfb], self.pb[bk])
                eng = "act" if g % 2 == 0 else "dve"
                self.copy(eng, xo[s][:, g * 512:(g + 1) * 512], self.bank(bk), [self.pb[bk]],
                          partial=[xob[s]])
            self.dma("sp", self.out[tt * 128:(tt + 1) * 128, :], xo[s][:, :], [xob[s]],
                     partial=[self.outb], owner=self.outb)
        A.reset(m)

    def rmsnorm(self, li, gname, xn, xnb, t0, n, tag):
        A, P = self.A, self.P
        m = A.mark()
        halves = sorted(set([t0 // 1024, (t0 + n - 1) // 1024]))
        rb = [self.xb[h] for h in halves]
        W = 512
        xc = [A.alloc([128, W], F32) for _ in range(2)]
        xcb = P.bufs("xc" + tag, 2)
        sq = [A.alloc([128, W], BF16) for _ in range(2)]
        sqb = P.bufs("sq" + tag, 2)
        rstd = A.alloc([128, n], F32)
        rsb = P.buf("rstd" + tag)
        nb = n // W
        k = 0
        q = 0
        for b in range(nb):
            bk = self.next_bank()
            for c in range(NCH):
                s = k % 2
                k += 1
                self.dma("sp", xc[s][:, :], self.xres[c, :, t0 + b * W:t0 + (b + 1) * W], rb,
                         [xcb[s]], owner=xcb[s])
                q = (q + 1) % 2
                self.act(sq[q][:, :], xc[s][:, :], AF.Square, [xcb[s]], [sqb[q]])
                self.mm(self.bank(bk), self.ones_b, sq[q][:, :], c == 0, c == NCH - 1,
                        [sqb[q], self.cbb], self.pb[bk])
            self.ts("dve", rstd[:, b * W:(b + 1) * W], self.bank(bk), 1.0 / D, RMS_EPS,
                    ALU.mult, ALU.add, [self.pb[bk]], partial=[rsb])
        self.act(rstd[:, :], rstd[:, :], AF.Sqrt, [rsb], [rsb])
        self.P.op("dve", lambda e: e.reciprocal(out=rstd[:, :], in_=rstd[:, :]), reads=[rsb],
                  writes=[rsb])
        vb = self.vecs[li][1]
        for b in range(nb):
            for c in range(NCH):
                s = k % 2
                k += 1
                self.dma("sp", xc[s][:, :], self.xres[c, :, t0 + b * W:t0 + (b + 1) * W], rb,
                         [xcb[s]], owner=xcb[s])
                self.stt(xn[:, c, b * W:(b + 1) * W], xc[s][:, :], self.vcol(li, gname, c),
                         rstd[:, b * W:(b + 1) * W], ALU.mult, ALU.mult, [xcb[s], rsb, vb],
                         partial=[xnb])
        A.reset(m)

    def ffn(self, li):
        A, P = self.A, self.P
        m = A.mark()
        p = "l%d_" % li
        w_up = self.w[p + "ffn_up"]
        w_dn = self.w[p + "ffn_down"].rearrange("(j q) n -> q j n", q=128)
        n = 1024
        W = 512
        xn = A.alloc([128, NCH, n], BF16)
        xnb = P.buf("fxn")
        aT = A.alloc([128, NJ, n], BF16)
        aTb = P.buf("faT")
        wup = [A.alloc([128, 2, NCH, 128], BF16) for _ in range(3)]
        wupb = P.bufs("fwup", 3)
        hh = [[A.alloc([128, W + 2], F32) for _ in range(2)] for _ in range(2)]
        hhb = [P.bufs("fh%d_" % gv, 2) for gv in range(2)]
        cg = A.alloc([128, W], F32)
        cv = A.alloc([128, W], F32)
        cgb, cvb = P.buf("fcg"), P.buf("fcv")
        halo = A.alloc([128, 2 * NJ, 2], F32)
        halob = P.buf("fhalo")
        wdn = [A.alloc([128, NJ, 128], BF16) for _ in range(2)]
        wdnb = P.bufs("fwdn", 2)
        xr = [A.alloc([128, W], F32) for _ in range(2)]
        xrb = P.bufs("fxr", 2)
        vb = self.vecs[li][1]
        self.P.op("dve", lambda e: e.memset(halo[:, :, :], 0.0), reads=[], writes=[halob])
        wk = 0
        hk = 0
        xk = 0
        for th in range(2):
            t0 = th * n
            self.rmsnorm(li, "norm_ffn", xn, xnb, t0, n, "f")
            for j in range(NJ):
                s = wk % 3
                wk += 1
                for gv in range(2):
                    c0 = gv * DFF + j * 128
                    src = w_up[:, c0:c0 + 128].rearrange("(c q) n -> q c n", q=128)
                    if gv == 0:
                        self.dma("pool", wup[s][:, gv, :, :], src, [], writes=[wupb[s]], owner=wupb[s])
                    else:
                        self.dma("pool", wup[s][:, gv, :, :], src, [], partial=[wupb[s]], owner=wupb[s])
                for tb in range(n // W):
                    hs = hk % 2
                    hk += 1
                    for gv in range(2):
                        bk = self.next_bank()
                        for c in range(NCH):
                            self.mm(self.bank(bk), wup[s][:, gv, c, :], xn[:, c, tb * W:(tb + 1) * W],
                                    c == 0, c == NCH - 1, [wupb[s], xnb], self.pb[bk])
                        h = hh[gv][hs]
                        hB = hhb[gv][hs]
                        col = gv * NJ + j
                        self.copy("act", h[:, 0:2], halo[:, col, :], [halob], writes=[hB])
                        self.copy("act", h[:, 2:W + 2], self.bank(bk), [self.pb[bk]], partial=[hB])
                        self.copy("act", halo[:, col, :], h[:, W:W + 2], [hB], partial=[halob])
                        cdst, cB = (cg, cgb) if gv == 0 else (cv, cvb)
                        self.ts("dve", cdst[:, :], h[:, 0:W], self.vcol(li, "ffn_conv", 0 * 88 + col),
                                None, ALU.mult, None, [hB, vb], writes=[cB])
                        self.stt(cdst[:, :], h[:, 1:W + 1], self.vcol(li, "ffn_conv", 1 * 88 + col),
                                 cdst[:, :], ALU.mult, ALU.add, [hB, vb, cB], writes=[cB])
                        self.stt(cdst[:, :], h[:, 2:W + 2], self.vcol(li, "ffn_conv", 2 * 88 + col),
                                 cdst[:, :], ALU.mult, ALU.add, [hB, vb, cB], writes=[cB])
                    self.act(cg[:, :], cg[:, :], AF.Silu, [cgb], writes=[cgb])
                    self.tt("dve", aT[:, j, tb * W:(tb + 1) * W], cg[:, :], cv[:, :], ALU.mult,
                            [cgb, cvb], partial=[aTb])
            for mch in range(NCH):
                s = mch % 2
                self.dma("pool", wdn[s][:, :, :], w_dn[:, :, mch * 128:(mch + 1) * 128], [],
                         writes=[wdnb[s]], owner=wdnb[s])
                for tb in range(n // W):
                    xs = xk % 2
                    xk += 1
                    tsl = slice(t0 + tb * W, t0 + (tb + 1) * W)
                    self.dma("sp", xr[xs][:, :], self.xres[mch, :, tsl], [self.xb[th]],
                             writes=[xrb[xs]], owner=xrb[xs])
                    bk = self.next_bank()
                    for j in range(NJ):
                        self.mm(self.bank(bk), wdn[s][:, j, :], aT[:, j, tb * W:(tb + 1) * W],
                                j == 0, j == NJ - 1, [wdnb[s], aTb], self.pb[bk])
                    self.tt("dve", xr[xs][:, :], self.bank(bk), xr[xs][:, :], ALU.add,
                            [self.pb[bk], xrb[xs]], writes=[xrb[xs]])
                    self.dma("sp", self.xres[mch, :, tsl], xr[xs][:, :], [xrb[xs]],
                             partial=[self.xb[th]], owner=self.xb[th])
        A.reset(m)


    def out_proj(self, w_ap, yT, yTb, nk, tag):
        A, P = self.A, self.P
        W = 512
        w_r = w_ap.rearrange("(j q) n -> q j n", q=128)
        wo = [A.alloc([128, nk, 128], BF16) for _ in range(2)]
        wob = P.bufs("wo" + tag, 2)
        xr = [A.alloc([128, W], F32) for _ in range(3)]
        xrb = P.bufs("xr" + tag, 3)
        xk = 0
        for mch in range(NCH):
            s = mch % 2
            self.dma("pool", wo[s][:, :, :], w_r[:, :, mch * 128:(mch + 1) * 128], [],
                     writes=[wob[s]], owner=wob[s])
            for tb in range(T // W):
                xs = xk % 3
                xk += 1
                th = (tb * W) // 1024
                tsl = slice(tb * W, (tb + 1) * W)
                self.dma("sp", xr[xs][:, :], self.xres[mch, :, tsl], [self.xb[th]],
                         writes=[xrb[xs]], owner=xrb[xs])
                bk = self.next_bank()
                for j in range(nk):
                    self.mm(self.bank(bk), wo[s][:, j, :], yT[:, j, tsl], j == 0, j == nk - 1,
                            [wob[s], yTb], self.pb[bk])
                self.tt("dve", xr[xs][:, :], self.bank(bk), xr[xs][:, :], ALU.add,
                        [self.pb[bk], xrb[xs]], writes=[xrb[xs]])
                self.dma("sp", self.xres[mch, :, tsl], xr[xs][:, :], [xrb[xs]],
                         partial=[self.xb[th]], owner=self.xb[th])

    def sconv(self, li):
        A, P = self.A, self.P
        m = A.mark()
        p = "l%d_" % li
        w_in = self.w[p + "conv_in"]
        W = 512
        xn = A.alloc([128, NCH, T], BF16)
        xnb = P.buf("sxn")
        zT = A.alloc([128, NCH, T], BF16)
        zTb = P.buf("szT")
        m2 = A.mark()
        wci = [A.alloc([128, 3, NCH, 128], BF16) for _ in range(3)]
        wcib = P.bufs("swci", 3)
        usb = [A.alloc([128, W], F32) for _ in range(2)]
        usbb = P.bufs("susb", 2)
        cu = [A.alloc([128, W + 2], F32) for _ in range(2)]
        cub = P.bufs("scu", 2)
        cc = [A.alloc([128, W], F32) for _ in range(2)]
        ccb = P.bufs("scc", 2)
        self.rmsnorm(li, "norm_mix", xn, xnb, 0, T, "s")
        vb = self.vecs[li][1]
        k = 0
        for j in range(NCH):
            s = j % 3
            for g in range(3):
                c0 = g * D + j * 128
                src = w_in[:, c0:c0 + 128].rearrange("(c q) n -> q c n", q=128)
                if g == 0:
                    self.dma("pool", wci[s][:, g, :, :], src, [], writes=[wcib[s]], owner=wcib[s])
                else:
                    self.dma("pool", wci[s][:, g, :, :], src, [], partial=[wcib[s]], owner=wcib[s])
            for tb in range(T // W):
                q = k % 2
                k += 1
                tsl = slice(tb * W, (tb + 1) * W)
                bks = []
                for g in range(3):
                    bk = self.next_bank()
                    bks.append(bk)
                    for c in range(NCH):
                        self.mm(self.bank(bk), wci[s][:, g, c, :], xn[:, c, tsl], c == 0, c == NCH - 1,
                                [wcib[s], xnb], self.pb[bk])
                self.copy("act", usb[q][:, :], self.bank(bks[2]), [self.pb[bks[2]]], writes=[usbb[q]])
                if tb == 0:
                    self.P.op("dve", (lambda t: (lambda e: e.memset(t, 0.0)))(cu[q][:, 0:2]), reads=[],
                              writes=[cub[q]])
                else:
                    self.copy("act", cu[q][:, 0:2], cu[1 - q][:, W:W + 2], [cub[1 - q]], writes=[cub[q]])
                self.tt("dve", cu[q][:, 2:W + 2], self.bank(bks[1]), usb[q][:, :], ALU.mult,
                        [self.pb[bks[1]], usbb[q]], partial=[cub[q]])
                for tap in range(3):
                    wcol = self.vcol(li, "conv_w", tap * 16 + j)
                    if tap == 0:
                        self.ts("dve", cc[q][:, :], cu[q][:, 0:W], wcol, None, ALU.mult, None,
                                [cub[q], vb], writes=[ccb[q]])
                    else:
                        self.stt(cc[q][:, :], cu[q][:, tap:W + tap], wcol, cc[q][:, :], ALU.mult, ALU.add,
                                 [cub[q], vb, ccb[q]], writes=[ccb[q]])
                self.tt("dve", zT[:, j, tsl], self.bank(bks[0]), cc[q][:, :], ALU.mult,
                        [self.pb[bks[0]], ccb[q]], partial=[zTb])
        A.reset(m2)
        P.fence()
        self.out_proj(self.w[p + "conv_out"], zT, zTb, NCH, "s")
        A.reset(m)

    def attention(self, li, xn, xnb, yT, yTb):
        A, P = self.A, self.P
        p = "l%d_" % li
        w_in = self.w[p + "w_in"]
        W = 512
        vb = self.vecs[li][1]
        w3 = [A.alloc([128, 3, NCH, 128], BF16) for _ in range(2)]
        w3b = P.bufs("aw3", 2)
        qT = A.alloc([128, T], BF16)
        kT = A.alloc([128, T], BF16)
        V = A.alloc([128, T // 128, 128], BF16)
        qTb, kTb, Vb = P.buf("aqT"), P.buf("akT"), P.buf("aV")
        sqt = [A.alloc([128, W], BF16) for _ in range(2)]
        sqtb = P.bufs("asq", 2)
        rqt = [A.alloc([128, W], F32) for _ in range(2)]
        rqtb = P.bufs("arq", 2)
        gqs = A.alloc([128, 1], F32)
        gqsb = P.buf("agqs")
        km = A.alloc([128, 8], F32)
        kmb16 = A.alloc([128, 8], BF16)
        kmb = P.buf("akm")
        gm = A.alloc([128, 64], F32)
        gmb = P.buf("agm")
        top8 = A.alloc([128, 8], F32)
        top8b = P.buf("atop8")
        selb = A.alloc([128, 64], F32)
        selbb = P.buf("aselb")
        selT = A.alloc([8, 1024], BF16)
        selTb = P.buf("aselT")
        pT = [A.alloc([128, W], BF16) for _ in range(3)]
        pTb = P.bufs("apT", 3)
        rl = [A.alloc([128, 128], F32) for _ in range(2)]
        rlb = P.bufs("arl", 2)
        cba = A.alloc([8, NCONST_BF - 384], BF16)
        cbab = P.buf("acba")
        self.dma("pool", cba[:, :], self.constsb_d[0:8, 384:NCONST_BF], [], writes=[cbab], owner=cbab)
        self.ts("dve", gqs[:, :], self.vcol(li, "q_gain"), float(128 ** -0.5), None, ALU.mult, None,
                [vb], writes=[gqsb])
        cbb, cfb = self.cbb, self.cfb
        pk = 0
        for h in range(int(os.environ.get('NAH', A_HEADS))):
            s = h % 2
            for g, blk in enumerate((h, 8 + h, 16 + h)):
                srcw = w_in[:, blk * 128:(blk + 1) * 128].rearrange("(c q) n -> q c n", q=128)
                if g == 0:
                    self.dma("pool", w3[s][:, g, :, :], srcw, [], writes=[w3b[s]], owner=w3b[s])
                else:
                    self.dma("pool", w3[s][:, g, :, :], srcw, [], partial=[w3b[s]], owner=w3b[s])
            for tb in range(T // W):
                tsl = slice(tb * W, (tb + 1) * W)
                for g, (dst, dstb, gain) in enumerate(((qT, qTb, gqs[:, 0:1]),
                                                       (kT, kTb, self.vcol(li, "k_gain")))):
                    bk = self.next_bank()
                    for c in range(NCH):
                        self.mm(self.bank(bk), w3[s][:, g, c, :], xn[:, c, tsl], c == 0, c == NCH - 1,
                                [w3b[s], xnb], self.pb[bk])
                    z = pk % 2
                    pk += 1
                    self.act(sqt[z][:, :], self.bank(bk), AF.Square, [self.pb[bk]], writes=[sqtb[z]])
                    b2 = self.next_bank()
                    self.mm(self.bank(b2), self.ones_b, sqt[z][:, :], True, True, [sqtb[z], cbb],
                            self.pb[b2])
                    self.ts("dve", rqt[z][:, :], self.bank(b2), 1.0 / 128, RMS_EPS, ALU.mult, ALU.add,
                            [self.pb[b2]], writes=[rqtb[z]])
                    self.act(rqt[z][:, :], rqt[z][:, :], AF.Ln, [rqtb[z]], writes=[rqtb[z]])
                    self.act(rqt[z][:, :], rqt[z][:, :], AF.Exp, [rqtb[z]], writes=[rqtb[z]], scale=-0.5)
                    self.stt(dst[:, tsl], self.bank(bk), gain, rqt[z][:, :], ALU.mult, ALU.mult,
                             [self.pb[bk], rqtb[z], vb, gqsb], partial=[dstb])
            for g4 in range(4):
                bv = self.next_bank()
                for i in range(4):
                    tt = g4 * 4 + i
                    for c in range(NCH):
                        self.mm(self.bank(bv)[:, i * 128:(i + 1) * 128], xn[:, c, tt * 128:(tt + 1) * 128],
                                w3[s][:, 2, c, :], c == 0, c == NCH - 1, [w3b[s], xnb], self.pb[bv])
                self.copy("act", V[:, g4 * 4:(g4 + 1) * 4, :],
                          self.bank(bv).rearrange("p (i d) -> p i d", i=4), [self.pb[bv]], partial=[Vb])
            self.P.op("dve", lambda e: e.tensor_reduce(out=km[:, :],
                                                       in_=kT[:, :].rearrange("p (n s) -> p n s", n=8),
                                                       axis=AX.X, op=ALU.add),
                      reads=[kTb], writes=[kmb])
            self.copy("dve", kmb16[:, :], km[:, :], [kmb], writes=[kmb])
            bg = self.next_bank()
            for i in range(8):
                qt = 8 + i
                self.mm(self.bank(bg)[:, i * 8:(i + 1) * 8], qT[:, qt * 128:(qt + 1) * 128], kmb16[:, :],
                        True, True, [qTb, kmb], self.pb[bg])
            self.tt("dve", gm[:, :], self.bank(bg)[:, 0:64], self.cf[:, C_GMASK:C_GMASK + 64], ALU.add,
                    [self.pb[bg], cfb], writes=[gmb])
            for i in range(8):
                self.P.op("dve", (lambda o, a: (lambda e: e.max(out=o, in_=a)))(top8[:, :],
                                                                               gm[:, i * 8:(i + 1) * 8]),
                          reads=[gmb], writes=[top8b])
                self.ts("dve", selb[:, i * 8:(i + 1) * 8], gm[:, i * 8:(i + 1) * 8], top8[:, 2:3], -30000.0,
                        ALU.is_lt, ALU.mult, [gmb, top8b], partial=[selbb])
            for g2 in range(2):
                bt = self.next_bank()
                for i in range(4):
                    ii = g2 * 4 + i
                    self.tr(self.bank(bt)[0:8, i * 128:(i + 1) * 128], selb[:, ii * 8:(ii + 1) * 8],
                            self.ident_f, [selbb, cfb], self.pb[bt])
                self.copy("act", selT[0:8, g2 * 512:(g2 + 1) * 512], self.bank(bt)[0:8, :], [self.pb[bt]],
                          partial=[selTb])
            pending = []
            for qt in range(T // 128):
                qb = qt // 2
                bo = 4 + qt % 2
                bl = 6 + qt % 2
                qsl = slice(qt * 128, (qt + 1) * 128)
                for g0 in range(0, qt + 1, 4):
                    grp = list(range(g0, min(g0 + 4, qt + 1)))
                    bs = self.next_bank(0, 4)
                    for i, kt in enumerate(grp):
                        o = self.bank(bs)[:, i * 128:(i + 1) * 128]
                        extras = [(cba[0:1, B_DS - 384 + h * 128:B_DS - 384 + (h + 1) * 128],
                                   cba[0:1, B_DL - 384 + (qt - kt) * 128:B_DL - 384 + (qt - kt + 1) * 128], [cbab])]
                        if qb >= 4 and kt // 2 < qb:
                            kb = kt // 2
                            extras.append((cba[0:8, B_ONEHOT - 384 + kb * 128:B_ONEHOT - 384 + (kb + 1) * 128],
                                           selT[0:8, (qt - 8) * 128:(qt - 7) * 128], [cbab, selTb]))
                        if kt == qt:
                            extras.append((self.ident_b, self.cb[:, B_CAUSAL:B_CAUSAL + 128], [cbb]))
                        self.mm(o, kT[:, kt * 128:(kt + 1) * 128], qT[:, qsl], True, False, [kTb, qTb],
                                self.pb[bs])
                        for ei, (l_, r_, rd) in enumerate(extras):
                            self.mm(o, l_, r_, False, ei == len(extras) - 1, rd, self.pb[bs])
                    z = pk % 3
                    pk += 1
                    n_ = len(grp) * 128
                    self.act(pT[z][:, 0:n_], self.bank(bs)[:, 0:n_], AF.Exp, [self.pb[bs], cfb],
                             writes=[pTb[z]], bias=self.cf[:, C_BIASCOL + h:C_BIASCOL + h + 1])
                    for fn_ in pending:
                        fn_()
                    pending = []

                    def pv(grp=grp, z=z, bo=bo, bl=bl, qt=qt):
                        for i, kt in enumerate(grp):
                            self.mm(self.bank(bo)[:, 0:128], V[:, kt, :], pT[z][:, i * 128:(i + 1) * 128],
                                    kt == 0, kt == qt, [Vb, pTb[z]], self.pb[bo])
                            self.mm(self.bank(bl)[:, 0:128], self.ones_b, pT[z][:, i * 128:(i + 1) * 128],
                                    kt == 0, kt == qt, [cbb, pTb[z]], self.pb[bl])
                    pending.append(pv)

                def norm(qt=qt, bo=bo, bl=bl, qsl=qsl, h=h):
                    z2 = qt % 2
                    self.P.op("dve", (lambda o, a: (lambda e: e.reciprocal(out=o, in_=a)))(
                        rl[z2][:, :], self.bank(bl)[:, 0:128]), reads=[self.pb[bl]], writes=[rlb[z2]])
                    self.tt("dve", yT[:, h, qsl], self.bank(bo)[:, 0:128], rl[z2][:, :], ALU.mult,
                            [self.pb[bo], rlb[z2]], partial=[yTb])
                pending.append(norm)
            for fn_ in pending:
                fn_()

    def rwkv(self, li, xn, xnb, yT, yTb, m_xn, xn_bytes):
        A, P = self.A, self.P
        p = "l%d_" % li
        first = li == 0
        w_in = self.w[p + "w_in"]
        RW0 = 3072
        n = 256
        NC = 4
        vb = self.vecs[li][1]
        cfb, cbb = self.cfb, self.cbb
        bones = self.cf[:, C_BONES:C_BONES + 128]
        c0 = float(np.exp(-0.5))

        lw = A.alloc([128, 1024], BF16)
        lg0 = A.alloc([128, 1024], BF16)
        lg1 = A.alloc([64, 1024], BF16)
        lwb = P.buf("rlw")
        self.dma("pool", lw[0:64, :], self.w[p + "w_lora"], [], writes=[lwb], owner=lwb)
        self.dma("pool", lw[64:128, :], self.w[p + "a_lora"], [], partial=[lwb], owner=lwb)
        self.dma("pool", lg0[:, :], self.w[p + "g_lora"][0:128, :], [], partial=[lwb], owner=lwb)
        self.dma("pool", lg1[0:32, :], self.w[p + "g_lora"][128:160, :], [], partial=[lwb], owner=lwb)
        if not first:
            self.dma("pool", lg1[32:64, :], self.w[p + "v_lora"], [], partial=[lwb], owner=lwb)
        zwa = A.alloc([128, T], BF16)
        sg0 = A.alloc([128, T], BF16)
        sgv = A.alloc([64, T], BF16)
        zb = P.buf("rz")
        mz = A.mark()
        wz = [A.alloc([128, NCH, 128], BF16) for _ in range(2)]
        wzb = P.bufs("rwz", 2)
        zraw = [A.alloc([128, 513], F32) for _ in range(2)]
        zrawb = P.bufs("rzraw", 2)
        zd = [A.alloc([128, 512], F32) for _ in range(2)]
        zdb = P.bufs("rzd", 2)
        zk = 0
        nz2 = 32 if first else 64
        items = [("z", zi, 3072 + zi * 128, (128, 128, nz2)[zi], 24 + zi) for zi in range(3)]
        items += [("p", b, b * 128, 128, b) for b in range(24)]
        for ii, (kind, zi, coff, ncol, mui) in enumerate(items):
            col0 = RW0 + coff
            ws = ii % 2
            self.dma("pool", wz[ws][:, :, 0:ncol],
                     w_in[:, col0:col0 + ncol].rearrange("(c q) n -> q c n", q=128),
                     [], writes=[wzb[ws]], owner=wzb[ws])
            mu = self.vcol(li, "shift_mu", mui)
            for tb in range(4):
                q = zk % 2
                zk += 1
                tsl = slice(tb * 512, (tb + 1) * 512)
                bk = self.next_bank()
                for c in range(NCH):
                    self.mm(self.bank(bk)[0:ncol, :], wz[ws][:, c, 0:ncol], xn[:, c, tsl], c == 0, c == NCH - 1,
                            [wzb[ws], xnb], self.pb[bk])
                if tb == 0:
                    self.P.op("dve", (lambda t: (lambda e: e.memset(t, 0.0)))(zraw[q][0:ncol, 0:1]), reads=[],
                              partial=[zrawb[q]])
                else:
                    self.copy("act", zraw[q][0:ncol, 0:1], zraw[1 - q][0:ncol, 512:513], [zrawb[1 - q]],
                              partial=[zrawb[q]])
                self.copy("act", zraw[q][0:ncol, 1:513], self.bank(bk)[0:ncol, :], [self.pb[bk]],
                          writes=[zrawb[q]])
                self.tt("dve", zd[q][0:ncol, :], zraw[q][0:ncol, 0:512], zraw[q][0:ncol, 1:513], ALU.subtract,
                        [zrawb[q]], writes=[zdb[q]])
                self.stt(zd[q][0:ncol, :], zd[q][0:ncol, :], mu[0:ncol, :], zraw[q][0:ncol, 1:513], ALU.mult,
                         ALU.add, [zdb[q], zrawb[q], vb], writes=[zdb[q]])
                if kind == "p":
                    self.dma("sp", self.rkv[zi, :, tsl], zd[q][:, :], [zdb[q]], partial=[self.rkvb],
                             owner=self.rkvb)
                    if first and zi >= 16:
                        self.dma("sp", self.vfirst[zi - 16, :, tsl], zd[q][:, :], [zdb[q]],
                                 partial=[self.vfirstb], owner=self.vfirstb)
                elif zi == 0:
                    self.act(zwa[0:64, tsl], zd[q][0:64, :], AF.Tanh, [zdb[q]], partial=[zb])
                    self.copy("act", zwa[64:128, tsl], zd[q][64:128, :], [zdb[q]], partial=[zb])
                elif zi == 1:
                    self.act(sg0[:, tsl], zd[q][:, :], AF.Sigmoid, [zdb[q]], partial=[zb])
                else:
                    self.act(sgv[0:32, tsl], zd[q][0:32, :], AF.Sigmoid, [zdb[q]], partial=[zb])
                    if not first:
                        self.copy("act", sgv[32:64, tsl], zd[q][32:64, :], [zdb[q]], partial=[zb])
        def stream(Ax, hps):
            def f32(shape=(128, n)):
                return Ax.alloc(list(shape), F32)
            rr, kx, vv = f32(), f32(), f32()
            rrb, kxb, vvb = P.buf("rr"), P.buf("rk"), P.buf("rv")
            dtmp = f32()
            dtmpb = P.buf("rdt")
            sw, asig, gg, vs = f32(), f32(), f32(), f32()
            swb, asigb, ggb, vsb = P.buf("rsw"), P.buf("rasig"), P.buf("rgg"), P.buf("rvs")
            vf = f32()
            vfb_ = P.buf("rvf")
            kk, sq, rn = f32(), f32(), f32()
            kkb, sqb, rnb = P.buf("rkk"), P.buf("rsq"), P.buf("rrn")
            k2, bvec = f32(), f32()
            k2b, bvecb = P.buf("rk2"), P.buf("rbvec")
            Lp, Lx = f32(), f32()
            Lpb, Lxb = P.buf("rLp"), P.buf("rLx")
            Win, Wout, Wex, Wend = f32(), f32(), f32(), f32()
            Winb, Woutb, Wexb, Wendb = P.buf("rWin"), P.buf("rWout"), P.buf("rWex"), P.buf("rWend")
            AR = Ax.alloc([128, NC, 2, 64], F32)
            BK = Ax.alloc([128, NC, 2, 64], F32)
            ARb, BKb = P.buf("rAR"), P.buf("rBK")
            khat, bhat = Wex, Wout
            khatb, bhatb = Wexb, Woutb
            rk, bon = sq, f32()
            rkb, bonb = sqb, P.buf("rbon")
            Vtm = Ax.alloc([64, NC, 128], F32)
            Ktm = Ax.alloc([64, NC, 128], F32)
            Btm = Ax.alloc([64, NC, 128], F32)
            Vtmb, Ktmb, Btmb = P.buf("rVtm"), P.buf("rKtm"), P.buf("rBtm")
            Ak = Ax.alloc([64, 8, 128], F32)
            Ab = Ax.alloc([64, 8, 128], F32)
            NT = Ax.alloc([64, 8, 64], BF16)
            P0b = Ax.alloc([64, 8, 64], BF16)
            Akb, Abb, NTb, P0bb = P.buf("rAk"), P.buf("rAb"), P.buf("rNT"), P.buf("rP0b")
            Pw = [Ax.alloc([64, 8, 64], BF16) for _ in range(2)]
            PTw = [Ax.alloc([64, 8, 64], BF16) for _ in range(2)]
            Pwb, PTwb = P.bufs("rPw", 2), P.bufs("rPTw", 2)
            Tm = Ax.alloc([64, 8, 64], BF16)
            Tmb = P.buf("rTm")
            TmF = Ax.alloc([64, 8, 64], F32)
            TmFb = P.buf("rTmF")
            Xs = Ax.alloc([64, 128], F32)
            Us = Ax.alloc([64, 128], F32)
            Xsb, Usb = P.buf("rXs"), P.buf("rUs")
            S = Ax.alloc([128, 64], F32)
            Sb = P.buf("rS")
            yraw, yc, ysq, yrs = Lp, Lx, sq, rn
            yrawb, ycb, ysqb, yrsb = Lpb, Lxb, sqb, rnb
            ones64 = self.cf[:, C_ONE64:C_ONE64 + 64]
            amask = self.cf[0:64, C_AMASK:C_AMASK + 512]
            ntmask = self.cf[0:64, C_NTMASK:C_NTMASK + 512]
            id8 = self.cf[0:64, C_ID8:C_ID8 + 512]
            pk = 0
            for hp in hps:
                cols = slice(hp * 128, (hp + 1) * 128)
                self.P.op("dve", lambda e: e.memset(S[:, :], 0.0), reads=[], writes=[Sb])
                for tb in range(T // n):
                    q = pk % 2
                    pk += 1
                    tsl = slice(tb * n, (tb + 1) * n)
                    for g, (dst, dstb) in enumerate(((rr, rrb), (kx, kxb), (vv, vvb))):
                        self.dma("sp", dst[:, :], self.rkv[g * 8 + hp, :, tsl], [self.rkvb], writes=[dstb],
                                 owner=dstb)
                    yield
                    if RSTOP == 1:
                        return
                    bw = self.next_bank()
                    self.mm(self.bank(bw)[:, 0:n], lw[0:64, cols], zwa[0:64, tsl], True, True, [lwb, zb],
                            self.pb[bw])
                    self.act(sw[:, :], self.bank(bw)[:, 0:n], AF.Sigmoid, [self.pb[bw], vb], writes=[swb],
                             bias=self.vcol(li, "w0", hp))
                    ba = self.next_bank()
                    self.mm(self.bank(ba)[:, 0:n], lw[64:128, cols], zwa[64:128, tsl], True, True, [lwb, zb],
                            self.pb[ba])
                    self.act(asig[:, :], self.bank(ba)[:, 0:n], AF.Sigmoid, [self.pb[ba], vb], writes=[asigb],
                             bias=self.vcol(li, "a0", hp))
                    bg = self.next_bank()
                    self.mm(self.bank(bg)[:, 0:n], lg0[:, cols], sg0[:, tsl], True, False, [lwb, zb], self.pb[bg])
                    self.mm(self.bank(bg)[:, 0:n], lg1[0:32, cols], sgv[0:32, tsl], False, True, [lwb, zb],
                            self.pb[bg])
                    self.copy("act", gg[:, :], self.bank(bg)[:, 0:n], [self.pb[bg]], writes=[ggb])
                    yield
                    if RSTOP == 2:
                        return
                    if not first:
                        bv = self.next_bank()
                        self.mm(self.bank(bv)[:, 0:n], lg1[32:64, cols], sgv[32:64, tsl], True, True, [lwb, zb],
                                self.pb[bv])
                        self.act(vs[:, :], self.bank(bv)[:, 0:n], AF.Sigmoid, [self.pb[bv], vb], writes=[vsb],
                                 bias=self.vcol(li, "v0", hp))
                        self.dma("sp", vf[:, :], self.vfirst[hp, :, tsl], [self.vfirstb], writes=[vfb_],
                                 owner=vfb_)
                        self.tt("dve", dtmp[:, :], vf[:, :], vv[:, :], ALU.subtract, [vfb_, vvb], writes=[dtmpb])
                        self.tt("dve", dtmp[:, :], dtmp[:, :], vs[:, :], ALU.mult, [dtmpb, vsb], writes=[dtmpb])
                        self.tt("dve", vv[:, :], vv[:, :], dtmp[:, :], ALU.add, [vvb, dtmpb], writes=[vvb])
                    yield
                    if RSTOP == 3:
                        return
                    self.ts("dve", kk[:, :], kx[:, :], self.vcol(li, "k_k", hp), None, ALU.mult, None, [kxb, vb],
                            writes=[kkb])
                    self.act(sq[:, :], kk[:, :], AF.Square, [kkb], writes=[sqb])
                    bn = self.next_bank()
                    self.mm(self.bank(bn)[:, 0:n], bones, sq[:, :], True, True, [cfb, sqb], self.pb[bn])
                    self.act(rn[:, :], self.bank(bn)[:, 0:n], AF.Sqrt, [self.pb[bn]], writes=[rnb])
                    self.ts("dve", rn[:, :], rn[:, :], 1e-12, None, ALU.max, None, [rnb], writes=[rnb])
                    self.P.op("dve", lambda e: e.reciprocal(out=rn[:, :], in_=rn[:, :]), reads=[rnb], writes=[rnb])
                    self.tt("dve", kk[:, :], kk[:, :], rn[:, :], ALU.mult, [kkb, rnb], writes=[kkb])
                    self.ts("dve", dtmp[:, :], asig[:, :], -1.0, self.vcol(li, "k_a", hp), ALU.add, ALU.mult,
                            [asigb, vb], writes=[dtmpb])
                    self.stt(k2[:, :], dtmp[:, :], 1.0, kx[:, :], ALU.add, ALU.mult, [dtmpb, kxb], writes=[k2b])
                    self.tt("dve", bvec[:, :], kk[:, :], asig[:, :], ALU.mult, [kkb, asigb], writes=[bvecb])
                    yield
                    if RSTOP == 4:
                        return
                    for c in range(NC):
                        cs = slice(c * 64, (c + 1) * 64)
                        self.P.op("dve", (lambda o, d1: (lambda e: e.tensor_tensor_scan(
                            out=o, data0=ones64, data1=d1, initial=0.0, op0=ALU.mult, op1=ALU.add)))(
                            Lp[:, cs], sw[:, cs]), reads=[swb, cfb], partial=[Lpb])
                    self.act(Win[:, :], Lp[:, :], AF.Exp, [Lpb], writes=[Winb], scale=-c0)
                    self.act(Wout[:, :], Lp[:, :], AF.Exp, [Lpb], writes=[Woutb], scale=c0)
                    self.tt("dve", Lx[:, :], Lp[:, :], sw[:, :], ALU.subtract, [Lpb, swb], writes=[Lxb])
                    self.act(Wex[:, :], Lx[:, :], AF.Exp, [Lxb], writes=[Wexb], scale=-c0)
                    for c in range(NC):
                        cs = slice(c * 64, (c + 1) * 64)
                        self.ts("dve", Lx[:, cs], Lp[:, cs], -1.0, Lp[:, c * 64 + 63:c * 64 + 64], ALU.mult,
                                ALU.add, [Lpb, Wexb], partial=[Lxb])
                    self.act(Wend[:, :], Lx[:, :], AF.Exp, [Lxb], writes=[Wendb], scale=-c0)
                    yield
                    if RSTOP == 5:
                        return

                    def v3(t):
                        return t[:, :].rearrange("p (c s) -> p c s", c=NC)
                    self.stt(AR[:, :, 0, :], v3(kk), -1.0, v3(Wex), ALU.mult, ALU.mult, [kkb, Wexb], partial=[ARb])
                    self.tt("dve", AR[:, :, 1, :], v3(rr), v3(Win), ALU.mult, [rrb, Winb], partial=[ARb])
                    self.tt("dve", BK[:, :, 0, :], v3(bvec), v3(Wout), ALU.mult, [bvecb, Woutb], partial=[BKb])
                    self.tt("dve", BK[:, :, 1, :], v3(k2), v3(Wout), ALU.mult, [k2b, Woutb], partial=[BKb])
                    self.tt("dve", khat[:, :], k2[:, :], Wend[:, :], ALU.mult, [k2b, Wendb], writes=[khatb])
                    self.tt("dve", bhat[:, :], bvec[:, :], Wend[:, :], ALU.mult, [bvecb, Wendb], writes=[bhatb])
                    self.stt(rk[:, :], rr[:, :], self.vcol(li, "r_k", hp), k2[:, :], ALU.mult, ALU.mult,
                             [rrb, k2b, vb], writes=[rkb])
                    bb_ = self.next_bank()
                    self.mm(self.bank(bb_)[:, 0:n], bones, rk[:, :], True, True, [cfb, rkb], self.pb[bb_])
                    self.tt("dve", bon[:, :], self.bank(bb_)[:, 0:n], vv[:, :], ALU.mult, [self.pb[bb_], vvb],
                            writes=[bonb])
                    yield
                    if RSTOP == 6:
                        return
                    for (src_, srcb, dst_, dstb_) in ((vv, vvb, Vtm, Vtmb), (khat, khatb, Ktm, Ktmb),
                                                      (bhat, bhatb, Btm, Btmb)):
                        bt = self.next_bank()
                        for c in range(NC):
                            self.tr(self.bank(bt)[0:64, c * 128:(c + 1) * 128], src_[:, c * 64:(c + 1) * 64],
                                    self.ident_f, [srcb, cfb], self.pb[bt])
                        self.copy("act", dst_[:, :, :], self.bank(bt)[0:64, :].rearrange("p (c f) -> p c f", c=NC),
                                  [self.pb[bt]], writes=[dstb_])
                    yield
                    if RSTOP == 7:
                        return
                    for (dst_, dstb_, which) in ((Ak, Akb, 1), (Ab, Abb, 0)):
                        for hh in range(2):
                            hs = slice(hh * 64, hh * 64 + 64)
                            bsx = self.next_bank()
                            for c in range(NC):
                                self.mm(self.bank(bsx)[0:64, c * 128:(c + 1) * 128], BK[hs, c, which, :],
                                        AR[hs, c, :, :].rearrange("p a s -> p (a s)"), True, True, [BKb, ARb],
                                        self.pb[bsx])
                            if R8SUB == 1 or (R8SUB == 3 and hh == 1):
                                return
                            self.tt("dve", dst_[:, hh * 4:(hh + 1) * 4, :],
                                    self.bank(bsx)[0:64, :].rearrange("p (u f) -> p u f", u=4),
                                    amask.rearrange("p (u f) -> p u f", u=4), ALU.mult, [self.pb[bsx], cfb],
                                    partial=[dstb_])
                            if R8SUB == 2:
                                return
                    for hh in range(2):
                        hs = slice(hh * 64, hh * 64 + 64)
                        bsx = self.next_bank()
                        for c in range(NC):
                            self.mm(self.bank(bsx)[0:64, c * 64:(c + 1) * 64], AR[hs, c, 0, :], BK[hs, c, 0, :], True,
                                    True, [BKb, ARb], self.pb[bsx])
                        self.tt("dve", NT[:, hh * 4:(hh + 1) * 4, :],
                                self.bank(bsx)[0:64, 0:256].rearrange("p (u f) -> p u f", u=4),
                                ntmask[:, 0:256].rearrange("p (u f) -> p u f", u=4), ALU.mult, [self.pb[bsx], cfb],
                                partial=[NTb])
                    yield
                    if RSTOP == 8:
                        return
                    self.tt("dve", Tm[:, :, :], Ab[:, :, 0:64], id8.rearrange("p (u f) -> p u f", u=8), ALU.add,
                            [Abb, cfb], writes=[Tmb])
                    self.copy("act", P0b[:, :, :], Ab[:, :, 0:64], [Abb], writes=[P0bb])
                    curP = lambda u: P0b[:, u, :]
                    curPT = lambda u: NT[:, u, :]
                    curPb, curPTb = P0bb, NTb
                    for lv in range(5):
                        yield
                        z = lv % 2
                        last = lv == 4
                        bpt = self.next_bank()
                        for u in range(8):
                            self.mm(self.bank(bpt)[0:64, u * 64:(u + 1) * 64], curP(u), curPT(u), True, True,
                                    [curPb, curPTb], self.pb[bpt])
                        if not last:
                            bp = self.next_bank()
                            for u in range(8):
                                self.mm(self.bank(bp)[0:64, u * 64:(u + 1) * 64], curPT(u), curP(u), True, True,
                                        [curPb, curPTb], self.pb[bp])
                        self.copy("act", PTw[z][:, :, :], self.bank(bpt)[0:64, :].rearrange("p (u f) -> p u f", u=8),
                                  [self.pb[bpt]], writes=[PTwb[z]])
                        if not last:
                            self.copy("act", Pw[z][:, :, :], self.bank(bp)[0:64, :].rearrange("p (u f) -> p u f", u=8),
                                      [self.pb[bp]], writes=[Pwb[z]])
                        bT = self.next_bank()
                        for u in range(8):
                            self.mm(self.bank(bT)[0:64, u * 64:(u + 1) * 64], PTw[z][:, u, :], Tm[:, u, :], True, True,
                                    [PTwb[z], Tmb], self.pb[bT])
                        if not last:
                            self.tt("dve", Tm[:, :, :], self.bank(bT)[0:64, :].rearrange("p (u f) -> p u f", u=8),
                                    Tm[:, :, :], ALU.add, [self.pb[bT], Tmb], writes=[Tmb])
                        else:
                            self.tt("dve", TmF[:, :, :], self.bank(bT)[0:64, :].rearrange("p (u f) -> p u f", u=8),
                                    Tm[:, :, :], ALU.add, [self.pb[bT], Tmb], writes=[TmFb])
                        curP = (lambda zz: (lambda u: Pw[zz][:, u, :]))(z)
                        curPT = (lambda zz: (lambda u: PTw[zz][:, u, :]))(z)
                        curPb, curPTb = Pwb[z], PTwb[z]
                    yield
                    if RSTOP == 9:
                        return
                    for c in range(NC):
                        yield
                        for hh in range(2):
                            u = hh * 4 + c
                            hs = slice(hh * 64, hh * 64 + 64)
                            fs = slice(hh * 64, hh * 64 + 64)
                            bX = self.next_bank()
                            o = self.bank(bX)[0:64, 0:64]
                            if hh == 0:
                                self.mm(o, AR[hs, c, 0, :], S[hs, :], True, False, [ARb, Sb], self.pb[bX])
                                self.mm(o, Ak[:, u, 0:64], Vtm[:, c, fs], False, True, [Akb, Vtmb], self.pb[bX])
                                self.copy("act", Xs[:, fs], o, [self.pb[bX]], partial=[Xsb])
                            else:
                                self.mm(o, AR[hs, c, 0, :], S[hs, :], True, True, [ARb, Sb], self.pb[bX])
                                bX2 = self.next_bank()
                                o2 = self.bank(bX2)[0:64, 0:64]
                                self.mm(o2, Ak[:, u, 0:64], Vtm[:, c, fs], True, True, [Akb, Vtmb], self.pb[bX2])
                                self.copy("act", Xs[:, fs], o, [self.pb[bX]], partial=[Xsb])
                                self.tt("dve", Xs[:, fs], o2, Xs[:, fs], ALU.add, [self.pb[bX2], Xsb], partial=[Xsb])
                        bU = self.next_bank()
                        for hh in range(2):
                            u = hh * 4 + c
                            fs = slice(hh * 64, hh * 64 + 64)
                            self.mm(self.bank(bU)[0:64, fs], TmF[:, u, :], Xs[:, fs], True, True, [TmFb, Xsb],
                                    self.pb[bU])
                        self.copy("act", Us[:, :], self.bank(bU)[0:64, 0:128], [self.pb[bU]], writes=[Usb])
                        for hh in range(2):
                            u = hh * 4 + c
                            hs = slice(hh * 64, hh * 64 + 64)
                            fs = slice(hh * 64, hh * 64 + 64)
                            cs = slice(c * 64, (c + 1) * 64)
                            bY = self.next_bank()
                            bS = self.next_bank()
                            oy = self.bank(bY)[hs, 0:64]
                            os_ = self.bank(bS)[hs, 0:64]
                            if hh == 0:
                                self.mm(oy, S[hs, :], AR[hs, c, 1, :], True, False, [Sb, ARb], self.pb[bY])
                                self.mm(oy, Us[:, fs], Ab[:, u, 64:128], False, False, [Usb, Abb], self.pb[bY])
                                self.mm(oy, Vtm[:, c, fs], Ak[:, u, 64:128], False, True, [Vtmb, Akb], self.pb[bY])
                                self.copy("act", yraw[hs, cs], oy, [self.pb[bY]], partial=[yrawb])
                            else:
                                self.mm(oy, S[hs, :], AR[hs, c, 1, :], True, True, [Sb, ARb], self.pb[bY])
                                bY2 = self.next_bank()
                                oy2 = self.bank(bY2)[hs, 0:64]
                                self.mm(oy2, Us[:, fs], Ab[:, u, 64:128], True, False, [Usb, Abb], self.pb[bY2])
                                self.mm(oy2, Vtm[:, c, fs], Ak[:, u, 64:128], False, True, [Vtmb, Akb], self.pb[bY2])
                                self.copy("act", yraw[hs, cs], oy, [self.pb[bY]], partial=[yrawb])
                                self.tt("dve", yraw[hs, cs], oy2, yraw[hs, cs], ALU.add, [self.pb[bY2], yrawb],
                                        partial=[yrawb])
                            self.mm(os_, Btm[:, c, fs], Us[:, fs], True, False, [Btmb, Usb], self.pb[bS])
                            self.mm(os_, Ktm[:, c, fs], Vtm[:, c, fs], False, True, [Ktmb, Vtmb], self.pb[bS])
                            self.stt(S[hs, :], S[hs, :], Win[hs, c * 64 + 63:c * 64 + 64], os_,
                                     ALU.mult, ALU.add, [Sb, Winb, self.pb[bS]], partial=[Sb])
                    yield
                    if RSTOP == 10:
                        return
                    bm = self.next_bank()
                    self.mm(self.bank(bm)[:, 0:n], bones, yraw[:, :], True, True, [cfb, yrawb], self.pb[bm])
                    self.stt(yc[:, :], self.bank(bm)[:, 0:n], -1.0 / 64, yraw[:, :], ALU.mult, ALU.add,
                             [self.pb[bm], yrawb], writes=[ycb])
                    self.act(ysq[:, :], yc[:, :], AF.Square, [ycb], writes=[ysqb])
                    bv2 = self.next_bank()
                    self.mm(self.bank(bv2)[:, 0:n], bones, ysq[:, :], True, True, [cfb, ysqb], self.pb[bv2])
                    self.ts("dve", yrs[:, :], self.bank(bv2)[:, 0:n], 1.0 / 64, GN_EPS, ALU.mult, ALU.add,
                            [self.pb[bv2]], writes=[yrsb])
                    self.act(yrs[:, :], yrs[:, :], AF.Sqrt, [yrsb], writes=[yrsb])
                    self.P.op("dve", lambda e: e.reciprocal(out=yrs[:, :], in_=yrs[:, :]), reads=[yrsb],
                              writes=[yrsb])
                    self.tt("dve", yc[:, :], yc[:, :], yrs[:, :], ALU.mult, [ycb, yrsb], writes=[ycb])
                    self.ts("dve", yc[:, :], yc[:, :], self.vcol(li, "gn_w", hp), self.vcol(li, "gn_b", hp),
                            ALU.mult, ALU.add, [ycb, vb], writes=[ycb])
                    self.tt("dve", yc[:, :], yc[:, :], bon[:, :], ALU.add, [ycb, bonb], writes=[ycb])
                    self.tt("dve", yT[:, hp, tsl], yc[:, :], gg[:, :], ALU.mult, [ycb, ggb], partial=[yTb])

        A.reset(mz)
        P.fence()
        A2 = Arena(self.nc)
        A2.off = m_xn
        A2.limit = m_xn + xn_bytes
        nhp = int(os.environ.get('NHP', 8))
        gens = [stream(A2, [h for h in range(nhp) if h % 2 == 0]),
                stream(A, [h for h in range(nhp) if h % 2 == 1])]
        active = list(gens)
        while active:
            for g_ in list(active):
                try:
                    next(g_)
                except StopIteration:
                    active.remove(g_)
        self.A.peak = max(self.A.peak, A2.peak)

    def moba_rwkv(self, li, only=None):
        A, P = self.A, self.P
        m = A.mark()
        p = "l%d_" % li
        yT = A.alloc([128, 8, T], BF16)
        yTb = P.buf("eyT")
        m_xn = A.mark()
        xn = A.alloc([128, NCH, T], BF16)
        xnb = P.buf("exn")
        m1 = A.mark()
        xn_bytes = m1 - m_xn
        if only == "B":
            A.off = m1 + 70 * 1024
            self.rmsnorm(li, "norm_mix", xn, xnb, 0, T, "e")
            A.reset(m1)
            P.fence()
            self.rwkv(li, xn, xnb, yT, yTb, m_xn, xn_bytes)
            for h in range(int(os.environ.get('NHP', 8))):
                self.dump(yT[:, h, :], h * T, T, [yTb])
            A.reset(m)
            return
        self._attn_phase(li, xn, xnb, yT, yTb)
        if only == "A":
            for h in range(8):
                self.dump(yT[:, h, :], h * T, T, [yTb])
            A.reset(m)
            return
        A.reset(m1)
        P.fence()
        self.out_proj(self.w[p + "w_out"][0:1024, :], yT, yTb, 8, "ea")
        A.reset(m1)
        P.fence()
        self.rwkv(li, xn, xnb, yT, yTb, m_xn, xn_bytes)
        A.reset(m1)
        P.fence()
        self.out_proj(self.w[p + "w_out"][1024:2048, :], yT, yTb, 8, "eb")
        A.reset(m)

    def _attn_phase(self, li, xn, xnb, yT, yTb):
        A = self.A
        m = A.mark()
        top = A.mark()
        self.attention_alloc_only = True
        A.reset(top)
        A.off = top + 62 * 1024
        self.rmsnorm(li, "norm_mix", xn, xnb, 0, T, "e")
        A.reset(top)
        self.attention(li, xn, xnb, yT, yTb)
        assert A.off <= top + 62 * 1024, A.off - top
        A.reset(m)

    def mixer(self, li):
        if li % 2 == 1:
            self.sconv(li)
        else:
            self.moba_rwkv(li)


BIG_WEIGHTS = {
    0: [("w_in", (D, 6432)), ("w_out", (D, D)), ("w_lora", (64, 1024)), ("a_lora", (64, 1024)),
        ("g_lora", (160, 1024)), ("ffn_up", (D, 2 * DFF)), ("ffn_down", (DFF, D))],
    1: [("conv_in", (D, 3 * D)), ("conv_out", (D, D)), ("ffn_up", (D, 2 * DFF)), ("ffn_down", (DFF, D))],
    2: [("w_in", (D, 6464)), ("w_out", (D, D)), ("w_lora", (64, 1024)), ("a_lora", (64, 1024)),
        ("g_lora", (160, 1024)), ("v_lora", (32, 1024)), ("ffn_up", (D, 2 * DFF)), ("ffn_down", (DFF, D))],
    3: [("conv_in", (D, 3 * D)), ("conv_out", (D, D)), ("ffn_up", (D, 2 * DFF)), ("ffn_down", (DFF, D))],
}


def build_program(stages):
    B = Builder()
    layers = sorted(set(int(s[-1]) for s in stages))
    B.load_consts()
    for li in layers:
        B.load_vecs(li)
    for st in stages:
        li = int(st[-1])
        for nm, shp in BIG_WEIGHTS[li]:
            key = "l%d_%s" % (li, nm)
            if key not in B.w and ((st.startswith("ffn") and nm.startswith("ffn")) or
                                   (st.startswith("mix") and not nm.startswith("ffn"))):
                B.dram_in(key, shp)
    B.P.fence()
    B.prologue()
    for st in stages:
        li = int(st[-1])
        B.P.fence()
        if st.startswith("ffn"):
            B.ffn(li)
        elif st.startswith("mixA"):
            B.moba_rwkv(li, only="A")
        elif st.startswith("mixB"):
            B.moba_rwkv(li, only="B")
        else:
            B.mixer(li)
    B.P.fence()
    B.epilogue()
    B.P.emit(final_waits=[B.outb, B.dbgb])
    return B


def make_in_map(B, inputs, xb):
    cf, cb = build_consts()
    m = {"x": np.ascontiguousarray(xb, dtype=np.float32), "consts": cf, "constsb": cb}
    for li in B.vecs:
        m["vecs%d" % li] = build_layer_vecs(li, inputs)
    for key in B.w:
        m[key] = np.ascontiguousarray(inputs[key], dtype=np.float32)
    return m


ALL_STAGES = ["mix0", "ffn0", "mix1", "ffn1", "mix2", "ffn2", "mix3", "ffn3"]
_CACHE = {}


def kernel(**inputs):
    x = np.asarray(inputs["x"], dtype=np.float32)
    nb = x.shape[0]
    if "B" not in _CACHE:
        _CACHE["B"] = build_program(ALL_STAGES)
    B = _CACHE["B"]
    shared = make_in_map(B, inputs, x[0])
    in_maps = []
    for b in range(nb):
        m = dict(shared)
        m["x"] = np.ascontiguousarray(x[b])
        in_maps.append(m)
    res = run_bass_kernel_spmd(B.nc, in_maps, core_ids=list(range(nb)))
    out = np.stack([np.asarray(res.results[b]["out"], dtype=np.float32) for b in range(nb)], axis=0)
    return out
```
